# Optimizing a Trainium2 kernel written in Bass

```python
import jax
import jax.numpy as jnp
from jax import lax
import numpy as np


D_MODEL = 1024
BATCH = 4
SEQ = 8192
DEPTH = 2

GRID_W = 64
CTX_LEN = 256
HEAD_DIM = 64
ATT_Q_HEADS = 8
ATT_KV_HEADS = 2
ATT_GROUP = ATT_Q_HEADS // ATT_KV_HEADS
WINDOW = 128
ATT_BLOCK = 128
ROPE_BASE = 10000.0
ROPE_AXIS_DIM = HEAD_DIM // 2
RWKV_HEADS = 4
RWKV_WIDTH = RWKV_HEADS * HEAD_DIM
W_LORA = 32
A_LORA = 32
V_LORA = 32
G_LORA = 64
RWKV_GN_EPS = 64e-5
MLSTM_HEADS = 4
MLSTM_HEAD_DIM = 64
MLSTM_WIDTH = MLSTM_HEADS * MLSTM_HEAD_DIM
MLSTM_CHUNK = 64
MLSTM_GN_EPS = 1e-5
N_DIRS = 2
ATT_Q_W = ATT_Q_HEADS * HEAD_DIM
ATT_KV_W = ATT_KV_HEADS * HEAD_DIM
ATT_IN = ATT_Q_W + 2 * ATT_KV_W
RWKV_IN = 3 * RWKV_WIDTH + N_DIRS * (W_LORA + A_LORA) + G_LORA
RWKV_SPLITS = [RWKV_WIDTH, 2 * RWKV_WIDTH, 3 * RWKV_WIDTH, 3 * RWKV_WIDTH + N_DIRS * W_LORA, 3 * RWKV_WIDTH + N_DIRS * (W_LORA + A_LORA)]
MLSTM_GATES = N_DIRS * 2 * MLSTM_HEADS
MLSTM_IN = 4 * MLSTM_WIDTH + MLSTM_GATES
IN_WIDTH = ATT_IN + RWKV_IN + MLSTM_IN
MIX_WIDTH = ATT_Q_W + RWKV_WIDTH + MLSTM_WIDTH
N_EXPERTS = 32
TOP_K = 4
D_FF = D_MODEL
SWIGLU_LIMIT = 7.0
SWIGLU_ALPHA = 1.702
MOE_BLOCK = 256
N_MOD = 6
NORM_EPS = 1e-6
CONV_W = 3

kernel_name = "hybrid_rwkv7_mlstm_swa_moe_dit"


def rms_norm(x, g):
    xf = x.astype(jnp.float32)
    y = xf * lax.rsqrt(jnp.mean(xf * xf, axis=-1, keepdims=True) + NORM_EPS)
    return (y * g.astype(jnp.float32)).astype(x.dtype)


def modulate(h, shift, scale):
    return h * (1 + scale) + shift


def head_norm(y, w, b, eps):
    mu = jnp.mean(y, axis=-1, keepdims=True)
    yc = y - mu
    yn = yc * lax.rsqrt(jnp.mean(yc * yc, axis=-1, keepdims=True) + eps) * w.reshape(y.shape[-2:])
    return yn if b is None else yn + b.reshape(y.shape[-2:])


def heads(t, n_heads):
    return t.reshape(*t.shape[:-1], n_heads, t.shape[-1] // n_heads)


def conv3(x, w):
    xp = jnp.pad(x, ((0, 0), (1, 1), (0, 0)))
    return xp[:, :-2] * w[0] + xp[:, 1:-1] * w[1] + xp[:, 2:] * w[2]


def axial_rope_tables(T):
    rows = T // GRID_W
    row = jnp.repeat(jnp.arange(rows, dtype=jnp.float32), GRID_W)
    col = jnp.tile(jnp.arange(GRID_W, dtype=jnp.float32), rows)
    inv_freq = ROPE_BASE ** (-jnp.arange(0, ROPE_AXIS_DIM, 2, dtype=jnp.float32) / ROPE_AXIS_DIM)
    ang = jnp.concatenate([row[:, None] * inv_freq, col[:, None] * inv_freq], axis=-1)
    return jnp.cos(ang), jnp.sin(ang)


def apply_axial_rope(x, cos, sin):
    T = x.shape[1]
    half = ROPE_AXIS_DIM // 2
    xr = x.astype(jnp.float32).reshape(*x.shape[:-1], 2, 2, half)
    c = cos.reshape(T, 2, half)[None, :, None]
    s = sin.reshape(T, 2, half)[None, :, None]
    x1, x2 = xr[..., 0, :], xr[..., 1, :]
    out = jnp.stack([x1 * c - x2 * s, x2 * c + x1 * s], axis=-2)
    return out.reshape(x.shape).astype(x.dtype)


def sink_column(sink, lead_shape):
    return jnp.broadcast_to(sink.reshape(ATT_KV_HEADS, ATT_GROUP, 1, 1).astype(jnp.float32), lead_shape + (1,))


def window_attention(q, k, v, kc, vc, sink):
    B, T, _, d = q.shape
    Lc = kc.shape[1]
    nb = T // ATT_BLOCK
    span = ATT_BLOCK + 2 * WINDOW
    scale = d ** -0.5
    qb = jnp.moveaxis(q.reshape(B, nb, ATT_BLOCK, ATT_KV_HEADS, ATT_GROUP, d), 1, 0)
    kp = jnp.pad(k, ((0, 0), (WINDOW, WINDOW), (0, 0), (0, 0)))
    vp = jnp.pad(v, ((0, 0), (WINDOW, WINDOW), (0, 0), (0, 0)))

    def one_block(args):
        qblk, j = args
        start = j * ATT_BLOCK
        kb = lax.dynamic_slice_in_dim(kp, start, span, axis=1)
        vb = lax.dynamic_slice_in_dim(vp, start, span, axis=1)
        qpos = start + jnp.arange(ATT_BLOCK)
        kpos = start - WINDOW + jnp.arange(span)
        ok = (jnp.abs(qpos[:, None] - kpos[None, :]) <= WINDOW) & (kpos[None, :] >= 0) & (kpos[None, :] < T)
        s_loc = jnp.where(ok, jnp.einsum('bqkgd,bskd->bkgqs', qblk, kb).astype(jnp.float32) * scale, -jnp.inf)
        s_ctx = jnp.einsum('bqkgd,bskd->bkgqs', qblk, kc).astype(jnp.float32) * scale
        p = jax.nn.softmax(jnp.concatenate([s_loc, s_ctx, sink_column(sink, s_loc.shape[:-1])], axis=-1), axis=-1).astype(v.dtype)
        return (jnp.einsum('bkgqs,bskd->bqkgd', p[..., :span], vb)
                + jnp.einsum('bkgqs,bskd->bqkgd', p[..., span:span + Lc], vc))

    ob = lax.map(one_block, (qb, jnp.arange(nb)))
    return jnp.moveaxis(ob, 0, 1).reshape(B, T, ATT_Q_HEADS * d)


def context_attention(qc, kc, vc, sink):
    B, L, _, d = qc.shape
    qg = qc.reshape(B, L, ATT_KV_HEADS, ATT_GROUP, d)
    s = jnp.einsum('bqkgd,bskd->bkgqs', qg, kc).astype(jnp.float32) * d ** -0.5
    p = jax.nn.softmax(jnp.concatenate([s, sink_column(sink, s.shape[:-1])], axis=-1), axis=-1)[..., :L].astype(vc.dtype)
    return jnp.einsum('bkgqs,bskd->bqkgd', p, vc).reshape(B, L, ATT_Q_HEADS * d)


def attention_group(u, uc, sink, cos, sin, need_ctx):
    q, k, v = jnp.split(u, [ATT_Q_W, ATT_Q_W + ATT_KV_W], axis=-1)
    qc, kc, vc = jnp.split(uc, [ATT_Q_W, ATT_Q_W + ATT_KV_W], axis=-1)
    q = apply_axial_rope(heads(q, ATT_Q_HEADS), cos, sin)
    k = apply_axial_rope(heads(k, ATT_KV_HEADS), cos, sin)
    kc, vc = heads(kc, ATT_KV_HEADS), heads(vc, ATT_KV_HEADS)
    o = window_attention(q, k, heads(v, ATT_KV_HEADS), kc, vc, sink)
    oc = context_attention(heads(qc, ATT_Q_HEADS), kc, vc, sink) if need_ctx else None
    return o, oc


def bidirectional(scan_fn, lat_seqs, ctx_seqs, init, need_ctx):
    ys, ycs = [], []
    for d in range(N_DIRS):
        rev = d == 1
        flip = (lambda t: jnp.flip(t, axis=1)) if rev else (lambda t: t)
        yc, state = scan_fn(tuple(flip(t) for t in ctx_seqs[d]), init, need_ctx)
        y, _ = scan_fn(tuple(flip(t) for t in lat_seqs[d]), state, True)
        ys.append(flip(y))
        ycs.append(flip(yc) if need_ctx else None)
    return ys, ycs


def rwkv7_scan(seqs, state, emit):
    def step(S, inp):
        w_t, k_t, v_t, kk_t, a_t = inp[:5]
        sa = jnp.einsum('bhvk,bhk->bhv', S, -kk_t)
        S = (S * w_t[:, :, None, :] + sa[..., None] * (kk_t * a_t)[:, :, None, :]
             + v_t[..., None] * k_t[:, :, None, :])
        y = jnp.einsum('bhvk,bhk->bhv', S, inp[5]) if emit else None
        return S, y
    S, ys = lax.scan(step, state, tuple(jnp.moveaxis(t, 1, 0) for t in seqs))
    return (jnp.moveaxis(ys, 0, 1) if emit else None), S


def value_residual(h, v, v_first, v0, v_down, v_up):
    return v + (v_first - v) * jax.nn.sigmoid(v0 + (h @ v_down) @ v_up)


def rwkv_mixer(f, fc, w0, w_up, a0, a_up, g_up, k_k, k_a, r_k, ln_w, ln_b, need_ctx):
    f32 = jnp.float32

    def scan_inputs(feats, d, emit):
        r, k, v, xw, xa, _ = feats
        logit = (w0[d] + jnp.tanh(xw[..., d * W_LORA:(d + 1) * W_LORA]) @ w_up[d]).astype(f32)
        decay = jnp.exp(-jnp.exp(-jax.nn.softplus(-logit) - 0.5))
        a = jax.nn.sigmoid((a0[d] + xa[..., d * A_LORA:(d + 1) * A_LORA] @ a_up[d]).astype(f32))
        kk = heads((k * k_k).astype(f32), RWKV_HEADS)
        kk = kk / jnp.maximum(jnp.sqrt(jnp.sum(kk * kk, axis=-1, keepdims=True)), 1e-12)
        k_d = k.astype(f32) * (1.0 + (a - 1.0) * k_a)
        seqs = (heads(decay, RWKV_HEADS), heads(k_d, RWKV_HEADS), heads(v.astype(f32), RWKV_HEADS), kk, heads(a, RWKV_HEADS))
        return seqs + ((heads(r.astype(f32), RWKV_HEADS),) if emit else ())

    def readout(y, seqs):
        k_d, v, r = seqs[1], seqs[2], seqs[5]
        bonus = jnp.sum(r * k_d * r_k, axis=-1, keepdims=True) * v
        return head_norm(y, ln_w, ln_b, RWKV_GN_EPS) + bonus

    def merge(ys, seqs_list, xg):
        o = sum(readout(y, s) for y, s in zip(ys, seqs_list))
        o = o.reshape(*o.shape[:2], RWKV_WIDTH)
        return (o * (jax.nn.sigmoid(xg.astype(f32)) @ g_up)).astype(xg.dtype)

    lat = [scan_inputs(f, d, True) for d in range(N_DIRS)]
    ctx = [scan_inputs(fc, d, need_ctx) for d in range(N_DIRS)]
    init = jnp.zeros((f[0].shape[0], RWKV_HEADS, HEAD_DIM, HEAD_DIM), f32)
    ys, ycs = bidirectional(rwkv7_scan, lat, ctx, init, need_ctx)
    return merge(ys, lat, f[5]), (merge(ycs, ctx, fc[5]) if need_ctx else None)


def mlstm_chunk_scan(seqs, state, emit):
    k = seqs[0]
    B, T, H, _ = k.shape
    L = MLSTM_CHUNK
    nc = T // L

    def chunks(t):
        t = t.reshape(B, nc, L, H, *t.shape[3:])
        return jnp.moveaxis(jnp.moveaxis(t, 1, 0), 3, 2)

    tril = jnp.tril(jnp.ones((L, L), dtype=bool))

    def step(carry, inp):
        C, n, m = carry
        kc, vc, ic, fc = inp[:4]
        b = jnp.cumsum(fc, axis=-1)
        g = b[..., -1]
        lw = g[..., None] - b + ic
        m_new = jnp.maximum(g + m, jnp.max(lw, axis=-1))
        wk = jnp.exp(lw - m_new[..., None])
        keep = jnp.exp(g + m - m_new)
        C_new = keep[..., None, None] * C + jnp.einsum('bhs,bhsk,bhsv->bhkv', wk, kc, vc)
        n_new = keep[..., None] * n + jnp.einsum('bhs,bhsk->bhk', wk, kc)
        out = None
        if emit:
            qc = inp[4]
            dlog = jnp.where(tril, b[..., :, None] - b[..., None, :] + ic[..., None, :], -jnp.inf)
            m_inter = b + m[..., None]
            m_t = jnp.maximum(jnp.max(dlog, axis=-1), m_inter)
            w_inter = jnp.exp(m_inter - m_t)
            s = jnp.einsum('bhtk,bhsk->bhts', qc, kc) * jnp.exp(dlog - m_t[..., None])
            num = w_inter[..., None] * jnp.einsum('bhtk,bhkv->bhtv', qc, C) + jnp.einsum('bhts,bhsv->bhtv', s, vc)
            den = w_inter * jnp.einsum('bhtk,bhk->bht', qc, n) + jnp.sum(s, axis=-1)
            out = num / jnp.maximum(jnp.abs(den), jnp.exp(-m_t))[..., None]
        return (C_new, n_new, m_new), out

    state, hs = lax.scan(step, state, tuple(chunks(t) for t in seqs))
    if not emit:
        return None, state
    return jnp.moveaxis(jnp.moveaxis(hs, 2, 3), 0, 1).reshape(B, T, H, -1), state


def mlstm_features(u, conv_w):
    qk, v, o, g = jnp.split(u, [2 * MLSTM_WIDTH, 3 * MLSTM_WIDTH, 4 * MLSTM_WIDTH], axis=-1)
    q, k = jnp.split(jax.nn.silu(conv3(qk, conv_w)), 2, axis=-1)
    return (q, k, v, o, g)


def mlstm_mixer(f, fc, gate_b, ln_w, need_ctx):
    f32 = jnp.float32

    def scan_inputs(feats, d, emit):
        q, k, v, _, g = feats
        g = (g + gate_b).astype(f32).reshape(*g.shape[:-1], N_DIRS, 2, MLSTM_HEADS)
        seqs = (heads(k.astype(f32), MLSTM_HEADS) * MLSTM_HEAD_DIM ** -0.5, heads(v.astype(f32), MLSTM_HEADS),
                g[..., d, 0, :], jax.nn.log_sigmoid(g[..., d, 1, :]))
        return seqs + ((heads(q.astype(f32), MLSTM_HEADS),) if emit else ())

    def merge(ys, feats):
        hn = head_norm(sum(ys), ln_w, None, MLSTM_GN_EPS)
        hn = hn.reshape(*hn.shape[:2], MLSTM_WIDTH)
        return (jax.nn.sigmoid(feats[3].astype(f32)) * hn).astype(feats[3].dtype)

    B = f[0].shape[0]
    init = (jnp.zeros((B, MLSTM_HEADS, MLSTM_HEAD_DIM, MLSTM_HEAD_DIM), f32),
            jnp.zeros((B, MLSTM_HEADS, MLSTM_HEAD_DIM), f32), jnp.zeros((B, MLSTM_HEADS), f32))
    lat = [scan_inputs(f, d, True) for d in range(N_DIRS)]
    ctx = [scan_inputs(fc, d, need_ctx) for d in range(N_DIRS)]
    ys, ycs = bidirectional(mlstm_chunk_scan, lat, ctx, init, need_ctx)
    return merge(ys, f), (merge(ycs, fc) if need_ctx else None)


def expert_ffn(xb, w1, b1, w2, b2):
    glu, lin = jnp.split(xb @ w1 + b1, 2, axis=-1)
    glu = jnp.minimum(glu, SWIGLU_LIMIT)
    lin = jnp.clip(lin, -SWIGLU_LIMIT, SWIGLU_LIMIT)
    return (glu * jax.nn.sigmoid(SWIGLU_ALPHA * glu) * (lin + 1)) @ w2 + b2


def moe_ffn(h, router_w, router_b, w1, b1, w2, b2):
    T, D = h.shape
    logits = (h @ router_w + router_b).astype(jnp.float32)
    top_v, top_i = lax.top_k(logits, TOP_K)
    gates = jax.nn.softmax(top_v, axis=-1)
    n = T * TOP_K
    eid = top_i.reshape(n)
    tok = jnp.arange(n, dtype=jnp.int32) // TOP_K
    order = jnp.argsort(eid)
    eid_s, tok_s, g_s = eid[order], tok[order], gates.reshape(n)[order]
    counts = jnp.bincount(eid, length=N_EXPERTS)
    padded = (counts + MOE_BLOCK - 1) // MOE_BLOCK * MOE_BLOCK
    pend = jnp.cumsum(padded)
    pstart = pend - padded
    ustart = jnp.cumsum(counts) - counts
    dest = pstart[eid_s] + jnp.arange(n, dtype=jnp.int32) - ustart[eid_s]
    n_pad = (n + N_EXPERTS * (MOE_BLOCK - 1) + MOE_BLOCK - 1) // MOE_BLOCK * MOE_BLOCK
    nblk = n_pad // MOE_BLOCK
    ptok = jnp.full((n_pad,), T, jnp.int32).at[dest].set(tok_s)
    pgate = jnp.zeros((n_pad,), h.dtype).at[dest].set(g_s.astype(h.dtype))
    blk_e = jnp.minimum(jnp.searchsorted(pend, jnp.arange(nblk) * MOE_BLOCK, side='right'), N_EXPERTS - 1)
    hpad = jnp.concatenate([h, jnp.zeros((1, D), h.dtype)], axis=0)
    xb = hpad[ptok].reshape(nblk, MOE_BLOCK, D)

    def one_block(args):
        xblk, e = args
        return expert_ffn(xblk, w1[e], b1[e], w2[e], b2[e])

    yb = lax.map(one_block, (xb, blk_e)).reshape(n_pad, D) * pgate[:, None]
    return jnp.zeros((T + 1, D), h.dtype).at[ptok].add(yb)[:T]


def setup_inputs(seed: int = 0) -> dict:
    key = jax.random.key(seed)
    ks = iter(jax.random.split(key, 40))

    def nrm(shape, scale):
        return scale * jax.random.normal(next(ks), shape, jnp.float32)

    L = DEPTH
    conv_base = jnp.array([0.25, 0.5, 0.25], jnp.float32)[None, :, None]
    fg_base = jnp.linspace(3.0, 6.0, MLSTM_HEADS, dtype=jnp.float32)
    return {
        'x': nrm((BATCH, SEQ, D_MODEL), 1.0),
        'c': nrm((BATCH, D_MODEL), 1.0),
        'ctx': nrm((BATCH, CTX_LEN, D_MODEL), 1.0),
        'c_ctx': nrm((D_MODEL,), 1.0),
        'norm1_g': 1.0 + nrm((L, D_MODEL), 0.1),
        'norm2_g': 1.0 + nrm((L, D_MODEL), 0.1),
        'ada_w': nrm((L, D_MODEL, N_MOD * D_MODEL), 0.5 * D_MODEL ** -0.5),
        'ada_b': nrm((L, N_MOD * D_MODEL), 0.02),
        'w_in': nrm((L, D_MODEL, IN_WIDTH), D_MODEL ** -0.5),
        'w_out': nrm((L, MIX_WIDTH, D_MODEL), MIX_WIDTH ** -0.5),
        'attn_sink': nrm((L, ATT_Q_HEADS), 0.5),
        'rwkv_conv': conv_base + nrm((L, CONV_W, RWKV_IN), 0.1),
        'rwkv_w0': jnp.linspace(-6.0, -1.0, RWKV_WIDTH, dtype=jnp.float32) + nrm((L, N_DIRS, RWKV_WIDTH), 0.1),
        'rwkv_w_up': nrm((L, N_DIRS, W_LORA, RWKV_WIDTH), 0.1),
        'rwkv_a0': nrm((L, N_DIRS, RWKV_WIDTH), 0.1),
        'rwkv_a_up': nrm((L, N_DIRS, A_LORA, RWKV_WIDTH), 0.5 * A_LORA ** -0.5),
        'rwkv_g_up': nrm((L, G_LORA, RWKV_WIDTH), G_LORA ** -0.5),
        'rwkv_k_k': 0.85 + nrm((L, RWKV_WIDTH), 0.05),
        'rwkv_k_a': 1.0 + nrm((L, RWKV_WIDTH), 0.05),
        'rwkv_r_k': nrm((L, RWKV_HEADS, HEAD_DIM), 0.1),
        'rwkv_ln_w': 1.0 + nrm((L, RWKV_WIDTH), 0.1),
        'rwkv_ln_b': nrm((L, RWKV_WIDTH), 0.02),
        'rwkv_v0': 1.0 + nrm((L - 1, RWKV_WIDTH), 0.1),
        'rwkv_v_down': nrm((L - 1, D_MODEL, V_LORA), D_MODEL ** -0.5),
        'rwkv_v_up': nrm((L - 1, V_LORA, RWKV_WIDTH), 0.5 * V_LORA ** -0.5),
        'mlstm_conv': conv_base + nrm((L, CONV_W, 2 * MLSTM_WIDTH), 0.1),
        'mlstm_gate_b': jnp.concatenate([nrm((L, N_DIRS, 1, MLSTM_HEADS), 0.1),
                                         fg_base + nrm((L, N_DIRS, 1, MLSTM_HEADS), 0.1)], axis=2).reshape(L, MLSTM_GATES),
        'mlstm_ln_w': 1.0 + nrm((L, MLSTM_WIDTH), 0.1),
        'router_w': nrm((L, D_MODEL, N_EXPERTS), D_MODEL ** -0.5),
        'router_b': nrm((L, N_EXPERTS), 0.01),
        'moe_w1': nrm((L, N_EXPERTS, D_MODEL, 2 * D_FF), D_MODEL ** -0.5),
        'moe_b1': nrm((L, N_EXPERTS, 2 * D_FF), 0.02),
        'moe_w2': nrm((L, N_EXPERTS, D_FF, D_MODEL), D_FF ** -0.5),
        'moe_b2': nrm((L, N_EXPERTS, D_MODEL), 0.02),
        'final_norm_g': 1.0 + nrm((D_MODEL,), 0.1),
    }


def reference(x, c, ctx, c_ctx, norm1_g, norm2_g, ada_w, ada_b, w_in, w_out, attn_sink,
              rwkv_conv, rwkv_w0, rwkv_w_up, rwkv_a0, rwkv_a_up, rwkv_g_up, rwkv_k_k, rwkv_k_a,
              rwkv_r_k, rwkv_ln_w, rwkv_ln_b, rwkv_v0, rwkv_v_down, rwkv_v_up,
              mlstm_conv, mlstm_gate_b, mlstm_ln_w,
              router_w, router_b, moe_w1, moe_b1, moe_w2, moe_b2, final_norm_g):
    B, T, D = x.shape
    cos, sin = axial_rope_tables(T)
    xc = ctx
    v_first = v_first_c = None
    for l in range(DEPTH):
        need_ctx = l < DEPTH - 1
        mod = (jax.nn.silu(c) @ ada_w[l] + ada_b[l]).reshape(B, N_MOD, 1, D)
        mod_c = (jax.nn.silu(c_ctx) @ ada_w[l] + ada_b[l]).reshape(N_MOD, D)
        h = modulate(rms_norm(x, norm1_g[l]), mod[:, 0], mod[:, 1])
        hc = modulate(rms_norm(xc, norm1_g[l]), mod_c[0], mod_c[1])
        u_att, u_rw, u_ml = jnp.split(h @ w_in[l], [ATT_IN, ATT_IN + RWKV_IN], axis=-1)
        c_att, c_rw, c_ml = jnp.split(hc @ w_in[l], [ATT_IN, ATT_IN + RWKV_IN], axis=-1)
        o_att, oc_att = attention_group(u_att, c_att, attn_sink[l], cos, sin, need_ctx)
        f_rw = list(jnp.split(conv3(u_rw, rwkv_conv[l]), RWKV_SPLITS, axis=-1))
        fc_rw = list(jnp.split(conv3(c_rw, rwkv_conv[l]), RWKV_SPLITS, axis=-1))
        if l == 0:
            v_first, v_first_c = f_rw[2], fc_rw[2]
        else:
            f_rw[2] = value_residual(h, f_rw[2], v_first, rwkv_v0[l - 1], rwkv_v_down[l - 1], rwkv_v_up[l - 1])
            fc_rw[2] = value_residual(hc, fc_rw[2], v_first_c, rwkv_v0[l - 1], rwkv_v_down[l - 1], rwkv_v_up[l - 1])
        o_rw, oc_rw = rwkv_mixer(f_rw, fc_rw, rwkv_w0[l], rwkv_w_up[l], rwkv_a0[l], rwkv_a_up[l], rwkv_g_up[l],
                                 rwkv_k_k[l], rwkv_k_a[l], rwkv_r_k[l], rwkv_ln_w[l], rwkv_ln_b[l], need_ctx)
        o_ml, oc_ml = mlstm_mixer(mlstm_features(u_ml, mlstm_conv[l]), mlstm_features(c_ml, mlstm_conv[l]),
                                  mlstm_gate_b[l], mlstm_ln_w[l], need_ctx)
        x = x + mod[:, 2] * (jnp.concatenate([o_att, o_rw, o_ml], axis=-1) @ w_out[l])
        h2 = modulate(rms_norm(x, norm2_g[l]), mod[:, 3], mod[:, 4])
        if need_ctx:
            xc = xc + mod_c[2] * (jnp.concatenate([oc_att, oc_rw, oc_ml], axis=-1) @ w_out[l])
            h2c = modulate(rms_norm(xc, norm2_g[l]), mod_c[3], mod_c[4])
            y = moe_ffn(jnp.concatenate([h2.reshape(B * T, D), h2c.reshape(-1, D)], axis=0),
                        router_w[l], router_b[l], moe_w1[l], moe_b1[l], moe_w2[l], moe_b2[l])
            x = x + mod[:, 5] * y[:B * T].reshape(B, T, D)
            xc = xc + mod_c[5] * y[B * T:].reshape(xc.shape)
        else:
            y = moe_ffn(h2.reshape(B * T, D), router_w[l], router_b[l], moe_w1[l], moe_b1[l], moe_w2[l], moe_b2[l])
            x = x + mod[:, 5] * y.reshape(B, T, D)
    return rms_norm(x, final_norm_g)
```

```python
import numpy as np
import ml_dtypes
from contextlib import ExitStack
import concourse.bass as bass
import concourse.mybir as mybir
from concourse.bass_utils import run_bass_kernel_spmd

F32 = mybir.dt.float32
BF16 = mybir.dt.bfloat16
AF = mybir.ActivationFunctionType
ALU = mybir.AluOpType
AX = mybir.AxisListType

D = 1024
T = 8192
LC = 256
N = T + LC
NT = N // 128
DEPTH = 2
HD = 64
NE = 32
import os as _os0
SEM_ROT = int(_os0.environ.get('SEM_ROT', 20000))


class K:
    def __init__(self, nc, es):
        self.nc = nc
        self.es = es
        self.eng = {'pe': nc.tensor, 'act': nc.scalar, 'dve': nc.vector, 'pool': nc.gpsimd, 'sp': nc.sync}
        self.esems = {e: [es.enter_context(nc.semaphore(f"s_{e}_0"))] for e in ('pe', 'act', 'dve', 'pool')}
        self.ecnt = {e: 0 for e in self.esems}
        self.dsems = [es.enter_context(nc.semaphore(f"s_dma_{i}")) for i in range(44)]
        self.dcnt = [0] * len(self.dsems)
        self.drr = 0
        self.seen = {e: {} for e in self.eng}
        self.res = {}
        self.semobj = {}
        for e, l in self.esems.items():
            self.semobj[id(l[0])] = l[0]
        for s in self.dsems:
            self.semobj[id(s)] = s
        self.psb = [es.enter_context(nc.psum_tensor(f"psb{i}", [128, 512], F32)) for i in range(8)]
        self.psi = 0
        self.ninst = 0
        self.E1 = es.enter_context(nc.semaphore("s_bar1"))
        self.E2 = es.enter_context(nc.semaphore("s_bar2"))
        self.epoch = 0

    def psum(self):
        p = self.psb[self.psi % 8]
        self.psi += 1
        return p

    def _wait(self, e, sem, cnt):
        sid = id(sem)
        if self.seen[e].get(sid, 0) >= cnt:
            return
        self.eng[e].wait_ge(sem, cnt)
        self.seen[e][sid] = cnt
        self.ninst += 1

    def _deps(self, e, reads, writes, acc):
        for ap in reads:
            r = self.res.get(ap.name)
            if r and r['w']:
                self._wait(e, *r['w'])
        for ap in writes:
            r = self.res.get(ap.name)
            if not r:
                continue
            if r['w'] and not (acc and e == 'pe' and r.get('we') == 'pe'):
                self._wait(e, *r['w'])
            for sid, (sem, cnt) in r['r'].items():
                self._wait(e, sem, cnt)

    def _record(self, e, sem, cnt, reads, writes):
        for ap in reads:
            r = self.res.setdefault(ap.name, {'w': None, 'r': {}})
            r['r'][id(sem)] = (sem, cnt)
        for ap in writes:
            r = self.res.setdefault(ap.name, {'w': None, 'r': {}})
            r['w'] = (sem, cnt)
            r['we'] = e
            r['r'] = {}

    def op(self, e, fn, reads, writes, acc=False):
        self._deps(e, reads, writes, acc)
        ins = fn(self.eng[e])
        if self.ecnt[e] >= SEM_ROT:
            s = self.es.enter_context(self.nc.semaphore(f"s_{e}_{len(self.esems[e])}"))
            self.esems[e].append(s)
            self.semobj[id(s)] = s
            self.ecnt[e] = 0
        sem = self.esems[e][-1]
        self.ecnt[e] += 1
        ins.then_inc(sem, 1)
        self._record(e, sem, self.ecnt[e], reads, writes)
        self.ninst += 1
        return ins

    def dma(self, out, in_, q='sp', **kw):
        self._deps(q, [in_], [out], False)
        i = self.drr % len(self.dsems)
        self.drr += 1
        sem = self.dsems[i]
        if self.dcnt[i]:
            self._wait(q, sem, self.dcnt[i])
        ins = self.eng[q].dma_start(out=out, in_=in_, **kw)
        self.dcnt[i] += 16
        ins.then_inc(sem, 16)
        self._record(q, sem, self.dcnt[i], [in_], [out])
        self.ninst += 1

    def barrier(self):
        for e in self.eng:
            for e2, l in self.esems.items():
                if self.ecnt[e2]:
                    self._wait(e, l[-1], self.ecnt[e2])
            for i, s in enumerate(self.dsems):
                if self.dcnt[i]:
                    self._wait(e, s, self.dcnt[i])
        self.res = {}

    def mm(self, out, lhsT, rhs, start, stop):
        return self.op('pe', lambda E: E.matmul(out, lhsT, rhs, start=start, stop=stop), [lhsT, rhs], [out],
                       acc=not start)

    def tr(self, out, in_, ident):
        return self.op('pe', lambda E: E.transpose(out, in_, ident), [in_, ident], [out])

    def act(self, out, in_, func, bias=None, scale=None, accum_out=None, e='act'):
        kw = {}
        rd = [in_]
        wr = [out]
        if bias is not None:
            kw['bias'] = bias
            if not isinstance(bias, (int, float)):
                rd.append(bias)
        if scale is not None:
            kw['scale'] = scale
            if not isinstance(scale, (int, float)):
                rd.append(scale)
        if accum_out is not None:
            kw['accum_out'] = accum_out
            wr.append(accum_out)
        return self.op(e, lambda E: E.activation(out, in_, func, **kw), rd, wr)

    def tt(self, out, a, b, op, e='dve'):
        return self.op(e, lambda E: E.tensor_tensor(out, a, b, op), [a, b], [out])

    def ts(self, out, a, s1, s2, op0, op1=None, e='dve', accum_out=None):
        rd = [a] + [s for s in (s1, s2) if s is not None and not isinstance(s, (int, float))]
        wr = [out] + ([accum_out] if accum_out is not None else [])
        kw = {}
        if op1 is not None:
            kw['op1'] = op1
        if accum_out is not None:
            kw['accum_out'] = accum_out
        return self.op(e, lambda E: E.tensor_scalar(out, a, s1, s2, op0, **kw), rd, wr)

    def stt(self, out, a, s, b, op0, op1, e='dve'):
        rd = [a, b] + ([] if isinstance(s, (int, float)) else [s])
        return self.op(e, lambda E: E.scalar_tensor_tensor(out, a, s, b, op0, op1), rd, [out])

    def copy(self, out, in_, e='dve'):
        if e == 'act':
            return self.op(e, lambda E: E.copy(out, in_), [in_], [out])
        return self.op(e, lambda E: E.tensor_copy(out, in_), [in_], [out])

    def memset(self, out, v, e='pool'):
        return self.op(e, lambda E: E.memset(out, v), [], [out])


ATT_IN = 768
RW0 = 768
ML0 = 1728
NIN = 2768


def _rope_swap_idx():
    i = np.arange(64)
    blk = i // 16
    return np.where(blk % 2 == 0, i + 16, i - 16)


def build_colplan(layer):
    src = []
    cw = []
    groups = []

    def add(name, kind, cols, conv):
        base = len(src)
        nterm = 3 if conv else 1
        for j in range(nterm):
            for c in cols:
                src.append(c)
                if conv == 1:
                    cw.append((1, j, c - RW0))
                elif conv == 2:
                    cw.append((2, j, c - ML0))
                else:
                    cw.append((0, 0, 0))
        groups.append(dict(name=name, kind=kind, base=base, n=len(cols), nterm=nterm))

    sw = _rope_swap_idx()
    q = np.arange(512)
    qs = (q // 64) * 64 + sw[q % 64]
    kk = 512 + np.arange(128)
    ks = 512 + (np.arange(128) // 64) * 64 + sw[np.arange(128) % 64]
    add('attq', 'fm', list(q), 0)
    add('attqs', 'fm', list(qs), 0)
    add('attk', 'fm', list(kk), 0)
    add('attks', 'fm', list(ks), 0)
    add('rwx', 'fm', list(RW0 + 768 + np.arange(192)), 1)
    add('mlqk', 'fm', list(ML0 + np.arange(512)), 2)
    if layer > 0:
        add('vdown', 'fm', list(NIN + np.arange(32)), 0)
    add('attv', 'tm', list(640 + np.arange(128)), 0)
    add('rwrkv', 'tm', list(RW0 + np.arange(768)), 1)
    add('mlvog', 'tm', list(ML0 + 512 + np.arange(528)), 0)
    return np.array(src), cw, groups


_SBN = [0]


def sb(st, nc, name, shape, dt):
    _SBN[0] += 1
    return st.enter_context(nc.sbuf_tensor(f"{name}_u{_SBN[0]}", shape, dt))


def host_consts():
    c = {}
    c['ident_f'] = np.eye(128, dtype=np.float32)
    c['ident_b'] = np.eye(128).astype(ml_dtypes.bfloat16)
    t = np.arange(T)
    row = (t // 64).astype(np.float32)
    col = (t % 64).astype(np.float32)
    inv = (10000.0 ** (-np.arange(0, 32, 2, dtype=np.float32) / 32)).astype(np.float32)
    ang = np.concatenate([row[:, None] * inv, col[:, None] * inv], axis=-1).astype(np.float32)
    cos = np.cos(ang).astype(np.float32)
    sin = np.sin(ang).astype(np.float32)
    ct = np.ones((64, N), np.float32)
    stb = np.zeros((64, N), np.float32)
    for d in range(64):
        blk = d // 16
        f = (blk // 2) * 16 + d % 16
        ct[d, :T] = cos[:, f]
        stb[d, :T] = (-sin[:, f]) if blk % 2 == 0 else sin[:, f]
    c['rope_c'] = np.concatenate([ct, ct], 0)
    c['rope_s'] = np.concatenate([stb, stb], 0)
    c.update(att_consts())
    return c


class Prog:
    def __init__(self, nlayers=DEPTH, stop=None, dbg=()):
        self.nlayers = nlayers
        self.stop = stop
        self.dbg = dbg
        self.es = ExitStack()
        nc = self.nc = bass.Bass("TRN2", target_bir_lowering=False)
        self.k = K(nc, self.es)
        self.io = {}
        self.plans = [build_colplan(l) for l in range(DEPTH)]

    def din(self, name, shape, dt=F32):
        self.io[name] = self.nc.dram_tensor(name, list(shape), dt, kind="ExternalInput").ap()
        return self.io[name]

    def dscr(self, name, shape, dt=F32):
        kind = "ExternalOutput" if name in self.dbg else "Internal"
        self.io[name] = self.nc.dram_tensor(name, list(shape), dt, kind=kind).ap()
        return self.io[name]

    def declare(self):
        self.din('x0', [N, D])
        self.din('cc', [2, D])
        for nm in ('ident_f', 'rope_c', 'rope_s'):
            self.din(nm, {'ident_f': [128, 128], 'rope_c': [128, N], 'rope_s': [128, N]}[nm])
        self.din('ident_b', [128, 128], BF16)
        self.din('norm1_g', [DEPTH, D]); self.din('norm2_g', [DEPTH, D])
        self.din('ada_w', [DEPTH, D, 6 * D]); self.din('ada_b', [DEPTH, 6 * D])
        for l in range(DEPTH):
            ncw = len(self.plans[l][0])
            self.din(f'wx{l}', [D, ncw]); self.din(f'cs{l}', [1, ncw])
            self.dscr(f'modR{l}', [2, 6 * D])
        self.dscr('attq_d', [512, N]); self.dscr('attk_d', [128, N]); self.dscr('rwx_d', [192, N])
        self.dscr('mlqk_d', [512, N]); self.dscr('vdown_d', [32, N])
        self.dscr('attv_d', [N, 128]); self.dscr('rwrkv_d', [N, 768]); self.dscr('mlvog_d', [N, 528])
        self.dscr('xs', [N, D])
        self.dscr('mix_d', [N, D])
        self.din('mask_ge', [128, 512], BF16); self.din('mask_le', [128, 512], BF16)
        self.din('attn_sink', [DEPTH, 8])
        for nm, shp in (('rwkv_w0', [DEPTH, 2, 256]), ('rwkv_w_up', [DEPTH, 2, 32, 256]), ('rwkv_a0', [DEPTH, 2, 256]),
                        ('rwkv_a_up', [DEPTH, 2, 32, 256]), ('rwkv_g_up', [DEPTH, 64, 256]), ('rwkv_k_k', [DEPTH, 256]),
                        ('rwkv_k_a', [DEPTH, 256]), ('rwkv_r_k', [DEPTH, 4, 64]), ('rwkv_ln_w', [DEPTH, 256]),
                        ('rwkv_ln_b', [DEPTH, 256]), ('rwkv_v0', [1, 256]), ('rwkv_v_up', [1, 32, 256])):
            self.din(nm, shp)
        for nm, shp in (('vfirst_d', [N, 256]), ('rv_d', [N, 256]), ('rfm_d', [256, N]), ('kkfm_d', [256, N]), ('rgate_d', [N, 256]),
                        ('rw_d', [2, 256, N]), ('rkka_d', [2, N, 256]), ('rkd_d', [2, N, 256]), ('rbonus_d', [2, N, 256]),
                        ('ryscan_d', [2, N, 256])):
            self.dscr(nm, shp)
        for nm, shp in (('w_out', [DEPTH, D, D]), ('router_w', [DEPTH, D, NE]), ('router_b', [DEPTH, NE]), ('moe_w1', [DEPTH, NE, D, 2 * D]),
                        ('moe_b1', [DEPTH, NE, 2 * D]), ('moe_w2', [DEPTH, NE, D, D]), ('moe_b2', [DEPTH, NE, D]), ('final_norm_g', [D])):
            self.din(nm, shp)
        self.din('tri_le', [128, 128]); self.din('tri_ge', [128, 128])
        self.din('mlstm_gate_b', [DEPTH, 16]); self.din('mlstm_ln_w', [DEPTH, 256])

    def stage_M(self, l):
        k, nc, io = self.k, self.nc, self.io
        with ExitStack() as st:
            cT = sb(st, nc, 'cT', [128, 16], F32)
            adab = sb(st, nc, 'adab', [2, 6 * D], F32)
            gg = sb(st, nc, 'gg', [2, 2 * D], F32)
            mod = sb(st, nc, 'mod', [2, 6 * D], F32)
            R = sb(st, nc, 'R', [2, 6 * D], F32)
            wt = [sb(st, nc, f'adawt{i}', [128, 8, 512], F32) for i in range(2)]
            for r in range(2):
                k.dma(cT[:, r * 8:(r + 1) * 8], io['cc'][r, :].rearrange("(kc p) -> p kc", p=128),
                      allow_slow_non_contiguous=True)
            for r in range(2):
                k.dma(adab[r:r + 1, :], io['ada_b'][l:l + 1, :])
                k.dma(gg[r:r + 1, 0:D], io['norm1_g'][l:l + 1, :])
                k.dma(gg[r:r + 1, D:2 * D], io['norm2_g'][l:l + 1, :])
            k.act(cT[:], cT[:], AF.Silu)
            for ng in range(12):
                w = wt[ng % 2]
                k.dma(w[:], io['ada_w'][l, :, ng * 512:(ng + 1) * 512].rearrange("(kc p) n -> p kc n", p=128))
                ps = k.psum()
                for kc in range(8):
                    k.mm(ps[0:2, :], cT[:, kc:16:8], w[:, kc, :], kc == 0, kc == 7)
                k.tt(mod[:, ng * 512:(ng + 1) * 512], ps[0:2, :], adab[:, ng * 512:(ng + 1) * 512], ALU.add)
            m = lambda i: mod[:, i * D:(i + 1) * D]
            k.stt(R[:, 0:D], m(1), 1.0, gg[:, 0:D], ALU.add, ALU.mult)
            k.copy(R[:, D:2 * D], m(0))
            k.copy(R[:, 2 * D:3 * D], m(2))
            k.stt(R[:, 3 * D:4 * D], m(4), 1.0, gg[:, D:2 * D], ALU.add, ALU.mult)
            k.copy(R[:, 4 * D:5 * D], m(3))
            k.copy(R[:, 5 * D:6 * D], m(5))
            k.dma(io[f'modR{l}'], R[:], q='pool')
            k.barrier()

    def bcast_rows(self, st, l, idxs, tag):
        k, nc, io = self.k, self.nc, self.io
        out = {}
        for r in range(2):
            for i in idxs:
                t_ = sb(st, nc, f'bc_{tag}_{r}_{i}', [128, D], F32)
                k.dma(t_[:], io[f'modR{l}'][r, i * D:(i + 1) * D].partition_broadcast(128))
                out[(r, i)] = t_
        return out

    def norm_tile(self, xt, A, S, hb, tmp, junk, ssq, eps=1e-6):
        k = self.k
        k.act(junk[:], xt[:], AF.Square, accum_out=ssq[:, 0:1])
        k.act(ssq[:, 1:2], ssq[:, 0:1], AF.Sqrt, bias=eps, scale=1.0 / D)
        k.op('dve', lambda E: E.reciprocal(ssq[:, 2:3], ssq[:, 1:2]), [ssq[:]], [ssq[:]])
        k.stt(tmp[:], xt[:], ssq[:, 2:3], A[:], ALU.mult, ALU.mult)
        k.tt(hb[:], tmp[:], S[:], ALU.add, e='pool')

    def stage_A(self, l):
        k, nc, io = self.k, self.nc, self.io
        src, cw, groups = self.plans[l]
        ncw = len(src)
        xsrc = io['x0'] if l == 0 else io['xs']
        with ExitStack() as st:
            W = sb(st, nc, 'W_sb', [128, 8, ncw], BF16)
            identb = sb(st, nc, 'identb', [128, 128], BF16)
            k.dma(identb[:], io['ident_b'])
            with ExitStack() as st2:
                csb = sb(st2, nc, 'csb', [128, ncw], F32)
                stg = [sb(st2, nc, f'wstg{i}', [128, 512], F32) for i in range(3)]
                k.dma(csb[:], io[f'cs{l}'][0, :].partition_broadcast(128))
                i = 0
                for c0 in range(0, ncw, 512):
                    c1 = min(ncw, c0 + 512)
                    for kc in range(8):
                        s_ = stg[i % 3]
                        i += 1
                        k.dma(s_[:, 0:c1 - c0], io[f'wx{l}'][kc * 128:(kc + 1) * 128, c0:c1])
                        k.tt(W[:, kc, c0:c1], s_[:, 0:c1 - c0], csb[:, c0:c1], ALU.mult, e=('dve' if i % 2 else 'pool'))
                k.barrier()
            bc = self.bcast_rows(st, l, (0, 1), 'A')
            hT = [sb(st, nc, f'hT{i}', [128, 8, 514], BF16) for i in range(3)]
            xt = [sb(st, nc, f'xt{i}', [128, D], F32) for i in range(2)]
            tmp = sb(st, nc, 'ntmp', [128, D], F32)
            junk = sb(st, nc, 'njunk', [128, D], BF16)
            hb = [sb(st, nc, f'hb{i}', [128, D], BF16) for i in range(2)]
            ssq = [sb(st, nc, f'ssq{i}', [128, 4], F32) for i in range(2)]
            ropec = sb(st, nc, 'ropec', [128, 512], F32)
            ropes = sb(st, nc, 'ropes', [128, 512], F32)
            osb = [sb(st, nc, f'osb{i}', [128, 512], F32) for i in range(4)]
            rsw = sb(st, nc, 'rsw', [128, 512], F32)
            rt1 = sb(st, nc, 'rt1', [128, 512], F32)
            glist = [(g * 512, 512, 0) for g in range(16)] + [(T, 256, 1)]
            ocnt = [0]

            def nexto():
                ocnt[0] += 1
                return osb[ocnt[0] % 4]

            def make_hT(gi):
                t0, n, seg = glist[gi]
                buf = hT[gi % 3]
                for ti in range(n // 128):
                    x_ = xt[ti % 2]
                    k.dma(x_[:], xsrc[t0 + ti * 128:t0 + (ti + 1) * 128, :])
                    self.norm_tile(x_, bc[(seg, 0)], bc[(seg, 1)], hb[ti % 2], tmp, junk, ssq[ti % 2])
                    ps = k.psum()
                    pb = ps[:].bitcast(BF16)
                    for kc in range(8):
                        k.tr(pb[:, kc * 128:(kc + 1) * 128], hb[ti % 2][:, kc * 128:(kc + 1) * 128], identb[:])
                    k.copy(buf[:, :, 1 + ti * 128:1 + (ti + 1) * 128], pb[:, 0:1024].rearrange("p (kc t) -> p kc t", kc=8),
                           e='act')
                if gi > 0 and glist[gi - 1][2] == seg:
                    k.copy(buf[:, :, 0:1], hT[(gi - 1) % 3][:, :, 512:513], e='pool')
                    k.copy(hT[(gi - 1) % 3][:, :, 513:514], buf[:, :, 1:2], e='pool')
                else:
                    k.memset(buf[:, :, 0:1], 0.0)
                    if gi > 0:
                        k.memset(hT[(gi - 1) % 3][:, :, 513:514], 0.0)
                if gi == len(glist) - 1:
                    k.memset(buf[:, :, 1 + n:2 + n], 0.0)

            def proj(gi):
                t0, n, seg = glist[gi]
                buf = hT[gi % 3]
                k.dma(ropec[:, 0:n], io['rope_c'][:, t0:t0 + n])
                k.dma(ropes[:, 0:n], io['rope_s'][:, t0:t0 + n])
                G = {g['name']: g for g in groups}

                def fm_acc(g, c0, m):
                    ps = k.psum()
                    nt_ = g['nterm']
                    tot = nt_ * 8
                    ii = 0
                    for j in range(nt_):
                        sh = (j - 1) if nt_ == 3 else 0
                        for kc in range(8):
                            cb = g['base'] + j * g['n'] + c0
                            k.mm(ps[0:m, 0:n], W[:, kc, cb:cb + m], buf[:, kc, 1 + sh:1 + sh + n], ii == 0, ii == tot - 1)
                            ii += 1
                    return ps

                for (ga, gb, dst) in (('attq', 'attqs', 'attq_d'), ('attk', 'attks', 'attk_d')):
                    for c0 in range(0, G[ga]['n'], 128):
                        p1 = fm_acc(G[ga], c0, 128)
                        p2 = fm_acc(G[gb], c0, 128)
                        o = nexto()
                        k.tt(rt1[:, 0:n], p1[:, 0:n], ropec[:, 0:n], ALU.mult)
                        k.copy(rsw[:, 0:n], p2[:, 0:n], e='act')
                        k.tt(rsw[:, 0:n], rsw[:, 0:n], ropes[:, 0:n], ALU.mult, e='pool')
                        k.tt(o[:, 0:n], rt1[:, 0:n], rsw[:, 0:n], ALU.add)
                        k.dma(io[dst][c0:c0 + 128, t0:t0 + n], o[:, 0:n], q='pool')
                for (ga, dst, fn) in (('rwx', 'rwx_d', None), ('mlqk', 'mlqk_d', AF.Silu), ('vdown', 'vdown_d', None)):
                    if ga not in G:
                        continue
                    for c0 in range(0, G[ga]['n'], 128):
                        m = min(128, G[ga]['n'] - c0)
                        p1 = fm_acc(G[ga], c0, m)
                        o = nexto()
                        if fn is None:
                            k.copy(o[0:m, 0:n], p1[0:m, 0:n], e='act')
                        else:
                            k.act(o[0:m, 0:n], p1[0:m, 0:n], fn)
                        k.dma(io[dst][c0:c0 + m, t0:t0 + n], o[0:m, 0:n], q='pool')
                for ti in range(n // 128):
                    for (ga, dst) in (('attv', 'attv_d'), ('rwrkv', 'rwrkv_d'), ('mlvog', 'mlvog_d')):
                        g = G[ga]
                        for c0 in range(0, g['n'], 512):
                            w = min(512, g['n'] - c0)
                            ps = k.psum()
                            nt_ = g['nterm']
                            tot = nt_ * 8
                            ii = 0
                            for j in range(nt_):
                                sh = (j - 1) if nt_ == 3 else 0
                                for kc in range(8):
                                    cb = g['base'] + j * g['n'] + c0
                                    a = 1 + ti * 128 + sh
                                    k.mm(ps[:, 0:w], buf[:, kc, a:a + 128], W[:, kc, cb:cb + w], ii == 0, ii == tot - 1)
                                    ii += 1
                            o = nexto()
                            k.copy(o[:, 0:w], ps[:, 0:w], e=('act' if (ti + c0) % 2 else 'dve'))
                            k.dma(io[dst][t0 + ti * 128:t0 + (ti + 1) * 128, c0:c0 + w], o[:, 0:w], q='pool')

            make_hT(0)
            for gi in range(len(glist)):
                if gi + 1 < len(glist):
                    make_hT(gi + 1)
                proj(gi)
            k.barrier()


def host_inputs(inp, b, consts, plans):
    m = {}
    m['x0'] = np.concatenate([inp['x'][b], inp['ctx'][b]], axis=0)
    m['cc'] = np.stack([inp['c'][b], inp['c_ctx']], axis=0)
    m.update(consts)
    for nm in ('norm1_g', 'norm2_g', 'ada_w', 'ada_b', 'attn_sink', 'mlstm_gate_b', 'mlstm_ln_w', 'rwkv_w0', 'rwkv_w_up', 'rwkv_a0', 'rwkv_a_up', 'rwkv_g_up',
               'rwkv_k_k', 'rwkv_k_a', 'rwkv_r_k', 'rwkv_ln_w', 'rwkv_ln_b', 'rwkv_v0', 'rwkv_v_up',
               'w_out', 'router_w', 'router_b', 'moe_w1', 'moe_b1', 'moe_w2', 'moe_b2', 'final_norm_g'):
        m[nm] = inp[nm]
    ones = np.ones((1,), np.float32)
    for l in range(DEPTH):
        src, cw, groups = plans[l]
        wext = inp['w_in'][l] if l == 0 else np.concatenate([inp['w_in'][l], inp['rwkv_v_down'][l - 1]], axis=1)
        m[f'wx{l}'] = np.ascontiguousarray(wext[:, src])
        cs = np.empty((1, len(src)), np.float32)
        which = np.array([c[0] for c in cw]); jj = np.array([c[1] for c in cw]); cc_ = np.array([c[2] for c in cw])
        cs[0, which == 0] = ones[0]
        cs[0, which == 1] = inp['rwkv_conv'][l][jj[which == 1], cc_[which == 1]]
        cs[0, which == 2] = inp['mlstm_conv'][l][jj[which == 2], cc_[which == 2]]
        m[f'cs{l}'] = cs
    return m


def att_consts():
    j = np.arange(128)[:, None]
    i = np.arange(128)[None, :]
    ge = (j >= i).astype(np.float32)
    le = (j <= i).astype(np.float32)
    extra = {'tri_le': le, 'tri_ge': ge}
    return {**extra, 'mask_ge': np.tile(ge, (1, 4)).astype(ml_dtypes.bfloat16), 'mask_le': np.tile(le, (1, 4)).astype(ml_dtypes.bfloat16)}


def stage_ATT(self, l):
    k, nc, io = self.k, self.nc, self.io
    need_ctx = l < DEPTH - 1
    with ExitStack() as st:
        kTb = sb(st, nc, 'kTb', [128, N], BF16)
        vb = sb(st, nc, 'vb', [128, NT, 2, 65], BF16)
        mge = sb(st, nc, 'mge', [128, 512], BF16)
        mle = sb(st, nc, 'mle', [128, 512], BF16)
        esink = sb(st, nc, 'esink', [128, 8], F32)
        stg = [sb(st, nc, f'astg{i}', [128, 512], F32) for i in range(2)]
        qf = [sb(st, nc, f'qf{i}', [128, 512], F32) for i in range(2)]
        qb_ = [sb(st, nc, f'qb{i}', [128, 512], BF16) for i in range(2)]
        pt = [sb(st, nc, f'pt{i}', [128, 512], BF16) for i in range(6)]
        osb = [sb(st, nc, f'aosb{i}', [128, 512], F32) for i in range(2)]
        den = sb(st, nc, 'aden', [128, 8], F32)
        k.dma(mge[:], io['mask_ge'])
        k.dma(mle[:], io['mask_le'])
        k.dma(esink[:], io['attn_sink'][l, :].partition_broadcast(128))
        k.act(esink[:], esink[:], AF.Exp)
        k.memset(vb[:, :, :, 64:65], 1.0)
        for c in range(0, N, 512):
            w = min(512, N - c)
            s_ = stg[(c // 512) % 2]
            k.dma(s_[:, 0:w], io['attk_d'][:, c:c + w])
            k.copy(kTb[:, c:c + w], s_[:, 0:w], e='pool')
        for ti in range(NT):
            s_ = stg[ti % 2]
            k.dma(s_[:, 0:128], io['attv_d'][ti * 128:(ti + 1) * 128, :])
            k.copy(vb[:, ti, :, 0:64], s_[:, 0:128].rearrange("p (g d) -> p g d", g=2), e='dve')
        qblocks = list(range(64)) + ([64, 65] if need_ctx else [])
        pi = 0
        for n_, qb in enumerate(qblocks):
            if qb < 64:
                keys = [(kb, m) for kb, m in ((qb - 1, mge), (qb, None), (qb + 1, mle)) if 0 <= kb < 64] + [(64, None), (65, None)]
            else:
                keys = [(64, None), (65, None)]
            q_f, q_b, o_ = qf[n_ % 2], qb_[n_ % 2], osb[n_ % 2]
            for g in range(2):
                k.dma(q_f[g * 64:(g + 1) * 64, :].rearrange("p (h t) -> p h t", h=4),
                      io['attq_d'][g * 256:(g + 1) * 256, qb * 128:(qb + 1) * 128].rearrange("(h d) t -> d h t", d=64))
            k.copy(q_b[:], q_f[:], e='pool')
            for g in range(2):
                ptl = []
                for kb, m in keys:
                    ps = k.psum()
                    k.mm(ps[:, :], kTb[g * 64:(g + 1) * 64, kb * 128:(kb + 1) * 128], q_b[g * 64:(g + 1) * 64, :], True, True)
                    p_ = pt[pi % 6]
                    pi += 1
                    k.act(p_[:], ps[:, :], AF.Exp, scale=0.125)
                    if m is not None:
                        k.tt(p_[:], p_[:], m[:], ALU.mult, e='pool')
                    ptl.append((p_, kb))
                po = k.psum()
                for h in range(4):
                    for i_, (p_, kb) in enumerate(ptl):
                        k.mm(po[:, h * 65:(h + 1) * 65], p_[:, h * 128:(h + 1) * 128], vb[:, kb, g, :], i_ == 0, i_ == len(ptl) - 1)
                pv = po[:, 0:260].rearrange("p (h e) -> p h e", h=4)
                k.tt(den[:, g * 4:(g + 1) * 4], pv[:, :, 64], esink[:, g * 4:(g + 1) * 4], ALU.add)
                k.op('dve', lambda E: E.reciprocal(den[:, g * 4:(g + 1) * 4], den[:, g * 4:(g + 1) * 4]), [den[:]], [den[:]])
                for h in range(4):
                    hh = g * 4 + h
                    k.ts(o_[:, hh * 64:(hh + 1) * 64], po[:, h * 65:h * 65 + 64], den[:, hh:hh + 1], None, ALU.mult)
            k.dma(io['mix_d'][qb * 128:(qb + 1) * 128, 0:512], o_[:], q='pool')
        k.barrier()


Prog.stage_ATT = stage_ATT


def stage_ML(self, l):
    k, nc, io = self.k, self.nc, self.io
    need_ctx = l < DEPTH - 1
    with ExitStack() as st:
        ysum = sb(st, nc, 'ysum', [128, NT, 256], F32)
        tri = {0: sb(st, nc, 'tri_le', [128, 128], F32), 1: sb(st, nc, 'tri_ge', [128, 128], F32)}
        ones = sb(st, nc, 'ones_f', [128, 128], F32)
        identb = sb(st, nc, 'identb', [128, 128], BF16)
        gb = sb(st, nc, 'gb', [128, 16], F32)
        lnw = sb(st, nc, 'lnw', [128, 256], F32)
        C = sb(st, nc, 'Cst', [128, 2, 65], F32)
        Cb = sb(st, nc, 'Cstb', [128, 2, 65], BF16)
        qkf = [sb(st, nc, f'qkf{i}', [128, 4, 128], F32) for i in range(2)]
        qkb = [sb(st, nc, f'qkb{i}', [128, 4, 128], BF16) for i in range(2)]
        vf = [sb(st, nc, f'vf{i}', [128, 528], F32) for i in range(2)]
        va = [sb(st, nc, f'va{i}', [128, 4, 65], BF16) for i in range(2)]
        gt = [sb(st, nc, f'gt{i}', [128, 48], F32) for i in range(2)]
        kpp = [sb(st, nc, f'kpp{i}', [128, 128], BF16) for i in range(2)]
        pm = [sb(st, nc, f'pm{i}', [128, 128], BF16) for i in range(2)]
        sc = [sb(st, nc, f'sc{i}', [128, 8], F32) for i in range(2)]
        k.dma(tri[0][:], io['tri_le']); k.dma(tri[1][:], io['tri_ge']); k.dma(identb[:], io['ident_b'])
        k.memset(ones[:], 1.0)
        k.dma(gb[:], io['mlstm_gate_b'][l, :].partition_broadcast(128))
        k.dma(lnw[:], io['mlstm_ln_w'][l, :].partition_broadcast(128))
        it = 0
        for d in range(2):
            order = [64, 65] + list(range(64)) if d == 0 else [65, 64] + list(range(63, -1, -1))
            k.memset(C[:], 0.0)
            k.memset(Cb[:], 0.0)
            for ti in order:
                i2 = it % 2
                it += 1
                t0 = ti * 128
                qf_, qb_, vf_, va_, g_, s_ = qkf[i2], qkb[i2], vf[i2], va[i2], gt[i2], sc[i2]
                k.dma(qf_[:], io['mlqk_d'][:, t0:t0 + 128].rearrange("(s p) t -> p s t", p=128))
                k.dma(vf_[:], io['mlvog_d'][t0:t0 + 128, :])
                k.copy(qb_[:], qf_[:], e='pool')
                k.copy(va_[:, :, 0:64], vf_[:, 0:256].rearrange("p (h e) -> p h e", h=4), e='pool')
                k.memset(va_[:, :, 64:65], 1.0)
                k.tt(g_[:, 0:8], vf_[:, 512 + d * 8:512 + d * 8 + 8], gb[:, d * 8:d * 8 + 8], ALU.add)
                k.act(g_[:, 8:12], g_[:, 4:8], AF.Exp, scale=-1.0)
                k.act(g_[:, 8:12], g_[:, 8:12], AF.Ln, bias=1.0)
                k.ts(g_[:, 4:8], g_[:, 8:12], -1.0, None, ALU.mult)
                ps = k.psum()
                k.mm(ps[:, 0:4], tri[d][:], g_[:, 4:8], True, True)
                k.mm(ps[:, 4:8], ones[:], g_[:, 4:8], True, True)
                k.copy(g_[:, 12:20], ps[:, 0:8])
                k.tt(g_[:, 20:24], g_[:, 0:4], g_[:, 12:16], ALU.subtract)
                k.act(g_[:, 24:28], g_[:, 20:24], AF.Exp)
                k.ts(g_[:, 24:28], g_[:, 24:28], 0.125, None, ALU.mult)
                k.tt(g_[:, 28:32], g_[:, 20:24], g_[:, 16:20], ALU.add)
                k.act(g_[:, 28:32], g_[:, 28:32], AF.Exp)
                k.ts(g_[:, 28:32], g_[:, 28:32], 0.125, None, ALU.mult)
                k.act(g_[:, 32:36], g_[:, 12:16], AF.Exp)
                k.act(g_[:, 36:40], g_[:, 16:20], AF.Exp)
                for pr in range(2):
                    pT = k.psum()
                    pTb = pT[:].bitcast(BF16)
                    k.tr(pTb[:, 0:128], qb_[:, 2 + pr, :], identb[:])
                    kp = kpp[pr]
                    for hh in range(2):
                        h = pr * 2 + hh
                        k.ts(kp[:, hh * 64:(hh + 1) * 64], pTb[:, hh * 64:(hh + 1) * 64], g_[:, 28 + h:29 + h], None, ALU.mult,
                             e=('dve' if hh else 'pool') if False else 'dve')
                    for hh in range(2):
                        h = pr * 2 + hh
                        p0 = hh * 64
                        pS = k.psum()
                        k.mm(pS[:, 0:128], qb_[p0:p0 + 64, 2 + pr, :], qb_[p0:p0 + 64, pr, :], True, True)
                        pm_ = pm[hh]
                        k.stt(pm_[:], pS[:, 0:128], g_[:, 24 + h:25 + h], tri[d][:], ALU.mult, ALU.mult)
                        pO = k.psum()
                        k.mm(pO[:, 0:65], pm_[:], va_[:, h, :], True, False)
                        k.mm(pO[:, 0:65], qb_[p0:p0 + 64, pr, :], Cb[p0:p0 + 64, pr, :], False, True)
                        k.ts(s_[:, 0:1], pO[:, 64:65], g_[:, 32 + h:33 + h], None, ALU.mult)
                        k.act(s_[:, 0:1], s_[:, 0:1], AF.Abs)
                        k.ts(s_[:, 0:1], s_[:, 0:1], 1.0, None, ALU.max)
                        k.op('dve', lambda E: E.reciprocal(s_[:, 1:2], s_[:, 0:1]), [s_[:]], [s_[:]])
                        k.tt(s_[:, 2:3], s_[:, 1:2], g_[:, 32 + h:33 + h], ALU.mult)
                        yv = ysum[:, ti, h * 64:(h + 1) * 64]
                        if d == 0:
                            k.ts(yv, pO[:, 0:64], s_[:, 2:3], None, ALU.mult)
                        else:
                            k.stt(yv, pO[:, 0:64], s_[:, 2:3], yv, ALU.mult, ALU.add)
                    pC = k.psum()
                    k.mm(pC[:, 0:130], kp[:], va_[:, pr * 2:pr * 2 + 2, :], True, True)
                    for hh in range(2):
                        h = pr * 2 + hh
                        p0 = hh * 64
                        k.stt(C[p0:p0 + 64, pr, :], C[p0:p0 + 64, pr, :], g_[p0:p0 + 64, 36 + h:37 + h],
                              pC[p0:p0 + 64, hh * 65:(hh + 1) * 65], ALU.mult, ALU.add)
                    k.copy(Cb[:, pr, :], C[:, pr, :], e='pool')
        tiles = list(range(64)) + ([64, 65] if need_ctx else [])
        for n_, ti in enumerate(tiles):
            i2 = n_ % 2
            vf_, g_ = vf[i2], gt[i2]
            o_ = qkf[i2][:, 0:2, :]
            yc = qkf[i2][:, 2:4, :]
            k.dma(vf_[:, 0:256], io['mlvog_d'][ti * 128:(ti + 1) * 128, 256:512])
            k.act(vf_[:, 0:256], vf_[:, 0:256], AF.Sigmoid)
            k.op('dve', lambda E: E.reduce_sum(g_[:, 0:4], ysum[:, ti, :].rearrange("p (h e) -> p h e", h=4), AX.X),
                 [ysum[:]], [g_[:]])
            k.ts(g_[:, 0:4], g_[:, 0:4], -1.0 / 64, None, ALU.mult)
            for h in range(4):
                k.ts(yc[:, h // 2, (h % 2) * 64:(h % 2) * 64 + 64], ysum[:, ti, h * 64:(h + 1) * 64], g_[:, h:h + 1], None, ALU.add)
                k.act(o_[:, h // 2, (h % 2) * 64:(h % 2) * 64 + 64], yc[:, h // 2, (h % 2) * 64:(h % 2) * 64 + 64], AF.Square,
                      accum_out=g_[:, 4 + h:5 + h])
            k.act(g_[:, 8:12], g_[:, 4:8], AF.Sqrt, bias=1e-5, scale=1.0 / 64)
            k.op('dve', lambda E: E.reciprocal(g_[:, 12:16], g_[:, 8:12]), [g_[:]], [g_[:]])
            for h in range(4):
                k.stt(o_[:, h // 2, (h % 2) * 64:(h % 2) * 64 + 64], yc[:, h // 2, (h % 2) * 64:(h % 2) * 64 + 64], g_[:, 12 + h:13 + h],
                      lnw[:, h * 64:(h + 1) * 64], ALU.mult, ALU.mult)
            k.tt(vf_[:, 256:512], o_.rearrange("p a b -> p (a b)"), vf_[:, 0:256], ALU.mult, e='pool')
            k.dma(io['mix_d'][ti * 128:(ti + 1) * 128, 768:1024], vf_[:, 256:512], q='pool')
        k.barrier()


Prog.stage_ML = stage_ML


def stage_RW(self, l):
    k, nc, io = self.k, self.nc, self.io
    need_ctx = l < DEPTH - 1
    with ExitStack() as st:
        identf = sb(st, nc, 'identf', [128, 128], F32)
        k.dma(identf[:], io['ident_f'])
        bc = {}
        for nm, src in (('k_k', io['rwkv_k_k'][l, :]), ('k_a', io['rwkv_k_a'][l, :]), ('r_k', io['rwkv_r_k'][l].rearrange("h e -> (h e)")),
                        ('a0_0', io['rwkv_a0'][l, 0, :]), ('a0_1', io['rwkv_a0'][l, 1, :])) + \
                (() if l == 0 else (('v0', io['rwkv_v0'][l - 1, :]),)):
            bc[nm] = sb(st, nc, 'rbc_' + nm, [128, 256], F32)
            k.dma(bc[nm][:], src.partition_broadcast(128))
        up = sb(st, nc, 'rw_up', [128, 2, 256], F32)
        gup = sb(st, nc, 'rw_gup', [64, 256], F32)
        vup = sb(st, nc, 'rw_vup', [32, 256], F32)
        w0c = sb(st, nc, 'rw_w0c', [128, 4], F32)
        for d in range(2):
            k.dma(up[d * 32:(d + 1) * 32, 0, :], io['rwkv_w_up'][l, d])
            k.dma(up[d * 32:(d + 1) * 32, 1, :], io['rwkv_a_up'][l, d])
            k.dma(w0c[:, d * 2:d * 2 + 2], io['rwkv_w0'][l, d, :].rearrange("(c p) -> p c", p=128), allow_slow_non_contiguous=True)
        k.dma(gup[:], io['rwkv_g_up'][l])
        if l > 0:
            k.dma(vup[:], io['rwkv_v_up'][l - 1])
        NB = 2
        xr = [sb(st, nc, f'rxr{i}', [128, 768], F32) for i in range(NB)]
        xf = [sb(st, nc, f'rxf{i}', [64, 128], F32) for i in range(NB)]
        xaf = [sb(st, nc, f'rxaf{i}', [64, 128], F32) for i in range(NB)]
        gf = [sb(st, nc, f'rgf{i}', [64, 128], F32) for i in range(NB)]
        lvf = [sb(st, nc, f'rlv{i}', [32, 128], F32) for i in range(NB)]
        vfst = [sb(st, nc, f'rvf{i}', [128, 256], F32) for i in range(NB)]
        kk = [sb(st, nc, f'rkk{i}', [128, 256], F32) for i in range(NB)]
        t1 = [sb(st, nc, f'rt1{i}', [128, 256], F32) for i in range(4)]
        t2 = [sb(st, nc, f'rt2{i}', [128, 256], F32) for i in range(4)]
        sm = [sb(st, nc, f'rsm{i}', [128, 16], F32) for i in range(NB)]
        fo = [sb(st, nc, f'rfo{i}', [128, 128], F32) for i in range(4)]
        cnt = [0]

        def T1():
            cnt[0] += 1
            return t1[cnt[0] % 4]

        def T2():
            cnt[0] += 1
            return t2[cnt[0] % 4]

        def FO():
            cnt[0] += 1
            return fo[cnt[0] % 4]

        for ti in range(NT):
            i2 = ti % NB
            t0 = ti * 128
            x_, xf_, gf_, kk_, sm_ = xr[i2], xf[i2], gf[i2], kk[i2], sm[i2]
            k.dma(x_[:], io['rwrkv_d'][t0:t0 + 128, :])
            k.dma(xf_[:], io['rwx_d'][0:64, t0:t0 + 128])
            k.dma(xaf[i2][:], io['rwx_d'][64:128, t0:t0 + 128])
            k.dma(gf_[:], io['rwx_d'][128:192, t0:t0 + 128])
            r_, k_, v_ = x_[:, 0:256], x_[:, 256:512], x_[:, 512:768]
            if l == 0:
                k.dma(io['vfirst_d'][t0:t0 + 128, :], v_, q='pool')
            else:
                k.dma(lvf[i2][:], io['vdown_d'][:, t0:t0 + 128])
                k.dma(vfst[i2][:], io['vfirst_d'][t0:t0 + 128, :])
                ps = k.psum()
                k.mm(ps[:, 0:256], lvf[i2][:], vup[:], True, True)
                a = T1()
                k.tt(a[:], ps[:, 0:256], bc['v0'][:], ALU.add)
                k.act(a[:], a[:], AF.Sigmoid)
                b_ = T2()
                k.tt(b_[:], vfst[i2][:], v_, ALU.subtract)
                k.tt(b_[:], b_[:], a[:], ALU.mult, e='pool')
                k.tt(v_, v_, b_[:], ALU.add)
            k.dma(io['rv_d'][t0:t0 + 128, :], v_, q='pool')
            k.tt(kk_[:], k_, bc['k_k'][:], ALU.mult)
            a = T1()
            for h in range(4):
                k.act(a[:, h * 64:(h + 1) * 64], kk_[:, h * 64:(h + 1) * 64], AF.Square, accum_out=sm_[:, h:h + 1])
            k.act(sm_[:, 4:8], sm_[:, 0:4], AF.Sqrt)
            k.ts(sm_[:, 4:8], sm_[:, 4:8], 1e-12, None, ALU.max)
            k.op('dve', lambda E: E.reciprocal(sm_[:, 8:12], sm_[:, 4:8]), [sm_[:]], [sm_[:]])
            for h in range(4):
                k.ts(kk_[:, h * 64:(h + 1) * 64], kk_[:, h * 64:(h + 1) * 64], sm_[:, 8 + h:9 + h], None, ALU.mult)
            for src_, dst in ((r_, 'rfm_d'), (kk_[:], 'kkfm_d')):
                for c in range(2):
                    ps = k.psum()
                    k.tr(ps[:, 0:128], src_[:, c * 128:(c + 1) * 128], identf[:])
                    f_ = FO()
                    k.copy(f_[:], ps[:, 0:128], e='act')
                    k.dma(io[dst][c * 128:(c + 1) * 128, t0:t0 + 128], f_[:], q='pool')
            k.act(gf_[:], gf_[:], AF.Sigmoid)
            ps = k.psum()
            k.mm(ps[:, 0:256], gf_[:], gup[:], True, True)
            a = T1()
            k.copy(a[:], ps[:, 0:256], e='act')
            k.dma(io['rgate_d'][t0:t0 + 128, :], a[:], q='pool')
            k.act(xf_[0:64, :], xf_[0:64, :], AF.Tanh)
            for d in range(2):
                for c in range(2):
                    ps = k.psum()
                    k.mm(ps[:, 0:128], up[d * 32:(d + 1) * 32, 0, c * 128:(c + 1) * 128], xf_[d * 32:(d + 1) * 32, :], True, True)
                    f_ = FO()
                    k.act(f_[:], ps[:, 0:128], AF.Sigmoid, bias=w0c[:, d * 2 + c:d * 2 + c + 1])
                    k.act(f_[:], f_[:], AF.Exp, scale=-0.6065306597126334)
                    k.dma(io['rw_d'][d, c * 128:(c + 1) * 128, t0:t0 + 128], f_[:], q='pool')
                ps = k.psum()
                k.mm(ps[:, 0:256], xaf[i2][d * 32:(d + 1) * 32, :], up[d * 32:(d + 1) * 32, 1, :], True, True)
                a = T1()
                k.tt(a[:], ps[:, 0:256], bc[f'a0_{d}'][:], ALU.add)
                k.act(a[:], a[:], AF.Sigmoid)
                b_ = T2()
                k.tt(b_[:], kk_[:], a[:], ALU.mult, e='pool')
                k.dma(io['rkka_d'][d, t0:t0 + 128, :], b_[:], q='pool')
                c_ = T2()
                k.stt(c_[:], a[:], -1.0, bc['k_a'][:], ALU.add, ALU.mult)
                k.stt(c_[:], c_[:], 1.0, k_, ALU.add, ALU.mult)
                k.dma(io['rkd_d'][d, t0:t0 + 128, :], c_[:], q='pool')
                e_ = T1()
                k.tt(e_[:], c_[:], bc['r_k'][:], ALU.mult, e='pool')
                k.tt(e_[:], e_[:], r_, ALU.mult)
                k.op('dve', lambda E: E.reduce_sum(sm_[:, 12:16], e_[:].rearrange("p (h e) -> p h e", h=4), AX.X), [e_[:]], [sm_[:]])
                f2 = T2()
                for h in range(4):
                    k.ts(f2[:, h * 64:(h + 1) * 64], v_[:, h * 64:(h + 1) * 64], sm_[:, 12 + h:13 + h], None, ALU.mult)
                k.dma(io['rbonus_d'][d, t0:t0 + 128, :], f2[:], q='pool')
        k.barrier()
    SB = 8
    with ExitStack() as st:
        S = [sb(st, nc, f'rS{g}', [128, 2, 64], F32) for g in range(2)]
        NBUF = 2
        mk = lambda nm, shp: [[sb(st, nc, f'{nm}{g}_{i}', shp, F32) for i in range(NBUF)] for g in range(2)]
        LAk = [mk(f'LAk{d}', [128, SB, 2]) for d in range(2)]
        LAr = [mk(f'LAr{d}', [128, SB, 2]) for d in range(2)]
        Wt = [mk(f'Wt{d}', [128, SB]) for d in range(2)]
        KA = [mk(f'KA{d}', [2, SB, 128]) for d in range(2)]
        KD = [mk(f'KD{d}', [2, SB, 128]) for d in range(2)]
        VV = [mk(f'VV{d}', [2, SB, 64]) for d in range(2)]
        YY = [mk(f'YY{d}', [2, SB, 64]) for d in range(2)]
        SA = mk('SA', [2, SB, 128])
        ftmp = [sb(st, nc, f'rftmp{i}', [128, SB], F32) for i in range(8)]
        for g in range(2):
            k.memset(S[g][:], 0.0)
            for d in range(2):
                for bi_ in range(NBUF):
                    for tl in (LAk, LAr, KA, KD):
                        k.memset(tl[d][g][bi_][:], 0.0)
        import os as _os
        nblk = int(_os.environ.get('RW_NBLK', N // SB))
        fc = [0]

        def tokrange(blk, d):
            s0 = blk * SB
            if s0 < LC:
                return T + s0 if d == 0 else T + LC - s0 - SB
            return s0 - LC if d == 0 else T - (s0 - LC) - SB

        def load_block(blk, g):
            bi = blk % NBUF
            for d in range(2):
                tk = tokrange(blk, d)
                for (src_, LA, sgn) in ((io['kkfm_d'], LAk, -1.0), (io['rfm_d'], LAr, 1.0)):
                    f_ = ftmp[fc[0] % 8]
                    fc[0] += 1
                    k.dma(f_[:], src_[g * 128:(g + 1) * 128, tk:tk + SB])
                    la = LA[d][g][bi]
                    for hh in range(2):
                        k.ts(la[hh * 64:(hh + 1) * 64, :, hh], f_[hh * 64:(hh + 1) * 64, :], sgn, None, ALU.mult, e='pool')
                k.dma(Wt[d][g][bi][:], io['rw_d'][d, g * 128:(g + 1) * 128, tk:tk + SB])
                for hh in range(2):
                    c0 = (g * 2 + hh) * 64
                    k.dma(KA[d][g][bi][hh:hh + 1, :, hh * 64:(hh + 1) * 64], io['rkka_d'][d, tk:tk + SB, c0:c0 + 64])
                    k.dma(KD[d][g][bi][hh:hh + 1, :, hh * 64:(hh + 1) * 64], io['rkd_d'][d, tk:tk + SB, c0:c0 + 64])
                    k.dma(VV[d][g][bi][hh:hh + 1, :, :], io['rv_d'][tk:tk + SB, c0:c0 + 64])

        def store_block(blk, g):
            bi = blk % NBUF
            for d in range(2):
                tk = tokrange(blk, d)
                for hh in range(2):
                    c0 = (g * 2 + hh) * 64
                    k.dma(io['ryscan_d'][d, tk:tk + SB, c0:c0 + 64], YY[d][g][bi][hh:hh + 1, :, :], q='pool')

        for g in range(2):
            load_block(0, g)
        for blk in range(nblk):
            if blk + 1 < nblk:
                for g in range(2):
                    load_block(blk + 1, g)
            bi = blk % NBUF
            for s in range(SB):
                col = (s, SB - 1 - s)
                for g in range(2):
                    pA = k.psum()
                    for d in range(2):
                        k.mm(pA[0:2, d * 64:(d + 1) * 64], LAk[d][g][bi][:, col[d], :], S[g][:, d, :], True, True)
                    sa = SA[g][bi]
                    k.copy(sa[0:2, s, :], pA[0:2, 0:128], e='act')
                    pB = k.psum()
                    for d in range(2):
                        k.mm(pB[:, d * 64:(d + 1) * 64], KA[d][g][bi][0:2, col[d], :], sa[0:2, s, d * 64:(d + 1) * 64], True, False)
                        k.mm(pB[:, d * 64:(d + 1) * 64], KD[d][g][bi][0:2, col[d], :], VV[d][g][bi][0:2, col[d], :], False, True)
                    for d in range(2):
                        k.stt(S[g][:, d, :], S[g][:, d, :], Wt[d][g][bi][:, col[d]:col[d] + 1], pB[:, d * 64:(d + 1) * 64], ALU.mult, ALU.add)
                    pY = k.psum()
                    for d in range(2):
                        k.mm(pY[0:2, d * 64:(d + 1) * 64], LAr[d][g][bi][:, col[d], :], S[g][:, d, :], True, True)
                    for d in range(2):
                        k.copy(YY[d][g][bi][0:2, col[d], :], pY[0:2, d * 64:(d + 1) * 64], e='act')
            for g in range(2):
                store_block(blk, g)
        k.barrier()
    with ExitStack() as st:
        bcw = sb(st, nc, 'rlnw', [128, 256], F32)
        bcb = sb(st, nc, 'rlnb', [128, 256], F32)
        k.dma(bcw[:], io['rwkv_ln_w'][l, :].partition_broadcast(128))
        k.dma(bcb[:], io['rwkv_ln_b'][l, :].partition_broadcast(128))
        yt = [sb(st, nc, f'ryt{i}', [128, 2, 256], F32) for i in range(2)]
        bt = [sb(st, nc, f'rbt{i}', [128, 2, 256], F32) for i in range(2)]
        gtt = [sb(st, nc, f'rgtt{i}', [128, 256], F32) for i in range(2)]
        acc = [sb(st, nc, f'racc{i}', [128, 256], F32) for i in range(2)]
        jk = sb(st, nc, 'rjk', [128, 64], F32)
        sm = [sb(st, nc, f'rsm2{i}', [128, 32], F32) for i in range(2)]
        tiles = list(range(64)) + ([64, 65] if need_ctx else [])
        for n_, ti in enumerate(tiles):
            i2 = n_ % 2
            t0 = ti * 128
            y_, b_, g_, a_, s_ = yt[i2], bt[i2], gtt[i2], acc[i2], sm[i2]
            for d in range(2):
                k.dma(y_[:, d, :], io['ryscan_d'][d, t0:t0 + 128, :])
                k.dma(b_[:, d, :], io['rbonus_d'][d, t0:t0 + 128, :])
            k.dma(g_[:], io['rgate_d'][t0:t0 + 128, :])
            k.op('dve', lambda E: E.reduce_sum(s_[:, 0:8], y_[:].rearrange("p d (h e) -> p (d h) e", h=4), AX.X), [y_[:]], [s_[:]])
            k.ts(s_[:, 0:8], s_[:, 0:8], -1.0 / 64, None, ALU.mult)
            for j in range(8):
                d, h = j // 4, j % 4
                k.ts(y_[:, d, h * 64:(h + 1) * 64], y_[:, d, h * 64:(h + 1) * 64], s_[:, j:j + 1], None, ALU.add)
                k.act(jk[:], y_[:, d, h * 64:(h + 1) * 64], AF.Square, accum_out=s_[:, 8 + j:9 + j])
            k.act(s_[:, 16:24], s_[:, 8:16], AF.Sqrt, bias=64e-5, scale=1.0 / 64)
            k.op('dve', lambda E: E.reciprocal(s_[:, 24:32], s_[:, 16:24]), [s_[:]], [s_[:]])
            for j in range(8):
                d, h = j // 4, j % 4
                k.stt(y_[:, d, h * 64:(h + 1) * 64], y_[:, d, h * 64:(h + 1) * 64], s_[:, 24 + j:25 + j], bcw[:, h * 64:(h + 1) * 64],
                      ALU.mult, ALU.mult)
            k.tt(a_[:], y_[:, 0, :], y_[:, 1, :], ALU.add)
            k.tt(b_[:, 0, :], b_[:, 0, :], b_[:, 1, :], ALU.add, e='pool')
            k.stt(a_[:], bcb[:], 2.0, a_[:], ALU.mult, ALU.add)
            k.tt(a_[:], a_[:], b_[:, 0, :], ALU.add)
            k.tt(a_[:], a_[:], g_[:], ALU.mult, e='pool')
            k.dma(io['mix_d'][t0:t0 + 128, 512:768], a_[:], q='pool')
        k.barrier()


Prog.stage_RW = stage_RW


def build_full():
    P = Prog()
    P.declare()
    out = P.nc.dram_tensor('out', [T, D], F32, kind="ExternalOutput").ap()
    for l in range(DEPTH):
        P.stage_M(l)
        P.stage_A(l)
        P.stage_ATT(l)
        P.stage_ML(l)
        P.stage_RW(l)
        P.stage_C(l, out_ap=out)
    return P


def kernel(**inputs):
    inp = {k_: np.asarray(v) for k_, v in inputs.items()}
    P = build_full()
    consts = host_consts()
    in_maps = []
    for b in range(4):
        m = host_inputs(inp, b, consts, P.plans)
        in_maps.append({k_: np.ascontiguousarray(v) for k_, v in m.items() if k_ in P.io})
    res = run_bass_kernel_spmd(P.nc, in_maps, core_ids=list(range(4)))
    return np.stack([r['out'] for r in res.results], axis=0).astype(np.float32)


def stage_C(self, l, out_ap=None):
    k, nc, io = self.k, self.nc, self.io
    last = l == DEPTH - 1
    xsrc = io['x0'] if l == 0 else io['xs']
    groups = [(g * 1024, 1024, 0) for g in range(8)] + ([] if last else [(T, 256, 1)])
    import os as _os
    if 'C_NGROUPS' in _os.environ:
        groups = groups[:int(_os.environ['C_NGROUPS'])]
    with ExitStack() as st:
        identb = sb(st, nc, 'identb', [128, 128], BF16)
        identf = sb(st, nc, 'identf', [128, 128], F32)
        k.dma(identb[:], io['ident_b']); k.dma(identf[:], io['ident_f'])
        Wout = sb(st, nc, 'Wout', [128, 8, D], BF16)
        routw = sb(st, nc, 'routw', [128, 8, NE], F32)
        routb = sb(st, nc, 'routb', [128, NE], F32)
        b2 = sb(st, nc, 'b2', [NE, D], F32)
        b1c = [sb(st, nc, f'b1c{i}', [128, 16], F32) for i in range(2)]
        wstg = [sb(st, nc, f'cwstg{i}', [128, 1024], F32) for i in range(3)]
        k.dma(routw[:], io['router_w'][l].rearrange("(kc p) e -> p kc e", p=128))
        k.dma(routb[:], io['router_b'][l, :].partition_broadcast(128))
        k.dma(b2[:], io['moe_b2'][l])
        for kc in range(8):
            s_ = wstg[kc % 3]
            k.dma(s_[:], io['w_out'][l, kc * 128:(kc + 1) * 128, :])
            k.copy(Wout[:, kc, :], s_[:], e='pool')
        fng = None
        if last:
            fng = sb(st, nc, 'fng', [128, D], F32)
            k.dma(fng[:], io['final_norm_g'].partition_broadcast(128))
        bct = {i: sb(st, nc, f'cbc{i}', [128, D], F32) for i in (2, 3, 4, 5)}
        yacc = sb(st, nc, 'yacc', [128, 8, D], F32)
        h2T = sb(st, nc, 'h2T', [128, 8, 1024], BF16)
        gates = sb(st, nc, 'gates', [128, 8, NE], F32)
        W1b = sb(st, nc, 'W1b', [128, 8, 2048], BF16)
        W2b = sb(st, nc, 'W2b', [128, 8, D], BF16)
        actT = sb(st, nc, 'actT', [128, 8, 512], BF16)
        xt = [sb(st, nc, f'cxt{i}', [128, D], F32) for i in range(2)]
        mixf = sb(st, nc, 'cmixf', [128, D], F32)
        mixb = sb(st, nc, 'cmixb', [128, D], BF16)
        mixT = sb(st, nc, 'cmixT', [128, 8, 128], BF16)
        tmp = sb(st, nc, 'ctmp', [128, D], F32)
        junk = sb(st, nc, 'cjunk', [128, D], BF16)
        hb = sb(st, nc, 'chb', [128, D], BF16)
        h2fT = sb(st, nc, 'ch2fT', [128, 8, 128], F32)
        ssq = sb(st, nc, 'cssq', [128, 4], F32)
        rt = [sb(st, nc, f'crt{i}', [128, 96], F32) for i in range(2)]
        gT = sb(st, nc, 'cgT', [NE, 128], F32)
        ev = [sb(st, nc, f'cev{i}', [128, 512], F32) for i in range(6)]
        evc = [0]

        def EV():
            evc[0] += 1
            return ev[evc[0] % 6]

        cur_seg = [None]
        wc = [0]
        for (g0, ntg, seg) in groups:
            if cur_seg[0] != seg:
                for i in (2, 3, 4, 5):
                    k.dma(bct[i][:], io[f'modR{l}'][seg, i * D:(i + 1) * D].partition_broadcast(128))
                cur_seg[0] = seg
            G2, A2, S2, G5 = bct[2], bct[3], bct[4], bct[5]
            nti = ntg // 128
            for ti in range(nti):
                t0 = g0 + ti * 128
                x_ = xt[ti % 2]
                k.dma(x_[:], xsrc[t0:t0 + 128, :])
                k.dma(mixf[:], io['mix_d'][t0:t0 + 128, :])
                k.copy(mixb[:], mixf[:], e='pool')
                ps = k.psum()
                pb = ps[:].bitcast(BF16)
                for kc in range(8):
                    k.tr(pb[:, kc * 128:(kc + 1) * 128], mixb[:, kc * 128:(kc + 1) * 128], identb[:])
                k.copy(mixT[:], pb[:, 0:1024].rearrange("p (kc t) -> p kc t", kc=8), e='act')
                for half in range(2):
                    ps = k.psum()
                    for kc in range(8):
                        k.mm(ps[:, :], mixT[:, kc, :], Wout[:, kc, half * 512:(half + 1) * 512], kc == 0, kc == 7)
                    k.tt(tmp[:, half * 512:(half + 1) * 512], ps[:, :], G2[:, half * 512:(half + 1) * 512], ALU.mult)
                k.tt(x_[:], x_[:], tmp[:], ALU.add, e='pool')
                k.dma(io['xs'][t0:t0 + 128, :], x_[:], q='pool')
                k.act(junk[:], x_[:], AF.Square, accum_out=ssq[:, 0:1])
                k.act(ssq[:, 1:2], ssq[:, 0:1], AF.Sqrt, bias=1e-6, scale=1.0 / D)
                k.op('dve', lambda E: E.reciprocal(ssq[:, 2:3], ssq[:, 1:2]), [ssq[:]], [ssq[:]])
                k.stt(tmp[:], x_[:], ssq[:, 2:3], A2[:], ALU.mult, ALU.mult)
                k.tt(tmp[:], tmp[:], S2[:], ALU.add, e='pool')
                k.copy(hb[:], tmp[:], e='pool')
                ps = k.psum()
                pb = ps[:].bitcast(BF16)
                for kc in range(8):
                    k.tr(pb[:, kc * 128:(kc + 1) * 128], hb[:, kc * 128:(kc + 1) * 128], identb[:])
                k.copy(h2T[:, :, ti * 128:(ti + 1) * 128], pb[:, 0:1024].rearrange("p (kc t) -> p kc t", kc=8), e='act')
                for hh in range(2):
                    ps = k.psum()
                    for q_ in range(4):
                        kc = hh * 4 + q_
                        k.tr(ps[:, q_ * 128:(q_ + 1) * 128], tmp[:, kc * 128:(kc + 1) * 128], identf[:])
                    k.copy(h2fT[:, hh * 4:(hh + 1) * 4, :], ps[:, :].rearrange("p (kc t) -> p kc t", kc=4), e='act')
                ps = k.psum()
                for kc in range(8):
                    k.mm(ps[:, 0:NE], h2fT[:, kc, :], routw[:, kc, :], kc == 0, kc == 7)
                r_ = rt[ti % 2]
                k.tt(r_[:, 0:32], ps[:, 0:NE], routb[:], ALU.add)
                k.op('dve', lambda E: E.max(r_[:, 32:40], r_[:, 0:32]), [r_[:]], [r_[:]])
                k.ts(r_[:, 40:41], r_[:, 32:33], -1.0, None, ALU.mult)
                k.ts(r_[:, 64:96], r_[:, 0:32], r_[:, 35:36], None, ALU.is_ge)
                k.act(r_[:, 0:32], r_[:, 0:32], AF.Exp, bias=r_[:, 40:41])
                k.tt(r_[:, 0:32], r_[:, 0:32], r_[:, 64:96], ALU.mult)
                k.op('dve', lambda E: E.reduce_sum(r_[:, 41:42], r_[:, 0:32], AX.X), [r_[:]], [r_[:]])
                k.op('dve', lambda E: E.reciprocal(r_[:, 42:43], r_[:, 41:42]), [r_[:]], [r_[:]])
                k.ts(gates[:, ti, :], r_[:, 0:32], r_[:, 42:43], None, ALU.mult)
                ps = k.psum()
                k.tr(ps[0:NE, 0:128], gates[:, ti, :], identf[:])
                k.copy(gT[:], ps[0:NE, 0:128], e='act')
                for half in range(2):
                    ps = k.psum()
                    k.mm(ps[:, :], gT[:], b2[:, half * 512:(half + 1) * 512], True, True)
                    k.copy(yacc[:, ti, half * 512:(half + 1) * 512], ps[:, :], e='act')
            for e in range(NE):
                b1 = b1c[e % 2]
                k.dma(b1[:], io['moe_b1'][l, e, :].rearrange("(c p) -> p c", p=128), allow_slow_non_contiguous=True)
                for kc in range(8):
                    for half in range(2):
                        s_ = wstg[wc[0] % 3]
                        wc[0] += 1
                        k.dma(s_[:], io['moe_w1'][l, e, kc * 128:(kc + 1) * 128, half * 1024:(half + 1) * 1024])
                        k.copy(W1b[:, kc, half * 1024:(half + 1) * 1024], s_[:], e=('act' if wc[0] % 2 else 'pool'))
                for fc in range(8):
                    s_ = wstg[wc[0] % 3]
                    wc[0] += 1
                    k.dma(s_[:], io['moe_w2'][l, e, fc * 128:(fc + 1) * 128, :])
                    k.copy(W2b[:, fc, :], s_[:], e=('act' if wc[0] % 2 else 'pool'))
                hsz = min(512, ntg)
                for hg in range(ntg // hsz):
                    c0 = hg * hsz
                    for i in range(8):
                        pg = k.psum()
                        for kc in range(8):
                            k.mm(pg[:, 0:hsz], W1b[:, kc, i * 128:(i + 1) * 128], h2T[:, kc, c0:c0 + hsz], kc == 0, kc == 7)
                        pl = k.psum()
                        for kc in range(8):
                            k.mm(pl[:, 0:hsz], W1b[:, kc, 1024 + i * 128:1024 + (i + 1) * 128], h2T[:, kc, c0:c0 + hsz], kc == 0, kc == 7)
                        g_, sg, lt = EV(), EV(), EV()
                        k.ts(g_[:, 0:hsz], pg[:, 0:hsz], b1[:, i:i + 1], 7.0, ALU.add, ALU.min)
                        k.act(sg[:, 0:hsz], g_[:, 0:hsz], AF.Sigmoid, scale=1.702)
                        k.ts(lt[:, 0:hsz], pl[:, 0:hsz], b1[:, 8 + i:9 + i], 7.0, ALU.add, ALU.min)
                        k.ts(lt[:, 0:hsz], lt[:, 0:hsz], -7.0, 1.0, ALU.max, ALU.add, e='pool')
                        k.tt(g_[:, 0:hsz], g_[:, 0:hsz], sg[:, 0:hsz], ALU.mult, e='pool')
                        k.tt(actT[:, i, 0:hsz], g_[:, 0:hsz], lt[:, 0:hsz], ALU.mult, e='pool')
                    for tt_ in range(hsz // 128):
                        ti = (c0 // 128) + tt_
                        for half in range(2):
                            po = k.psum()
                            for fc in range(8):
                                k.mm(po[:, :], actT[:, fc, tt_ * 128:(tt_ + 1) * 128], W2b[:, fc, half * 512:(half + 1) * 512], fc == 0, fc == 7)
                            ys = yacc[:, ti, half * 512:(half + 1) * 512]
                            k.stt(ys, po[:, :], gates[:, ti, e:e + 1], ys, ALU.mult, ALU.add)
            for ti in range(nti):
                t0 = g0 + ti * 128
                x_ = xt[ti % 2]
                k.dma(x_[:], io['xs'][t0:t0 + 128, :])
                k.tt(tmp[:], yacc[:, ti, :], G5[:], ALU.mult)
                k.tt(x_[:], x_[:], tmp[:], ALU.add, e='pool')
                if not last:
                    k.dma(io['xs'][t0:t0 + 128, :], x_[:], q='pool')
                else:
                    k.act(junk[:], x_[:], AF.Square, accum_out=ssq[:, 0:1])
                    k.act(ssq[:, 1:2], ssq[:, 0:1], AF.Sqrt, bias=1e-6, scale=1.0 / D)
                    k.op('dve', lambda E: E.reciprocal(ssq[:, 2:3], ssq[:, 1:2]), [ssq[:]], [ssq[:]])
                    k.stt(tmp[:], x_[:], ssq[:, 2:3], fng[:], ALU.mult, ALU.mult)
                    k.dma(out_ap[t0:t0 + 128, :], tmp[:], q='pool')
        k.barrier()


Prog.stage_C = stage_C
```

```python
import numpy as np
import ml_dtypes
from contextlib import ExitStack
import concourse.bass as bass
import concourse.mybir as mybir
from concourse.bass_utils import run_bass_kernel_spmd

F32 = mybir.dt.float32
BF16 = mybir.dt.bfloat16
AF = mybir.ActivationFunctionType
ALU = mybir.AluOpType
AX = mybir.AxisListType

D = 1024
T = 8192
LC = 256
N = T + LC
NT = N // 128
DEPTH = 2
HD = 64
NE = 32
import os as _os0
SEM_ROT = int(_os0.environ.get('SEM_ROT', 20000))


class K:
    def __init__(self, nc, es):
        self.nc = nc
        self.es = es
        self.eng = {'pe': nc.tensor, 'act': nc.scalar, 'dve': nc.vector, 'pool': nc.gpsimd, 'sp': nc.sync}
        self.esems = {e: [es.enter_context(nc.semaphore(f"s_{e}_0"))] for e in ('pe', 'act', 'dve', 'pool')}
        self.ecnt = {e: 0 for e in self.esems}
        self.dsems = [es.enter_context(nc.semaphore(f"s_dma_{i}")) for i in range(44)]
        self.dcnt = [0] * len(self.dsems)
        self.drr = 0
        self.seen = {e: {} for e in self.eng}
        self.res = {}
        self.semobj = {}
        for e, l in self.esems.items():
            self.semobj[id(l[0])] = l[0]
        for s in self.dsems:
            self.semobj[id(s)] = s
        self.psb = [es.enter_context(nc.psum_tensor(f"psb{i}", [128, 512], F32)) for i in range(8)]
        self.psi = 0
        self.ninst = 0
        self.E1 = es.enter_context(nc.semaphore("s_bar1"))
        self.E2 = es.enter_context(nc.semaphore("s_bar2"))
        self.epoch = 0

    def psum(self):
        p = self.psb[self.psi % 8]
        self.psi += 1
        return p

    def _wait(self, e, sem, cnt):
        sid = id(sem)
        if self.seen[e].get(sid, 0) >= cnt:
            return
        self.eng[e].wait_ge(sem, cnt)
        self.seen[e][sid] = cnt
        self.ninst += 1

    def _deps(self, e, reads, writes, acc):
        for ap in reads:
            r = self.res.get(ap.name)
            if r and r['w']:
                self._wait(e, *r['w'])
        for ap in writes:
            r = self.res.get(ap.name)
            if not r:
                continue
            if r['w'] and not (acc and e == 'pe' and r.get('we') == 'pe'):
                self._wait(e, *r['w'])
            for sid, (sem, cnt) in r['r'].items():
                self._wait(e, sem, cnt)

    def _record(self, e, sem, cnt, reads, writes):
        for ap in reads:
            r = self.res.setdefault(ap.name, {'w': None, 'r': {}})
            r['r'][id(sem)] = (sem, cnt)
        for ap in writes:
            r = self.res.setdefault(ap.name, {'w': None, 'r': {}})
            r['w'] = (sem, cnt)
            r['we'] = e
            r['r'] = {}

    def op(self, e, fn, reads, writes, acc=False):
        self._deps(e, reads, writes, acc)
        ins = fn(self.eng[e])
        if self.ecnt[e] >= SEM_ROT:
            s = self.es.enter_context(self.nc.semaphore(f"s_{e}_{len(self.esems[e])}"))
            self.esems[e].append(s)
            self.semobj[id(s)] = s
            self.ecnt[e] = 0
        sem = self.esems[e][-1]
        self.ecnt[e] += 1
        ins.then_inc(sem, 1)
        self._record(e, sem, self.ecnt[e], reads, writes)
        self.ninst += 1
        return ins

    def dma(self, out, in_, q='sp', **kw):
        self._deps(q, [in_], [out], False)
        i = self.drr % len(self.dsems)
        self.drr += 1
        sem = self.dsems[i]
        if self.dcnt[i]:
            self._wait(q, sem, self.dcnt[i])
        ins = self.eng[q].dma_start(out=out, in_=in_, **kw)
        self.dcnt[i] += 16
        ins.then_inc(sem, 16)
        self._record(q, sem, self.dcnt[i], [in_], [out])
        self.ninst += 1

    def barrier(self):
        for e in self.eng:
            for e2, l in self.esems.items():
                if self.ecnt[e2]:
                    self._wait(e, l[-1], self.ecnt[e2])
            for i, s in enumerate(self.dsems):
                if self.dcnt[i]:
                    self._wait(e, s, self.dcnt[i])
        self.res = {}

    def mm(self, out, lhsT, rhs, start, stop):
        return self.op('pe', lambda E: E.matmul(out, lhsT, rhs, start=start, stop=stop), [lhsT, rhs], [out],
                       acc=not start)

    def tr(self, out, in_, ident):
        return self.op('pe', lambda E: E.transpose(out, in_, ident), [in_, ident], [out])

    def act(self, out, in_, func, bias=None, scale=None, accum_out=None, e='act'):
        kw = {}
        rd = [in_]
        wr = [out]
        if bias is not None:
            kw['bias'] = bias
            if not isinstance(bias, (int, float)):
                rd.append(bias)
        if scale is not None:
            kw['scale'] = scale
            if not isinstance(scale, (int, float)):
                rd.append(scale)
        if accum_out is not None:
            kw['accum_out'] = accum_out
            wr.append(accum_out)
        return self.op(e, lambda E: E.activation(out, in_, func, **kw), rd, wr)

    def tt(self, out, a, b, op, e='dve'):
        return self.op(e, lambda E: E.tensor_tensor(out, a, b, op), [a, b], [out])

    def ts(self, out, a, s1, s2, op0, op1=None, e='dve', accum_out=None):
        rd = [a] + [s for s in (s1, s2) if s is not None and not isinstance(s, (int, float))]
        wr = [out] + ([accum_out] if accum_out is not None else [])
        kw = {}
        if op1 is not None:
            kw['op1'] = op1
        if accum_out is not None:
            kw['accum_out'] = accum_out
        return self.op(e, lambda E: E.tensor_scalar(out, a, s1, s2, op0, **kw), rd, wr)

    def stt(self, out, a, s, b, op0, op1, e='dve'):
        rd = [a, b] + ([] if isinstance(s, (int, float)) else [s])
        return self.op(e, lambda E: E.scalar_tensor_tensor(out, a, s, b, op0, op1), rd, [out])

    def copy(self, out, in_, e='dve'):
        if e == 'act':
            return self.op(e, lambda E: E.copy(out, in_), [in_], [out])
        return self.op(e, lambda E: E.tensor_copy(out, in_), [in_], [out])

    def memset(self, out, v, e='pool'):
        return self.op(e, lambda E: E.memset(out, v), [], [out])


ATT_IN = 768
RW0 = 768
ML0 = 1728
NIN = 2768


def _rope_swap_idx():
    i = np.arange(64)
    blk = i // 16
    return np.where(blk % 2 == 0, i + 16, i - 16)


def build_colplan(layer):
    src = []
    cw = []
    groups = []

    def add(name, kind, cols, conv):
        base = len(src)
        nterm = 3 if conv else 1
        for j in range(nterm):
            for c in cols:
                src.append(c)
                if conv == 1:
                    cw.append((1, j, c - RW0))
                elif conv == 2:
                    cw.append((2, j, c - ML0))
                else:
                    cw.append((0, 0, 0))
        groups.append(dict(name=name, kind=kind, base=base, n=len(cols), nterm=nterm))

    sw = _rope_swap_idx()
    q = np.arange(512)
    qs = (q // 64) * 64 + sw[q % 64]
    kk = 512 + np.arange(128)
    ks = 512 + (np.arange(128) // 64) * 64 + sw[np.arange(128) % 64]
    add('attq', 'fm', list(q), 0)
    add('attqs', 'fm', list(qs), 0)
    add('attk', 'fm', list(kk), 0)
    add('attks', 'fm', list(ks), 0)
    add('rwx', 'fm', list(RW0 + 768 + np.arange(192)), 1)
    add('mlqk', 'fm', list(ML0 + np.arange(512)), 2)
    if layer > 0:
        add('vdown', 'fm', list(NIN + np.arange(32)), 0)
    add('attv', 'tm', list(640 + np.arange(128)), 0)
    add('rwrkv', 'tm', list(RW0 + np.arange(768)), 1)
    add('mlvog', 'tm', list(ML0 + 512 + np.arange(528)), 0)
    return np.array(src), cw, groups


_SBN = [0]


def sb(st, nc, name, shape, dt):
    _SBN[0] += 1
    return st.enter_context(nc.sbuf_tensor(f"{name}_u{_SBN[0]}", shape, dt))


def host_consts():
    c = {}
    c['ident_f'] = np.eye(128, dtype=np.float32)
    c['ident_b'] = np.eye(128).astype(ml_dtypes.bfloat16)
    t = np.arange(T)
    row = (t // 64).astype(np.float32)
    col = (t % 64).astype(np.float32)
    inv = (10000.0 ** (-np.arange(0, 32, 2, dtype=np.float32) / 32)).astype(np.float32)
    ang = np.concatenate([row[:, None] * inv, col[:, None] * inv], axis=-1).astype(np.float32)
    cos = np.cos(ang).astype(np.float32)
    sin = np.sin(ang).astype(np.float32)
    ct = np.ones((64, N), np.float32)
    stb = np.zeros((64, N), np.float32)
    for d in range(64):
        blk = d // 16
        f = (blk // 2) * 16 + d % 16
        ct[d, :T] = cos[:, f]
        stb[d, :T] = (-sin[:, f]) if blk % 2 == 0 else sin[:, f]
    c['rope_c'] = np.concatenate([ct, ct], 0)
    c['rope_s'] = np.concatenate([stb, stb], 0)
    c.update(att_consts())
    return c


class Prog:
    def __init__(self, nlayers=DEPTH, stop=None, dbg=()):
        self.nlayers = nlayers
        self.stop = stop
        self.dbg = dbg
        self.es = ExitStack()
        nc = self.nc = bass.Bass("TRN2", target_bir_lowering=False)
        self.k = K(nc, self.es)
        self.io = {}
        self.plans = [build_colplan(l) for l in range(DEPTH)]

    def din(self, name, shape, dt=F32):
        self.io[name] = self.nc.dram_tensor(name, list(shape), dt, kind="ExternalInput").ap()
        return self.io[name]

    def dscr(self, name, shape, dt=F32):
        kind = "ExternalOutput" if name in self.dbg else "Internal"
        self.io[name] = self.nc.dram_tensor(name, list(shape), dt, kind=kind).ap()
        return self.io[name]

    def declare(self):
        self.din('x0', [N, D])
        self.din('cc', [2, D])
        for nm in ('ident_f', 'rope_c', 'rope_s'):
            self.din(nm, {'ident_f': [128, 128], 'rope_c': [128, N], 'rope_s': [128, N]}[nm])
        self.din('ident_b', [128, 128], BF16)
        self.din('norm1_g', [DEPTH, D]); self.din('norm2_g', [DEPTH, D])
        self.din('ada_w', [DEPTH, D, 6 * D]); self.din('ada_b', [DEPTH, 6 * D])
        for l in range(DEPTH):
            ncw = len(self.plans[l][0])
            self.din(f'wx{l}', [D, ncw]); self.din(f'cs{l}', [1, ncw])
            self.dscr(f'modR{l}', [2, 6 * D])
        self.dscr('attq_d', [512, N]); self.dscr('attk_d', [128, N]); self.dscr('rwx_d', [192, N])
        self.dscr('mlqk_d', [512, N]); self.dscr('vdown_d', [32, N])
        self.dscr('attv_d', [N, 128]); self.dscr('rwrkv_d', [N, 768]); self.dscr('mlvog_d', [N, 528])
        self.dscr('xs', [N, D])
        self.dscr('mix_d', [N, D])
        self.din('mask_ge', [128, 512], BF16); self.din('mask_le', [128, 512], BF16)
        self.din('attn_sink', [DEPTH, 8])
        for nm, shp in (('rwkv_w0', [DEPTH, 2, 256]), ('rwkv_w_up', [DEPTH, 2, 32, 256]), ('rwkv_a0', [DEPTH, 2, 256]),
                        ('rwkv_a_up', [DEPTH, 2, 32, 256]), ('rwkv_g_up', [DEPTH, 64, 256]), ('rwkv_k_k', [DEPTH, 256]),
                        ('rwkv_k_a', [DEPTH, 256]), ('rwkv_r_k', [DEPTH, 4, 64]), ('rwkv_ln_w', [DEPTH, 256]),
                        ('rwkv_ln_b', [DEPTH, 256]), ('rwkv_v0', [1, 256]), ('rwkv_v_up', [1, 32, 256])):
            self.din(nm, shp)
        for nm, shp in (('vfirst_d', [N, 256]), ('rfm_d', [256, N]), ('rgate_d', [N, 256]),
                        ('rw_d', [2, 256, N]), ('rbonus_d', [2, N, 256]), ('ryscan_d', [2, N, 256])):
            self.dscr(nm, shp)
        for nm, shp in (('rv_d', [N, 256]), ('rnk_d', [N, 256]), ('rkka_d', [2, N, 256]), ('rkd_d', [2, N, 256])):
            self.dscr(nm, shp, BF16)
        for nm, shp in (('w_out', [DEPTH, D, D]), ('router_w', [DEPTH, D, NE]), ('router_b', [DEPTH, NE]), ('moe_w1', [DEPTH, NE, D, 2 * D]),
                        ('moe_b1', [DEPTH, NE, 2 * D]), ('moe_w2', [DEPTH, NE, D, D]), ('moe_b2', [DEPTH, NE, D]), ('final_norm_g', [D])):
            self.din(nm, shp)
        self.din('tri_le', [128, 128]); self.din('tri_ge', [128, 128])
        self.din('mlstm_gate_b', [DEPTH, 16]); self.din('mlstm_ln_w', [DEPTH, 256])

    def stage_M(self, l):
        k, nc, io = self.k, self.nc, self.io
        with ExitStack() as st:
            cT = sb(st, nc, 'cT', [128, 16], F32)
            adab = sb(st, nc, 'adab', [2, 6 * D], F32)
            gg = sb(st, nc, 'gg', [2, 2 * D], F32)
            mod = sb(st, nc, 'mod', [2, 6 * D], F32)
            R = sb(st, nc, 'R', [2, 6 * D], F32)
            wt = [sb(st, nc, f'adawt{i}', [128, 8, 512], F32) for i in range(2)]
            for r in range(2):
                k.dma(cT[:, r * 8:(r + 1) * 8], io['cc'][r, :].rearrange("(kc p) -> p kc", p=128),
                      allow_slow_non_contiguous=True)
            for r in range(2):
                k.dma(adab[r:r + 1, :], io['ada_b'][l:l + 1, :])
                k.dma(gg[r:r + 1, 0:D], io['norm1_g'][l:l + 1, :])
                k.dma(gg[r:r + 1, D:2 * D], io['norm2_g'][l:l + 1, :])
            k.act(cT[:], cT[:], AF.Silu)
            for ng in range(12):
                w = wt[ng % 2]
                k.dma(w[:], io['ada_w'][l, :, ng * 512:(ng + 1) * 512].rearrange("(kc p) n -> p kc n", p=128))
                ps = k.psum()
                for kc in range(8):
                    k.mm(ps[0:2, :], cT[:, kc:16:8], w[:, kc, :], kc == 0, kc == 7)
                k.tt(mod[:, ng * 512:(ng + 1) * 512], ps[0:2, :], adab[:, ng * 512:(ng + 1) * 512], ALU.add)
            m = lambda i: mod[:, i * D:(i + 1) * D]
            k.stt(R[:, 0:D], m(1), 1.0, gg[:, 0:D], ALU.add, ALU.mult)
            k.copy(R[:, D:2 * D], m(0))
            k.copy(R[:, 2 * D:3 * D], m(2))
            k.stt(R[:, 3 * D:4 * D], m(4), 1.0, gg[:, D:2 * D], ALU.add, ALU.mult)
            k.copy(R[:, 4 * D:5 * D], m(3))
            k.copy(R[:, 5 * D:6 * D], m(5))
            k.dma(io[f'modR{l}'], R[:], q='pool')
            k.barrier()

    def bcast_rows(self, st, l, idxs, tag):
        k, nc, io = self.k, self.nc, self.io
        out = {}
        for r in range(2):
            for i in idxs:
                t_ = sb(st, nc, f'bc_{tag}_{r}_{i}', [128, D], F32)
                k.dma(t_[:], io[f'modR{l}'][r, i * D:(i + 1) * D].partition_broadcast(128))
                out[(r, i)] = t_
        return out

    def norm_tile(self, xt, A, S, hb, tmp, junk, ssq, eps=1e-6):
        k = self.k
        k.act(junk[:], xt[:], AF.Square, accum_out=ssq[:, 0:1])
        k.act(ssq[:, 1:2], ssq[:, 0:1], AF.Sqrt, bias=eps, scale=1.0 / D)
        k.op('dve', lambda E: E.reciprocal(ssq[:, 2:3], ssq[:, 1:2]), [ssq[:]], [ssq[:]])
        k.stt(tmp[:], xt[:], ssq[:, 2:3], A[:], ALU.mult, ALU.mult)
        k.tt(hb[:], tmp[:], S[:], ALU.add, e='pool')

    def stage_A(self, l):
        k, nc, io = self.k, self.nc, self.io
        src, cw, groups = self.plans[l]
        ncw = len(src)
        xsrc = io['x0'] if l == 0 else io['xs']
        with ExitStack() as st:
            W = sb(st, nc, 'W_sb', [128, 8, ncw], BF16)
            identb = sb(st, nc, 'identb', [128, 128], BF16)
            k.dma(identb[:], io['ident_b'])
            with ExitStack() as st2:
                csb = sb(st2, nc, 'csb', [128, ncw], F32)
                stg = [sb(st2, nc, f'wstg{i}', [128, 512], F32) for i in range(3)]
                k.dma(csb[:], io[f'cs{l}'][0, :].partition_broadcast(128))
                i = 0
                for c0 in range(0, ncw, 512):
                    c1 = min(ncw, c0 + 512)
                    for kc in range(8):
                        s_ = stg[i % 3]
                        i += 1
                        k.dma(s_[:, 0:c1 - c0], io[f'wx{l}'][kc * 128:(kc + 1) * 128, c0:c1])
                        k.tt(W[:, kc, c0:c1], s_[:, 0:c1 - c0], csb[:, c0:c1], ALU.mult, e=('dve' if i % 2 else 'pool'))
                k.barrier()
            bc = self.bcast_rows(st, l, (0, 1), 'A')
            hT = [sb(st, nc, f'hT{i}', [128, 8, 514], BF16) for i in range(3)]
            xt = [sb(st, nc, f'xt{i}', [128, D], F32) for i in range(2)]
            tmp = sb(st, nc, 'ntmp', [128, D], F32)
            junk = sb(st, nc, 'njunk', [128, D], BF16)
            hb = [sb(st, nc, f'hb{i}', [128, D], BF16) for i in range(2)]
            ssq = [sb(st, nc, f'ssq{i}', [128, 4], F32) for i in range(2)]
            ropec = sb(st, nc, 'ropec', [128, 512], F32)
            ropes = sb(st, nc, 'ropes', [128, 512], F32)
            osb = [sb(st, nc, f'osb{i}', [128, 512], F32) for i in range(4)]
            rsw = sb(st, nc, 'rsw', [128, 512], F32)
            rt1 = sb(st, nc, 'rt1', [128, 512], F32)
            glist = [(g * 512, 512, 0) for g in range(16)] + [(T, 256, 1)]
            ocnt = [0]

            def nexto():
                ocnt[0] += 1
                return osb[ocnt[0] % 4]

            def make_hT(gi):
                t0, n, seg = glist[gi]
                buf = hT[gi % 3]
                for ti in range(n // 128):
                    x_ = xt[ti % 2]
                    k.dma(x_[:], xsrc[t0 + ti * 128:t0 + (ti + 1) * 128, :])
                    self.norm_tile(x_, bc[(seg, 0)], bc[(seg, 1)], hb[ti % 2], tmp, junk, ssq[ti % 2])
                    ps = k.psum()
                    pb = ps[:].bitcast(BF16)
                    for kc in range(8):
                        k.tr(pb[:, kc * 128:(kc + 1) * 128], hb[ti % 2][:, kc * 128:(kc + 1) * 128], identb[:])
                    k.copy(buf[:, :, 1 + ti * 128:1 + (ti + 1) * 128], pb[:, 0:1024].rearrange("p (kc t) -> p kc t", kc=8),
                           e='act')
                if gi > 0 and glist[gi - 1][2] == seg:
                    k.copy(buf[:, :, 0:1], hT[(gi - 1) % 3][:, :, 512:513], e='pool')
                    k.copy(hT[(gi - 1) % 3][:, :, 513:514], buf[:, :, 1:2], e='pool')
                else:
                    k.memset(buf[:, :, 0:1], 0.0)
                    if gi > 0:
                        k.memset(hT[(gi - 1) % 3][:, :, 513:514], 0.0)
                if gi == len(glist) - 1:
                    k.memset(buf[:, :, 1 + n:2 + n], 0.0)

            def proj(gi):
                t0, n, seg = glist[gi]
                buf = hT[gi % 3]
                k.dma(ropec[:, 0:n], io['rope_c'][:, t0:t0 + n])
                k.dma(ropes[:, 0:n], io['rope_s'][:, t0:t0 + n])
                G = {g['name']: g for g in groups}

                def fm_acc(g, c0, m):
                    ps = k.psum()
                    nt_ = g['nterm']
                    tot = nt_ * 8
                    ii = 0
                    for j in range(nt_):
                        sh = (j - 1) if nt_ == 3 else 0
                        for kc in range(8):
                            cb = g['base'] + j * g['n'] + c0
                            k.mm(ps[0:m, 0:n], W[:, kc, cb:cb + m], buf[:, kc, 1 + sh:1 + sh + n], ii == 0, ii == tot - 1)
                            ii += 1
                    return ps

                for (ga, gb, dst) in (('attq', 'attqs', 'attq_d'), ('attk', 'attks', 'attk_d')):
                    for c0 in range(0, G[ga]['n'], 128):
                        p1 = fm_acc(G[ga], c0, 128)
                        p2 = fm_acc(G[gb], c0, 128)
                        o = nexto()
                        k.tt(rt1[:, 0:n], p1[:, 0:n], ropec[:, 0:n], ALU.mult)
                        k.copy(rsw[:, 0:n], p2[:, 0:n], e='act')
                        k.tt(rsw[:, 0:n], rsw[:, 0:n], ropes[:, 0:n], ALU.mult, e='pool')
                        k.tt(o[:, 0:n], rt1[:, 0:n], rsw[:, 0:n], ALU.add)
                        k.dma(io[dst][c0:c0 + 128, t0:t0 + n], o[:, 0:n], q='pool')
                for (ga, dst, fn) in (('rwx', 'rwx_d', None), ('mlqk', 'mlqk_d', AF.Silu), ('vdown', 'vdown_d', None)):
                    if ga not in G:
                        continue
                    for c0 in range(0, G[ga]['n'], 128):
                        m = min(128, G[ga]['n'] - c0)
                        p1 = fm_acc(G[ga], c0, m)
                        o = nexto()
                        if fn is None:
                            k.copy(o[0:m, 0:n], p1[0:m, 0:n], e='act')
                        else:
                            k.act(o[0:m, 0:n], p1[0:m, 0:n], fn)
                        k.dma(io[dst][c0:c0 + m, t0:t0 + n], o[0:m, 0:n], q='pool')
                for ti in range(n // 128):
                    for (ga, dst) in (('attv', 'attv_d'), ('rwrkv', 'rwrkv_d'), ('mlvog', 'mlvog_d')):
                        g = G[ga]
                        for c0 in range(0, g['n'], 512):
                            w = min(512, g['n'] - c0)
                            ps = k.psum()
                            nt_ = g['nterm']
                            tot = nt_ * 8
                            ii = 0
                            for j in range(nt_):
                                sh = (j - 1) if nt_ == 3 else 0
                                for kc in range(8):
                                    cb = g['base'] + j * g['n'] + c0
                                    a = 1 + ti * 128 + sh
                                    k.mm(ps[:, 0:w], buf[:, kc, a:a + 128], W[:, kc, cb:cb + w], ii == 0, ii == tot - 1)
                                    ii += 1
                            o = nexto()
                            k.copy(o[:, 0:w], ps[:, 0:w], e=('act' if (ti + c0) % 2 else 'dve'))
                            k.dma(io[dst][t0 + ti * 128:t0 + (ti + 1) * 128, c0:c0 + w], o[:, 0:w], q='pool')

            make_hT(0)
            for gi in range(len(glist)):
                if gi + 1 < len(glist):
                    make_hT(gi + 1)
                proj(gi)
            k.barrier()


def host_inputs(inp, b, consts, plans):
    m = {}
    m['x0'] = np.concatenate([inp['x'][b], inp['ctx'][b]], axis=0)
    m['cc'] = np.stack([inp['c'][b], inp['c_ctx']], axis=0)
    m.update(consts)
    for nm in ('norm1_g', 'norm2_g', 'ada_w', 'ada_b', 'attn_sink', 'mlstm_gate_b', 'mlstm_ln_w', 'rwkv_w0', 'rwkv_w_up', 'rwkv_a0', 'rwkv_a_up', 'rwkv_g_up',
               'rwkv_k_k', 'rwkv_k_a', 'rwkv_r_k', 'rwkv_ln_w', 'rwkv_ln_b', 'rwkv_v0', 'rwkv_v_up',
               'w_out', 'router_w', 'router_b', 'moe_w1', 'moe_b1', 'moe_w2', 'moe_b2', 'final_norm_g'):
        m[nm] = inp[nm]
    ones = np.ones((1,), np.float32)
    for l in range(DEPTH):
        src, cw, groups = plans[l]
        wext = inp['w_in'][l] if l == 0 else np.concatenate([inp['w_in'][l], inp['rwkv_v_down'][l - 1]], axis=1)
        m[f'wx{l}'] = np.ascontiguousarray(wext[:, src])
        cs = np.empty((1, len(src)), np.float32)
        which = np.array([c[0] for c in cw]); jj = np.array([c[1] for c in cw]); cc_ = np.array([c[2] for c in cw])
        cs[0, which == 0] = ones[0]
        cs[0, which == 1] = inp['rwkv_conv'][l][jj[which == 1], cc_[which == 1]]
        cs[0, which == 2] = inp['mlstm_conv'][l][jj[which == 2], cc_[which == 2]]
        m[f'cs{l}'] = cs
    return m


def att_consts():
    j = np.arange(128)[:, None]
    i = np.arange(128)[None, :]
    ge = (j >= i).astype(np.float32)
    le = (j <= i).astype(np.float32)
    extra = {'tri_le': le, 'tri_ge': ge}
    return {**extra, 'mask_ge': np.tile(ge, (1, 4)).astype(ml_dtypes.bfloat16), 'mask_le': np.tile(le, (1, 4)).astype(ml_dtypes.bfloat16)}


def stage_ATT(self, l):
    k, nc, io = self.k, self.nc, self.io
    need_ctx = l < DEPTH - 1
    with ExitStack() as st:
        kTb = sb(st, nc, 'kTb', [128, N], BF16)
        vb = sb(st, nc, 'vb', [128, NT, 2, 65], BF16)
        mge = sb(st, nc, 'mge', [128, 512], BF16)
        mle = sb(st, nc, 'mle', [128, 512], BF16)
        esink = sb(st, nc, 'esink', [128, 8], F32)
        stg = [sb(st, nc, f'astg{i}', [128, 512], F32) for i in range(2)]
        qf = [sb(st, nc, f'qf{i}', [128, 512], F32) for i in range(2)]
        qb_ = [sb(st, nc, f'qb{i}', [128, 512], BF16) for i in range(2)]
        pt = [sb(st, nc, f'pt{i}', [128, 512], BF16) for i in range(6)]
        osb = [sb(st, nc, f'aosb{i}', [128, 512], F32) for i in range(2)]
        den = sb(st, nc, 'aden', [128, 8], F32)
        k.dma(mge[:], io['mask_ge'])
        k.dma(mle[:], io['mask_le'])
        k.dma(esink[:], io['attn_sink'][l, :].partition_broadcast(128))
        k.act(esink[:], esink[:], AF.Exp)
        k.memset(vb[:, :, :, 64:65], 1.0)
        for c in range(0, N, 512):
            w = min(512, N - c)
            s_ = stg[(c // 512) % 2]
            k.dma(s_[:, 0:w], io['attk_d'][:, c:c + w])
            k.copy(kTb[:, c:c + w], s_[:, 0:w], e='pool')
        for ti in range(NT):
            s_ = stg[ti % 2]
            k.dma(s_[:, 0:128], io['attv_d'][ti * 128:(ti + 1) * 128, :])
            k.copy(vb[:, ti, :, 0:64], s_[:, 0:128].rearrange("p (g d) -> p g d", g=2), e='dve')
        qblocks = list(range(64)) + ([64, 65] if need_ctx else [])
        pi = 0
        for n_, qb in enumerate(qblocks):
            if qb < 64:
                keys = [(kb, m) for kb, m in ((qb - 1, mge), (qb, None), (qb + 1, mle)) if 0 <= kb < 64] + [(64, None), (65, None)]
            else:
                keys = [(64, None), (65, None)]
            q_f, q_b, o_ = qf[n_ % 2], qb_[n_ % 2], osb[n_ % 2]
            for g in range(2):
                k.dma(q_f[g * 64:(g + 1) * 64, :].rearrange("p (h t) -> p h t", h=4),
                      io['attq_d'][g * 256:(g + 1) * 256, qb * 128:(qb + 1) * 128].rearrange("(h d) t -> d h t", d=64))
            k.copy(q_b[:], q_f[:], e='pool')
            for g in range(2):
                ptl = []
                for kb, m in keys:
                    ps = k.psum()
                    k.mm(ps[:, :], kTb[g * 64:(g + 1) * 64, kb * 128:(kb + 1) * 128], q_b[g * 64:(g + 1) * 64, :], True, True)
                    p_ = pt[pi % 6]
                    pi += 1
                    k.act(p_[:], ps[:, :], AF.Exp, scale=0.125)
                    if m is not None:
                        k.tt(p_[:], p_[:], m[:], ALU.mult, e='pool')
                    ptl.append((p_, kb))
                po = k.psum()
                for h in range(4):
                    for i_, (p_, kb) in enumerate(ptl):
                        k.mm(po[:, h * 65:(h + 1) * 65], p_[:, h * 128:(h + 1) * 128], vb[:, kb, g, :], i_ == 0, i_ == len(ptl) - 1)
                pv = po[:, 0:260].rearrange("p (h e) -> p h e", h=4)
                k.tt(den[:, g * 4:(g + 1) * 4], pv[:, :, 64], esink[:, g * 4:(g + 1) * 4], ALU.add)
                k.op('dve', lambda E: E.reciprocal(den[:, g * 4:(g + 1) * 4], den[:, g * 4:(g + 1) * 4]), [den[:]], [den[:]])
                for h in range(4):
                    hh = g * 4 + h
                    k.ts(o_[:, hh * 64:(hh + 1) * 64], po[:, h * 65:h * 65 + 64], den[:, hh:hh + 1], None, ALU.mult)
            k.dma(io['mix_d'][qb * 128:(qb + 1) * 128, 0:512], o_[:], q='pool')
        k.barrier()


Prog.stage_ATT = stage_ATT


def stage_ML(self, l):
    k, nc, io = self.k, self.nc, self.io
    need_ctx = l < DEPTH - 1
    with ExitStack() as st:
        ysum = sb(st, nc, 'ysum', [128, NT, 256], F32)
        tri = {0: sb(st, nc, 'tri_le', [128, 128], F32), 1: sb(st, nc, 'tri_ge', [128, 128], F32)}
        ones = sb(st, nc, 'ones_f', [128, 128], F32)
        identb = sb(st, nc, 'identb', [128, 128], BF16)
        gb = sb(st, nc, 'gb', [128, 16], F32)
        lnw = sb(st, nc, 'lnw', [128, 256], F32)
        C = sb(st, nc, 'Cst', [128, 2, 65], F32)
        Cb = sb(st, nc, 'Cstb', [128, 2, 65], BF16)
        qkf = [sb(st, nc, f'qkf{i}', [128, 4, 128], F32) for i in range(2)]
        qkb = [sb(st, nc, f'qkb{i}', [128, 4, 128], BF16) for i in range(2)]
        vf = [sb(st, nc, f'vf{i}', [128, 528], F32) for i in range(2)]
        va = [sb(st, nc, f'va{i}', [128, 4, 65], BF16) for i in range(2)]
        gt = [sb(st, nc, f'gt{i}', [128, 48], F32) for i in range(2)]
        kpp = [sb(st, nc, f'kpp{i}', [128, 128], BF16) for i in range(2)]
        pm = [sb(st, nc, f'pm{i}', [128, 128], BF16) for i in range(2)]
        sc = [sb(st, nc, f'sc{i}', [128, 8], F32) for i in range(2)]
        k.dma(tri[0][:], io['tri_le']); k.dma(tri[1][:], io['tri_ge']); k.dma(identb[:], io['ident_b'])
        k.memset(ones[:], 1.0)
        k.dma(gb[:], io['mlstm_gate_b'][l, :].partition_broadcast(128))
        k.dma(lnw[:], io['mlstm_ln_w'][l, :].partition_broadcast(128))
        it = 0
        for d in range(2):
            order = [64, 65] + list(range(64)) if d == 0 else [65, 64] + list(range(63, -1, -1))
            k.memset(C[:], 0.0)
            k.memset(Cb[:], 0.0)
            for ti in order:
                i2 = it % 2
                it += 1
                t0 = ti * 128
                qf_, qb_, vf_, va_, g_, s_ = qkf[i2], qkb[i2], vf[i2], va[i2], gt[i2], sc[i2]
                k.dma(qf_[:], io['mlqk_d'][:, t0:t0 + 128].rearrange("(s p) t -> p s t", p=128))
                k.dma(vf_[:], io['mlvog_d'][t0:t0 + 128, :])
                k.copy(qb_[:], qf_[:], e='pool')
                k.copy(va_[:, :, 0:64], vf_[:, 0:256].rearrange("p (h e) -> p h e", h=4), e='pool')
                k.memset(va_[:, :, 64:65], 1.0)
                k.tt(g_[:, 0:8], vf_[:, 512 + d * 8:512 + d * 8 + 8], gb[:, d * 8:d * 8 + 8], ALU.add)
                k.act(g_[:, 8:12], g_[:, 4:8], AF.Exp, scale=-1.0)
                k.act(g_[:, 8:12], g_[:, 8:12], AF.Ln, bias=1.0)
                k.ts(g_[:, 4:8], g_[:, 8:12], -1.0, None, ALU.mult)
                ps = k.psum()
                k.mm(ps[:, 0:4], tri[d][:], g_[:, 4:8], True, True)
                k.mm(ps[:, 4:8], ones[:], g_[:, 4:8], True, True)
                k.copy(g_[:, 12:20], ps[:, 0:8])
                k.tt(g_[:, 20:24], g_[:, 0:4], g_[:, 12:16], ALU.subtract)
                k.act(g_[:, 24:28], g_[:, 20:24], AF.Exp)
                k.ts(g_[:, 24:28], g_[:, 24:28], 0.125, None, ALU.mult)
                k.tt(g_[:, 28:32], g_[:, 20:24], g_[:, 16:20], ALU.add)
                k.act(g_[:, 28:32], g_[:, 28:32], AF.Exp)
                k.ts(g_[:, 28:32], g_[:, 28:32], 0.125, None, ALU.mult)
                k.act(g_[:, 32:36], g_[:, 12:16], AF.Exp)
                k.act(g_[:, 36:40], g_[:, 16:20], AF.Exp)
                for pr in range(2):
                    pT = k.psum()
                    pTb = pT[:].bitcast(BF16)
                    k.tr(pTb[:, 0:128], qb_[:, 2 + pr, :], identb[:])
                    kp = kpp[pr]
                    for hh in range(2):
                        h = pr * 2 + hh
                        k.ts(kp[:, hh * 64:(hh + 1) * 64], pTb[:, hh * 64:(hh + 1) * 64], g_[:, 28 + h:29 + h], None, ALU.mult,
                             e=('dve' if hh else 'pool') if False else 'dve')
                    for hh in range(2):
                        h = pr * 2 + hh
                        p0 = hh * 64
                        pS = k.psum()
                        k.mm(pS[:, 0:128], qb_[p0:p0 + 64, 2 + pr, :], qb_[p0:p0 + 64, pr, :], True, True)
                        pm_ = pm[hh]
                        k.stt(pm_[:], pS[:, 0:128], g_[:, 24 + h:25 + h], tri[d][:], ALU.mult, ALU.mult)
                        pO = k.psum()
                        k.mm(pO[:, 0:65], pm_[:], va_[:, h, :], True, False)
                        k.mm(pO[:, 0:65], qb_[p0:p0 + 64, pr, :], Cb[p0:p0 + 64, pr, :], False, True)
                        k.ts(s_[:, 0:1], pO[:, 64:65], g_[:, 32 + h:33 + h], None, ALU.mult)
                        k.act(s_[:, 0:1], s_[:, 0:1], AF.Abs)
                        k.ts(s_[:, 0:1], s_[:, 0:1], 1.0, None, ALU.max)
                        k.op('dve', lambda E: E.reciprocal(s_[:, 1:2], s_[:, 0:1]), [s_[:]], [s_[:]])
                        k.tt(s_[:, 2:3], s_[:, 1:2], g_[:, 32 + h:33 + h], ALU.mult)
                        yv = ysum[:, ti, h * 64:(h + 1) * 64]
                        if d == 0:
                            k.ts(yv, pO[:, 0:64], s_[:, 2:3], None, ALU.mult)
                        else:
                            k.stt(yv, pO[:, 0:64], s_[:, 2:3], yv, ALU.mult, ALU.add)
                    pC = k.psum()
                    k.mm(pC[:, 0:130], kp[:], va_[:, pr * 2:pr * 2 + 2, :], True, True)
                    for hh in range(2):
                        h = pr * 2 + hh
                        p0 = hh * 64
                        k.stt(C[p0:p0 + 64, pr, :], C[p0:p0 + 64, pr, :], g_[p0:p0 + 64, 36 + h:37 + h],
                              pC[p0:p0 + 64, hh * 65:(hh + 1) * 65], ALU.mult, ALU.add)
                    k.copy(Cb[:, pr, :], C[:, pr, :], e='pool')
        tiles = list(range(64)) + ([64, 65] if need_ctx else [])
        for n_, ti in enumerate(tiles):
            i2 = n_ % 2
            vf_, g_ = vf[i2], gt[i2]
            o_ = qkf[i2][:, 0:2, :]
            yc = qkf[i2][:, 2:4, :]
            k.dma(vf_[:, 0:256], io['mlvog_d'][ti * 128:(ti + 1) * 128, 256:512])
            k.act(vf_[:, 0:256], vf_[:, 0:256], AF.Sigmoid)
            k.op('dve', lambda E: E.reduce_sum(g_[:, 0:4], ysum[:, ti, :].rearrange("p (h e) -> p h e", h=4), AX.X),
                 [ysum[:]], [g_[:]])
            k.ts(g_[:, 0:4], g_[:, 0:4], -1.0 / 64, None, ALU.mult)
            for h in range(4):
                k.ts(yc[:, h // 2, (h % 2) * 64:(h % 2) * 64 + 64], ysum[:, ti, h * 64:(h + 1) * 64], g_[:, h:h + 1], None, ALU.add)
                k.act(o_[:, h // 2, (h % 2) * 64:(h % 2) * 64 + 64], yc[:, h // 2, (h % 2) * 64:(h % 2) * 64 + 64], AF.Square,
                      accum_out=g_[:, 4 + h:5 + h])
            k.act(g_[:, 8:12], g_[:, 4:8], AF.Sqrt, bias=1e-5, scale=1.0 / 64)
            k.op('dve', lambda E: E.reciprocal(g_[:, 12:16], g_[:, 8:12]), [g_[:]], [g_[:]])
            for h in range(4):
                k.stt(o_[:, h // 2, (h % 2) * 64:(h % 2) * 64 + 64], yc[:, h // 2, (h % 2) * 64:(h % 2) * 64 + 64], g_[:, 12 + h:13 + h],
                      lnw[:, h * 64:(h + 1) * 64], ALU.mult, ALU.mult)
            k.tt(vf_[:, 256:512], o_.rearrange("p a b -> p (a b)"), vf_[:, 0:256], ALU.mult, e='pool')
            k.dma(io['mix_d'][ti * 128:(ti + 1) * 128, 768:1024], vf_[:, 256:512], q='pool')
        k.barrier()


Prog.stage_ML = stage_ML


def stage_RW(self, l):
    k, nc, io = self.k, self.nc, self.io
    need_ctx = l < DEPTH - 1
    with ExitStack() as st:
        identf = sb(st, nc, 'identf', [128, 128], F32)
        k.dma(identf[:], io['ident_f'])
        bc = {}
        for nm, src in (('k_k', io['rwkv_k_k'][l, :]), ('k_a', io['rwkv_k_a'][l, :]), ('r_k', io['rwkv_r_k'][l].rearrange("h e -> (h e)")),
                        ('a0_0', io['rwkv_a0'][l, 0, :]), ('a0_1', io['rwkv_a0'][l, 1, :])) + \
                (() if l == 0 else (('v0', io['rwkv_v0'][l - 1, :]),)):
            bc[nm] = sb(st, nc, 'rbc_' + nm, [128, 256], F32)
            k.dma(bc[nm][:], src.partition_broadcast(128))
        up = sb(st, nc, 'rw_up', [128, 2, 256], F32)
        gup = sb(st, nc, 'rw_gup', [64, 256], F32)
        vup = sb(st, nc, 'rw_vup', [32, 256], F32)
        w0c = sb(st, nc, 'rw_w0c', [128, 4], F32)
        for d in range(2):
            k.dma(up[d * 32:(d + 1) * 32, 0, :], io['rwkv_w_up'][l, d])
            k.dma(up[d * 32:(d + 1) * 32, 1, :], io['rwkv_a_up'][l, d])
            k.dma(w0c[:, d * 2:d * 2 + 2], io['rwkv_w0'][l, d, :].rearrange("(c p) -> p c", p=128), allow_slow_non_contiguous=True)
        k.dma(gup[:], io['rwkv_g_up'][l])
        if l > 0:
            k.dma(vup[:], io['rwkv_v_up'][l - 1])
        NB = 2
        xr = [sb(st, nc, f'rxr{i}', [128, 768], F32) for i in range(NB)]
        xf = [sb(st, nc, f'rxf{i}', [64, 128], F32) for i in range(NB)]
        xaf = [sb(st, nc, f'rxaf{i}', [64, 128], F32) for i in range(NB)]
        gf = [sb(st, nc, f'rgf{i}', [64, 128], F32) for i in range(NB)]
        lvf = [sb(st, nc, f'rlv{i}', [32, 128], F32) for i in range(NB)]
        vfst = [sb(st, nc, f'rvf{i}', [128, 256], F32) for i in range(NB)]
        kk = [sb(st, nc, f'rkk{i}', [128, 256], F32) for i in range(NB)]
        t1 = [sb(st, nc, f'rt1{i}', [128, 256], F32) for i in range(4)]
        t2 = [sb(st, nc, f'rt2{i}', [128, 256], F32) for i in range(4)]
        sm = [sb(st, nc, f'rsm{i}', [128, 16], F32) for i in range(NB)]
        fo = [sb(st, nc, f'rfo{i}', [128, 128], F32) for i in range(4)]
        tb = [sb(st, nc, f'rtb{i}', [128, 256], BF16) for i in range(6)]
        cnt = [0]

        def T1():
            cnt[0] += 1
            return t1[cnt[0] % 4]

        def T2():
            cnt[0] += 1
            return t2[cnt[0] % 4]

        def FO():
            cnt[0] += 1
            return fo[cnt[0] % 4]

        def TB():
            cnt[0] += 1
            return tb[cnt[0] % 6]

        for ti in range(NT):
            i2 = ti % NB
            t0 = ti * 128
            x_, xf_, gf_, kk_, sm_ = xr[i2], xf[i2], gf[i2], kk[i2], sm[i2]
            k.dma(x_[:], io['rwrkv_d'][t0:t0 + 128, :])
            k.dma(xf_[:], io['rwx_d'][0:64, t0:t0 + 128])
            k.dma(xaf[i2][:], io['rwx_d'][64:128, t0:t0 + 128])
            k.dma(gf_[:], io['rwx_d'][128:192, t0:t0 + 128])
            r_, k_, v_ = x_[:, 0:256], x_[:, 256:512], x_[:, 512:768]
            if l == 0:
                k.dma(io['vfirst_d'][t0:t0 + 128, :], v_, q='pool')
            else:
                k.dma(lvf[i2][:], io['vdown_d'][:, t0:t0 + 128])
                k.dma(vfst[i2][:], io['vfirst_d'][t0:t0 + 128, :])
                ps = k.psum()
                k.mm(ps[:, 0:256], lvf[i2][:], vup[:], True, True)
                a = T1()
                k.tt(a[:], ps[:, 0:256], bc['v0'][:], ALU.add)
                k.act(a[:], a[:], AF.Sigmoid)
                b_ = T2()
                k.tt(b_[:], vfst[i2][:], v_, ALU.subtract)
                k.tt(b_[:], b_[:], a[:], ALU.mult, e='pool')
                k.tt(v_, v_, b_[:], ALU.add)
            vb_ = TB()
            k.copy(vb_[:], v_, e='pool')
            k.dma(io['rv_d'][t0:t0 + 128, :], vb_[:], q='pool')
            k.tt(kk_[:], k_, bc['k_k'][:], ALU.mult)
            a = T1()
            for h in range(4):
                k.act(a[:, h * 64:(h + 1) * 64], kk_[:, h * 64:(h + 1) * 64], AF.Square, accum_out=sm_[:, h:h + 1])
            k.act(sm_[:, 4:8], sm_[:, 0:4], AF.Sqrt)
            k.ts(sm_[:, 4:8], sm_[:, 4:8], 1e-12, None, ALU.max)
            k.op('dve', lambda E: E.reciprocal(sm_[:, 8:12], sm_[:, 4:8]), [sm_[:]], [sm_[:]])
            for h in range(4):
                k.ts(kk_[:, h * 64:(h + 1) * 64], kk_[:, h * 64:(h + 1) * 64], sm_[:, 8 + h:9 + h], None, ALU.mult)
            nk_ = TB()
            k.ts(nk_[:], kk_[:], -1.0, None, ALU.mult, e='pool')
            k.dma(io['rnk_d'][t0:t0 + 128, :], nk_[:], q='pool')
            for src_, dst in ((r_, 'rfm_d'),):
                for c in range(2):
                    ps = k.psum()
                    k.tr(ps[:, 0:128], src_[:, c * 128:(c + 1) * 128], identf[:])
                    f_ = FO()
                    k.copy(f_[:], ps[:, 0:128], e='act')
                    k.dma(io[dst][c * 128:(c + 1) * 128, t0:t0 + 128], f_[:], q='pool')
            k.act(gf_[:], gf_[:], AF.Sigmoid)
            ps = k.psum()
            k.mm(ps[:, 0:256], gf_[:], gup[:], True, True)
            a = T1()
            k.copy(a[:], ps[:, 0:256], e='act')
            k.dma(io['rgate_d'][t0:t0 + 128, :], a[:], q='pool')
            k.act(xf_[0:64, :], xf_[0:64, :], AF.Tanh)
            for d in range(2):
                for c in range(2):
                    ps = k.psum()
                    k.mm(ps[:, 0:128], up[d * 32:(d + 1) * 32, 0, c * 128:(c + 1) * 128], xf_[d * 32:(d + 1) * 32, :], True, True)
                    f_ = FO()
                    k.act(f_[:], ps[:, 0:128], AF.Sigmoid, bias=w0c[:, d * 2 + c:d * 2 + c + 1])
                    k.act(f_[:], f_[:], AF.Exp, scale=-0.6065306597126334)
                    k.dma(io['rw_d'][d, c * 128:(c + 1) * 128, t0:t0 + 128], f_[:], q='pool')
                ps = k.psum()
                k.mm(ps[:, 0:256], xaf[i2][d * 32:(d + 1) * 32, :], up[d * 32:(d + 1) * 32, 1, :], True, True)
                a = T1()
                k.tt(a[:], ps[:, 0:256], bc[f'a0_{d}'][:], ALU.add)
                k.act(a[:], a[:], AF.Sigmoid)
                b_ = TB()
                k.tt(b_[:], kk_[:], a[:], ALU.mult, e='pool')
                k.dma(io['rkka_d'][d, t0:t0 + 128, :], b_[:], q='pool')
                c_ = T2()
                k.stt(c_[:], a[:], -1.0, bc['k_a'][:], ALU.add, ALU.mult)
                k.stt(c_[:], c_[:], 1.0, k_, ALU.add, ALU.mult)
                cb_ = TB()
                k.copy(cb_[:], c_[:], e='pool')
                k.dma(io['rkd_d'][d, t0:t0 + 128, :], cb_[:], q='pool')
                e_ = T1()
                k.tt(e_[:], c_[:], bc['r_k'][:], ALU.mult, e='pool')
                k.tt(e_[:], e_[:], r_, ALU.mult)
                k.op('dve', lambda E: E.reduce_sum(sm_[:, 12:16], e_[:].rearrange("p (h e) -> p h e", h=4), AX.X), [e_[:]], [sm_[:]])
                f2 = T2()
                for h in range(4):
                    k.ts(f2[:, h * 64:(h + 1) * 64], v_[:, h * 64:(h + 1) * 64], sm_[:, 12 + h:13 + h], None, ALU.mult)
                k.dma(io['rbonus_d'][d, t0:t0 + 128, :], f2[:], q='pool')
        k.barrier()
    SB = 8
    with ExitStack() as st:
        sel = sb(st, nc, 'rsel', [128, 2], F32)
        k.memset(sel[:], 0.0)
        k.memset(sel[0:64, 0:1], 1.0)
        k.memset(sel[64:128, 1:2], 1.0)
        NBUF = 3
        mkf = lambda nm, shp, dt=F32: [[sb(st, nc, f'{nm}{g}_{i}', shp, dt) for i in range(NBUF)] for g in range(2)]
        Hist = mkf('rH', [128, SB, 2, 64])
        Sb = [[sb(st, nc, f'rSb{g}_{i}', [128, 2, 64], BF16) for i in range(2)] for g in range(2)]
        S0 = [sb(st, nc, f'rS0{g}', [128, 2, 64], F32) for g in range(2)]
        Ml = [[sb(st, nc, f'rMl{g}_{i}', [128, 2, 128], BF16) for i in range(3)] for g in range(2)]
        Rr = [mkf(f'rR{d}', [128, SB]) for d in range(2)]
        Wt = [mkf(f'Wt{d}', [128, SB]) for d in range(2)]
        NK = [mkf(f'NK{d}', [2, SB, 128], BF16) for d in range(2)]
        KA = [mkf(f'KA{d}', [2, SB, 128], BF16) for d in range(2)]
        KD = [mkf(f'KD{d}', [2, SB, 128], BF16) for d in range(2)]
        VV = [mkf(f'VV{d}', [2, SB, 64], BF16) for d in range(2)]
        YY = mkf('YY', [2, SB, 2, 64])
        ytmp = [sb(st, nc, f'rytmp{i}', [128, SB, 2, 64], F32) for i in range(2)]
        for g in range(2):
            k.memset(S0[g][:], 0.0)
            k.memset(Sb[g][0][:], 0.0)
            for d in range(2):
                for bi_ in range(NBUF):
                    for tl in (NK, KA, KD):
                        k.memset(tl[d][g][bi_][:], 0.0)
        import os as _os
        nblk = int(_os.environ.get('RW_NBLK', N // SB))

        def tokrange(blk, d):
            s0 = blk * SB
            if s0 < LC:
                return T + s0 if d == 0 else T + LC - s0 - SB
            return s0 - LC if d == 0 else T - (s0 - LC) - SB

        def load_block(blk, g):
            bi = blk % NBUF
            for d in range(2):
                tk = tokrange(blk, d)
                k.dma(Rr[d][g][bi][:], io['rfm_d'][g * 128:(g + 1) * 128, tk:tk + SB])
                k.dma(Wt[d][g][bi][:], io['rw_d'][d, g * 128:(g + 1) * 128, tk:tk + SB])
                for hh in range(2):
                    c0 = (g * 2 + hh) * 64
                    k.dma(NK[d][g][bi][hh:hh + 1, :, hh * 64:(hh + 1) * 64], io['rnk_d'][tk:tk + SB, c0:c0 + 64])
                    k.dma(KA[d][g][bi][hh:hh + 1, :, hh * 64:(hh + 1) * 64], io['rkka_d'][d, tk:tk + SB, c0:c0 + 64])
                    k.dma(KD[d][g][bi][hh:hh + 1, :, hh * 64:(hh + 1) * 64], io['rkd_d'][d, tk:tk + SB, c0:c0 + 64])
                    k.dma(VV[d][g][bi][hh:hh + 1, :, :], io['rv_d'][tk:tk + SB, c0:c0 + 64])

        nsteps = nblk * SB

        def cols(s):
            sl = s % SB
            return (sl, SB - 1 - sl)

        def build_M(s, g):
            bi = (s // SB) % NBUF
            col = cols(s)
            pM = k.psum()
            for d in range(2):
                k.mm(pM[:, d * 128:(d + 1) * 128], NK[d][g][bi][0:2, col[d], :], KA[d][g][bi][0:2, col[d], :], True, True)
            k.copy(Ml[g][s % 3][:].rearrange("p d c -> p (d c)"), pM[:, 0:256], e='act')

        def prev_state(s, g, d):
            if s == 0:
                return S0[g][:, d, :]
            ps_ = s - 1
            return Hist[g][(ps_ // SB) % NBUF][:, cols(ps_)[d], d, :]

        def block_out(blk, g):
            bi = blk % NBUF
            yt_ = ytmp[g]
            for d in range(2):
                k.tt(yt_[:, :, d, :], Hist[g][bi][:, :, d, :],
                     Rr[d][g][bi][:].unsqueeze(2).to_broadcast([128, SB, 64]), ALU.mult, e='pool')
            yv = yt_[:].rearrange("p s d v -> p (s d v)")
            yy = YY[g][bi]
            for hf in range(2):
                pY = k.psum()
                k.mm(pY[0:2, :], sel[:], yv[:, hf * 512:(hf + 1) * 512], True, True)
                k.copy(yy[:].rearrange("p s d v -> p (s d v)")[:, hf * 512:(hf + 1) * 512], pY[0:2, :], e='act')
            if not _os.environ.get('RW_NOSTORE'):
                for d in range(2):
                    tk = tokrange(blk, d)
                    for hh in range(2):
                        c0 = (g * 2 + hh) * 64
                        k.dma(io['ryscan_d'][d, tk:tk + SB, c0:c0 + 64], yy[hh:hh + 1, :, d, :], q='pool')

        for g in range(2):
            load_block(0, g)
            if nblk > 1:
                load_block(1, g)
        for g in range(2):
            build_M(0, g)
        for s in range(nsteps):
            blk, sl = s // SB, s % SB
            bi = blk % NBUF
            col = cols(s)
            if sl == 0 and blk + 2 < nblk and not _os.environ.get('RW_NOLOAD'):
                for g in range(2):
                    load_block(blk + 2, g)
            if s + 1 < nsteps:
                for g in range(2):
                    build_M(s + 1, g)
            pS = []
            for g in range(2):
                p_ = k.psum()
                so = Sb[g][s % 2]
                for d in range(2):
                    k.mm(p_[:, d * 64:(d + 1) * 64], Ml[g][s % 3][:, d, :], so[:, d, :], True, False)
                    k.mm(p_[:, d * 64:(d + 1) * 64], KD[d][g][bi][0:2, col[d], :], VV[d][g][bi][0:2, col[d], :], False, True)
                pS.append(p_)
            for g in range(2):
                sn = Sb[g][(s + 1) % 2]
                for d in range(2):
                    k.stt(sn[:, d, :], prev_state(s, g, d), Wt[d][g][bi][:, col[d]:col[d] + 1], pS[g][:, d * 64:(d + 1) * 64],
                          ALU.mult, ALU.add)
            for g in range(2):
                for d in range(2):
                    k.stt(Hist[g][bi][:, col[d], d, :], prev_state(s, g, d), Wt[d][g][bi][:, col[d]:col[d] + 1],
                          pS[g][:, d * 64:(d + 1) * 64], ALU.mult, ALU.add)
            if sl == SB - 1:
                for g in range(2):
                    block_out(blk, g)
        k.barrier()
    with ExitStack() as st:
        bcw = sb(st, nc, 'rlnw', [128, 256], F32)
        bcb = sb(st, nc, 'rlnb', [128, 256], F32)
        k.dma(bcw[:], io['rwkv_ln_w'][l, :].partition_broadcast(128))
        k.dma(bcb[:], io['rwkv_ln_b'][l, :].partition_broadcast(128))
        yt = [sb(st, nc, f'ryt{i}', [128, 2, 256], F32) for i in range(2)]
        bt = [sb(st, nc, f'rbt{i}', [128, 2, 256], F32) for i in range(2)]
        gtt = [sb(st, nc, f'rgtt{i}', [128, 256], F32) for i in range(2)]
        acc = [sb(st, nc, f'racc{i}', [128, 256], F32) for i in range(2)]
        jk = sb(st, nc, 'rjk', [128, 64], F32)
        sm = [sb(st, nc, f'rsm2{i}', [128, 32], F32) for i in range(2)]
        tiles = list(range(64)) + ([64, 65] if need_ctx else [])
        for n_, ti in enumerate(tiles):
            i2 = n_ % 2
            t0 = ti * 128
            y_, b_, g_, a_, s_ = yt[i2], bt[i2], gtt[i2], acc[i2], sm[i2]
            for d in range(2):
                k.dma(y_[:, d, :], io['ryscan_d'][d, t0:t0 + 128, :])
                k.dma(b_[:, d, :], io['rbonus_d'][d, t0:t0 + 128, :])
            k.dma(g_[:], io['rgate_d'][t0:t0 + 128, :])
            k.op('dve', lambda E: E.reduce_sum(s_[:, 0:8], y_[:].rearrange("p d (h e) -> p (d h) e", h=4), AX.X), [y_[:]], [s_[:]])
            k.ts(s_[:, 0:8], s_[:, 0:8], -1.0 / 64, None, ALU.mult)
            for j in range(8):
                d, h = j // 4, j % 4
                k.ts(y_[:, d, h * 64:(h + 1) * 64], y_[:, d, h * 64:(h + 1) * 64], s_[:, j:j + 1], None, ALU.add)
                k.act(jk[:], y_[:, d, h * 64:(h + 1) * 64], AF.Square, accum_out=s_[:, 8 + j:9 + j])
            k.act(s_[:, 16:24], s_[:, 8:16], AF.Sqrt, bias=64e-5, scale=1.0 / 64)
            k.op('dve', lambda E: E.reciprocal(s_[:, 24:32], s_[:, 16:24]), [s_[:]], [s_[:]])
            for j in range(8):
                d, h = j // 4, j % 4
                k.stt(y_[:, d, h * 64:(h + 1) * 64], y_[:, d, h * 64:(h + 1) * 64], s_[:, 24 + j:25 + j], bcw[:, h * 64:(h + 1) * 64],
                      ALU.mult, ALU.mult)
            k.tt(a_[:], y_[:, 0, :], y_[:, 1, :], ALU.add)
            k.tt(b_[:, 0, :], b_[:, 0, :], b_[:, 1, :], ALU.add, e='pool')
            k.stt(a_[:], bcb[:], 2.0, a_[:], ALU.mult, ALU.add)
            k.tt(a_[:], a_[:], b_[:, 0, :], ALU.add)
            k.tt(a_[:], a_[:], g_[:], ALU.mult, e='pool')
            k.dma(io['mix_d'][t0:t0 + 128, 512:768], a_[:], q='pool')
        k.barrier()


Prog.stage_RW = stage_RW


def build_full():
    P = Prog()
    P.declare()
    out = P.nc.dram_tensor('out', [T, D], F32, kind="ExternalOutput").ap()
    for l in range(DEPTH):
        P.stage_M(l)
        P.stage_A(l)
        P.stage_ATT(l)
        P.stage_ML(l)
        P.stage_RW(l)
        P.stage_C(l, out_ap=out)
    return P


def kernel(**inputs):
    inp = {k_: np.asarray(v) for k_, v in inputs.items()}
    P = build_full()
    consts = host_consts()
    in_maps = []
    for b in range(4):
        m = host_inputs(inp, b, consts, P.plans)
        in_maps.append({k_: np.ascontiguousarray(v) for k_, v in m.items() if k_ in P.io})
    res = run_bass_kernel_spmd(P.nc, in_maps, core_ids=list(range(4)))
    return np.stack([r['out'] for r in res.results], axis=0).astype(np.float32)


def stage_C(self, l, out_ap=None):
    k, nc, io = self.k, self.nc, self.io
    last = l == DEPTH - 1
    xsrc = io['x0'] if l == 0 else io['xs']
    groups = [(g * 1024, 1024, 0) for g in range(8)] + ([] if last else [(T, 256, 1)])
    import os as _os
    if 'C_NGROUPS' in _os.environ:
        groups = groups[:int(_os.environ['C_NGROUPS'])]
    with ExitStack() as st:
        identb = sb(st, nc, 'identb', [128, 128], BF16)
        identf = sb(st, nc, 'identf', [128, 128], F32)
        k.dma(identb[:], io['ident_b']); k.dma(identf[:], io['ident_f'])
        Wout = sb(st, nc, 'Wout', [128, 8, D], BF16)
        routw = sb(st, nc, 'routw', [128, 8, NE], F32)
        routb = sb(st, nc, 'routb', [128, NE], F32)
        b2 = sb(st, nc, 'b2', [NE, D], F32)
        b1c = [sb(st, nc, f'b1c{i}', [128, 16], F32) for i in range(2)]
        wstg = [sb(st, nc, f'cwstg{i}', [128, 1024], F32) for i in range(3)]
        k.dma(routw[:], io['router_w'][l].rearrange("(kc p) e -> p kc e", p=128))
        k.dma(routb[:], io['router_b'][l, :].partition_broadcast(128))
        k.dma(b2[:], io['moe_b2'][l])
        for kc in range(8):
            s_ = wstg[kc % 3]
            k.dma(s_[:], io['w_out'][l, kc * 128:(kc + 1) * 128, :])
            k.copy(Wout[:, kc, :], s_[:], e='pool')
        fng = None
        if last:
            fng = sb(st, nc, 'fng', [128, D], F32)
            k.dma(fng[:], io['final_norm_g'].partition_broadcast(128))
        bct = {i: sb(st, nc, f'cbc{i}', [128, D], F32) for i in (2, 3, 4, 5)}
        yacc = sb(st, nc, 'yacc', [128, 8, D], F32)
        h2T = sb(st, nc, 'h2T', [128, 8, 1024], BF16)
        gates = sb(st, nc, 'gates', [128, 8, NE], F32)
        W1b = sb(st, nc, 'W1b', [128, 8, 2048], BF16)
        W2b = sb(st, nc, 'W2b', [128, 8, D], BF16)
        actT = sb(st, nc, 'actT', [128, 8, 512], BF16)
        xt = [sb(st, nc, f'cxt{i}', [128, D], F32) for i in range(2)]
        mixf = sb(st, nc, 'cmixf', [128, D], F32)
        mixb = sb(st, nc, 'cmixb', [128, D], BF16)
        mixT = sb(st, nc, 'cmixT', [128, 8, 128], BF16)
        tmp = sb(st, nc, 'ctmp', [128, D], F32)
        junk = sb(st, nc, 'cjunk', [128, D], BF16)
        hb = sb(st, nc, 'chb', [128, D], BF16)
        h2fT = sb(st, nc, 'ch2fT', [128, 8, 128], F32)
        ssq = sb(st, nc, 'cssq', [128, 4], F32)
        rt = [sb(st, nc, f'crt{i}', [128, 96], F32) for i in range(2)]
        gT = sb(st, nc, 'cgT', [NE, 128], F32)
        ev = [sb(st, nc, f'cev{i}', [128, 512], F32) for i in range(6)]
        evc = [0]

        def EV():
            evc[0] += 1
            return ev[evc[0] % 6]

        cur_seg = [None]
        wc = [0]
        for (g0, ntg, seg) in groups:
            if cur_seg[0] != seg:
                for i in (2, 3, 4, 5):
                    k.dma(bct[i][:], io[f'modR{l}'][seg, i * D:(i + 1) * D].partition_broadcast(128))
                cur_seg[0] = seg
            G2, A2, S2, G5 = bct[2], bct[3], bct[4], bct[5]
            nti = ntg // 128
            for ti in range(nti):
                t0 = g0 + ti * 128
                x_ = xt[ti % 2]
                k.dma(x_[:], xsrc[t0:t0 + 128, :])
                k.dma(mixf[:], io['mix_d'][t0:t0 + 128, :])
                k.copy(mixb[:], mixf[:], e='pool')
                ps = k.psum()
                pb = ps[:].bitcast(BF16)
                for kc in range(8):
                    k.tr(pb[:, kc * 128:(kc + 1) * 128], mixb[:, kc * 128:(kc + 1) * 128], identb[:])
                k.copy(mixT[:], pb[:, 0:1024].rearrange("p (kc t) -> p kc t", kc=8), e='act')
                for half in range(2):
                    ps = k.psum()
                    for kc in range(8):
                        k.mm(ps[:, :], mixT[:, kc, :], Wout[:, kc, half * 512:(half + 1) * 512], kc == 0, kc == 7)
                    k.tt(tmp[:, half * 512:(half + 1) * 512], ps[:, :], G2[:, half * 512:(half + 1) * 512], ALU.mult)
                k.tt(x_[:], x_[:], tmp[:], ALU.add, e='pool')
                k.dma(io['xs'][t0:t0 + 128, :], x_[:], q='pool')
                k.act(junk[:], x_[:], AF.Square, accum_out=ssq[:, 0:1])
                k.act(ssq[:, 1:2], ssq[:, 0:1], AF.Sqrt, bias=1e-6, scale=1.0 / D)
                k.op('dve', lambda E: E.reciprocal(ssq[:, 2:3], ssq[:, 1:2]), [ssq[:]], [ssq[:]])
                k.stt(tmp[:], x_[:], ssq[:, 2:3], A2[:], ALU.mult, ALU.mult)
                k.tt(tmp[:], tmp[:], S2[:], ALU.add, e='pool')
                k.copy(hb[:], tmp[:], e='pool')
                ps = k.psum()
                pb = ps[:].bitcast(BF16)
                for kc in range(8):
                    k.tr(pb[:, kc * 128:(kc + 1) * 128], hb[:, kc * 128:(kc + 1) * 128], identb[:])
                k.copy(h2T[:, :, ti * 128:(ti + 1) * 128], pb[:, 0:1024].rearrange("p (kc t) -> p kc t", kc=8), e='act')
                for hh in range(2):
                    ps = k.psum()
                    for q_ in range(4):
                        kc = hh * 4 + q_
                        k.tr(ps[:, q_ * 128:(q_ + 1) * 128], tmp[:, kc * 128:(kc + 1) * 128], identf[:])
                    k.copy(h2fT[:, hh * 4:(hh + 1) * 4, :], ps[:, :].rearrange("p (kc t) -> p kc t", kc=4), e='act')
                ps = k.psum()
                for kc in range(8):
                    k.mm(ps[:, 0:NE], h2fT[:, kc, :], routw[:, kc, :], kc == 0, kc == 7)
                r_ = rt[ti % 2]
                k.tt(r_[:, 0:32], ps[:, 0:NE], routb[:], ALU.add)
                k.op('dve', lambda E: E.max(r_[:, 32:40], r_[:, 0:32]), [r_[:]], [r_[:]])
                k.ts(r_[:, 40:41], r_[:, 32:33], -1.0, None, ALU.mult)
                k.ts(r_[:, 64:96], r_[:, 0:32], r_[:, 35:36], None, ALU.is_ge)
                k.act(r_[:, 0:32], r_[:, 0:32], AF.Exp, bias=r_[:, 40:41])
                k.tt(r_[:, 0:32], r_[:, 0:32], r_[:, 64:96], ALU.mult)
                k.op('dve', lambda E: E.reduce_sum(r_[:, 41:42], r_[:, 0:32], AX.X), [r_[:]], [r_[:]])
                k.op('dve', lambda E: E.reciprocal(r_[:, 42:43], r_[:, 41:42]), [r_[:]], [r_[:]])
                k.ts(gates[:, ti, :], r_[:, 0:32], r_[:, 42:43], None, ALU.mult)
                ps = k.psum()
                k.tr(ps[0:NE, 0:128], gates[:, ti, :], identf[:])
                k.copy(gT[:], ps[0:NE, 0:128], e='act')
                for half in range(2):
                    ps = k.psum()
                    k.mm(ps[:, :], gT[:], b2[:, half * 512:(half + 1) * 512], True, True)
                    k.copy(yacc[:, ti, half * 512:(half + 1) * 512], ps[:, :], e='act')
            for e in range(NE):
                b1 = b1c[e % 2]
                k.dma(b1[:], io['moe_b1'][l, e, :].rearrange("(c p) -> p c", p=128), allow_slow_non_contiguous=True)
                for kc in range(8):
                    for half in range(2):
                        s_ = wstg[wc[0] % 3]
                        wc[0] += 1
                        k.dma(s_[:], io['moe_w1'][l, e, kc * 128:(kc + 1) * 128, half * 1024:(half + 1) * 1024])
                        k.copy(W1b[:, kc, half * 1024:(half + 1) * 1024], s_[:], e=('act' if wc[0] % 2 else 'pool'))
                for fc in range(8):
                    s_ = wstg[wc[0] % 3]
                    wc[0] += 1
                    k.dma(s_[:], io['moe_w2'][l, e, fc * 128:(fc + 1) * 128, :])
                    k.copy(W2b[:, fc, :], s_[:], e=('act' if wc[0] % 2 else 'pool'))
                hsz = min(512, ntg)
                for hg in range(ntg // hsz):
                    c0 = hg * hsz
                    for i in range(8):
                        pg = k.psum()
                        for kc in range(8):
                            k.mm(pg[:, 0:hsz], W1b[:, kc, i * 128:(i + 1) * 128], h2T[:, kc, c0:c0 + hsz], kc == 0, kc == 7)
                        pl = k.psum()
                        for kc in range(8):
                            k.mm(pl[:, 0:hsz], W1b[:, kc, 1024 + i * 128:1024 + (i + 1) * 128], h2T[:, kc, c0:c0 + hsz], kc == 0, kc == 7)
                        g_, sg, lt = EV(), EV(), EV()
                        k.ts(g_[:, 0:hsz], pg[:, 0:hsz], b1[:, i:i + 1], 7.0, ALU.add, ALU.min)
                        k.act(sg[:, 0:hsz], g_[:, 0:hsz], AF.Sigmoid, scale=1.702)
                        k.ts(lt[:, 0:hsz], pl[:, 0:hsz], b1[:, 8 + i:9 + i], 7.0, ALU.add, ALU.min)
                        k.ts(lt[:, 0:hsz], lt[:, 0:hsz], -7.0, 1.0, ALU.max, ALU.add, e='pool')
                        k.tt(g_[:, 0:hsz], g_[:, 0:hsz], sg[:, 0:hsz], ALU.mult, e='pool')
                        k.tt(actT[:, i, 0:hsz], g_[:, 0:hsz], lt[:, 0:hsz], ALU.mult, e='pool')
                    for tt_ in range(hsz // 128):
                        ti = (c0 // 128) + tt_
                        for half in range(2):
                            po = k.psum()
                            for fc in range(8):
                                k.mm(po[:, :], actT[:, fc, tt_ * 128:(tt_ + 1) * 128], W2b[:, fc, half * 512:(half + 1) * 512], fc == 0, fc == 7)
                            ys = yacc[:, ti, half * 512:(half + 1) * 512]
                            k.stt(ys, po[:, :], gates[:, ti, e:e + 1], ys, ALU.mult, ALU.add)
            for ti in range(nti):
                t0 = g0 + ti * 128
                x_ = xt[ti % 2]
                k.dma(x_[:], io['xs'][t0:t0 + 128, :])
                k.tt(tmp[:], yacc[:, ti, :], G5[:], ALU.mult)
                k.tt(x_[:], x_[:], tmp[:], ALU.add, e='pool')
                if not last:
                    k.dma(io['xs'][t0:t0 + 128, :], x_[:], q='pool')
                else:
                    k.act(junk[:], x_[:], AF.Square, accum_out=ssq[:, 0:1])
                    k.act(ssq[:, 1:2], ssq[:, 0:1], AF.Sqrt, bias=1e-6, scale=1.0 / D)
                    k.op('dve', lambda E: E.reciprocal(ssq[:, 2:3], ssq[:, 1:2]), [ssq[:]], [ssq[:]])
                    k.stt(tmp[:], x_[:], ssq[:, 2:3], fng[:], ALU.mult, ALU.mult)
                    k.dma(out_ap[t0:t0 + 128, :], tmp[:], q='pool')
        k.barrier()


Prog.stage_C = stage_C
```

```python
import numpy as np
import ml_dtypes
from contextlib import ExitStack
import concourse.bass as bass
import concourse.mybir as mybir
from concourse.bass_utils import run_bass_kernel_spmd

F32 = mybir.dt.float32
BF16 = mybir.dt.bfloat16
AF = mybir.ActivationFunctionType
ALU = mybir.AluOpType
AX = mybir.AxisListType

D = 1024
T = 8192
LC = 256
N = T + LC
NT = N // 128
DEPTH = 2
HD = 64
NE = 32
import os as _os0
SEM_ROT = int(_os0.environ.get('SEM_ROT', 20000))


class K:
    def __init__(self, nc, es):
        self.nc = nc
        self.es = es
        self.eng = {'pe': nc.tensor, 'act': nc.scalar, 'dve': nc.vector, 'pool': nc.gpsimd, 'sp': nc.sync}
        self.esems = {e: [es.enter_context(nc.semaphore(f"s_{e}_0"))] for e in ('pe', 'act', 'dve', 'pool')}
        self.ecnt = {e: 0 for e in self.esems}
        self.dsems = [es.enter_context(nc.semaphore(f"s_dma_{i}")) for i in range(44)]
        self.dcnt = [0] * len(self.dsems)
        self.drr = 0
        self.seen = {e: {} for e in self.eng}
        self.res = {}
        self.semobj = {}
        for e, l in self.esems.items():
            self.semobj[id(l[0])] = l[0]
        for s in self.dsems:
            self.semobj[id(s)] = s
        self.psb = [es.enter_context(nc.psum_tensor(f"psb{i}", [128, 512], F32)) for i in range(8)]
        self.psi = 0
        self.ninst = 0
        self.E1 = es.enter_context(nc.semaphore("s_bar1"))
        self.E2 = es.enter_context(nc.semaphore("s_bar2"))
        self.epoch = 0

    def psum(self):
        p = self.psb[self.psi % 8]
        self.psi += 1
        return p

    def _wait(self, e, sem, cnt):
        sid = id(sem)
        if self.seen[e].get(sid, 0) >= cnt:
            return
        self.eng[e].wait_ge(sem, cnt)
        self.seen[e][sid] = cnt
        self.ninst += 1

    def _deps(self, e, reads, writes, acc):
        for ap in reads:
            r = self.res.get(ap.name)
            if r and r['w']:
                self._wait(e, *r['w'])
        for ap in writes:
            r = self.res.get(ap.name)
            if not r:
                continue
            if r['w'] and not (acc and e == 'pe' and r.get('we') == 'pe'):
                self._wait(e, *r['w'])
            for sid, (sem, cnt) in r['r'].items():
                self._wait(e, sem, cnt)

    def _record(self, e, sem, cnt, reads, writes):
        for ap in reads:
            r = self.res.setdefault(ap.name, {'w': None, 'r': {}})
            r['r'][id(sem)] = (sem, cnt)
        for ap in writes:
            r = self.res.setdefault(ap.name, {'w': None, 'r': {}})
            r['w'] = (sem, cnt)
            r['we'] = e
            r['r'] = {}

    def op(self, e, fn, reads, writes, acc=False):
        self._deps(e, reads, writes, acc)
        ins = fn(self.eng[e])
        if self.ecnt[e] >= SEM_ROT:
            s = self.es.enter_context(self.nc.semaphore(f"s_{e}_{len(self.esems[e])}"))
            self.esems[e].append(s)
            self.semobj[id(s)] = s
            self.ecnt[e] = 0
        sem = self.esems[e][-1]
        self.ecnt[e] += 1
        ins.then_inc(sem, 1)
        self._record(e, sem, self.ecnt[e], reads, writes)
        self.ninst += 1
        return ins

    def dma(self, out, in_, q='sp', **kw):
        self._deps(q, [in_], [out], False)
        i = self.drr % len(self.dsems)
        self.drr += 1
        sem = self.dsems[i]
        if self.dcnt[i]:
            self._wait(q, sem, self.dcnt[i])
        ins = self.eng[q].dma_start(out=out, in_=in_, **kw)
        self.dcnt[i] += 16
        ins.then_inc(sem, 16)
        self._record(q, sem, self.dcnt[i], [in_], [out])
        self.ninst += 1

    def barrier(self):
        for e in self.eng:
            for e2, l in self.esems.items():
                if self.ecnt[e2]:
                    self._wait(e, l[-1], self.ecnt[e2])
            for i, s in enumerate(self.dsems):
                if self.dcnt[i]:
                    self._wait(e, s, self.dcnt[i])
        self.res = {}

    def mm(self, out, lhsT, rhs, start, stop):
        return self.op('pe', lambda E: E.matmul(out, lhsT, rhs, start=start, stop=stop), [lhsT, rhs], [out],
                       acc=not start)

    def tr(self, out, in_, ident):
        return self.op('pe', lambda E: E.transpose(out, in_, ident), [in_, ident], [out])

    def act(self, out, in_, func, bias=None, scale=None, accum_out=None, e='act'):
        kw = {}
        rd = [in_]
        wr = [out]
        if bias is not None:
            kw['bias'] = bias
            if not isinstance(bias, (int, float)):
                rd.append(bias)
        if scale is not None:
            kw['scale'] = scale
            if not isinstance(scale, (int, float)):
                rd.append(scale)
        if accum_out is not None:
            kw['accum_out'] = accum_out
            wr.append(accum_out)
        return self.op(e, lambda E: E.activation(out, in_, func, **kw), rd, wr)

    def tt(self, out, a, b, op, e='dve'):
        return self.op(e, lambda E: E.tensor_tensor(out, a, b, op), [a, b], [out])

    def ts(self, out, a, s1, s2, op0, op1=None, e='dve', accum_out=None):
        rd = [a] + [s for s in (s1, s2) if s is not None and not isinstance(s, (int, float))]
        wr = [out] + ([accum_out] if accum_out is not None else [])
        kw = {}
        if op1 is not None:
            kw['op1'] = op1
        if accum_out is not None:
            kw['accum_out'] = accum_out
        return self.op(e, lambda E: E.tensor_scalar(out, a, s1, s2, op0, **kw), rd, wr)

    def stt(self, out, a, s, b, op0, op1, e='dve'):
        rd = [a, b] + ([] if isinstance(s, (int, float)) else [s])
        return self.op(e, lambda E: E.scalar_tensor_tensor(out, a, s, b, op0, op1), rd, [out])

    def copy(self, out, in_, e='dve'):
        if e == 'act':
            return self.op(e, lambda E: E.copy(out, in_), [in_], [out])
        return self.op(e, lambda E: E.tensor_copy(out, in_), [in_], [out])

    def memset(self, out, v, e='pool'):
        return self.op(e, lambda E: E.memset(out, v), [], [out])


ATT_IN = 768
RW0 = 768
ML0 = 1728
NIN = 2768


def _rope_swap_idx():
    i = np.arange(64)
    blk = i // 16
    return np.where(blk % 2 == 0, i + 16, i - 16)


def build_colplan(layer):
    src = []
    cw = []
    groups = []

    def add(name, kind, cols, conv):
        base = len(src)
        nterm = 3 if conv else 1
        for j in range(nterm):
            for c in cols:
                src.append(c)
                if conv == 1:
                    cw.append((1, j, c - RW0))
                elif conv == 2:
                    cw.append((2, j, c - ML0))
                else:
                    cw.append((0, 0, 0))
        groups.append(dict(name=name, kind=kind, base=base, n=len(cols), nterm=nterm))

    sw = _rope_swap_idx()
    q = np.arange(512)
    qs = (q // 64) * 64 + sw[q % 64]
    kk = 512 + np.arange(128)
    ks = 512 + (np.arange(128) // 64) * 64 + sw[np.arange(128) % 64]
    add('attq', 'fm', list(q), 0)
    add('attqs', 'fm', list(qs), 0)
    add('attk', 'fm', list(kk), 0)
    add('attks', 'fm', list(ks), 0)
    add('rwx', 'fm', list(RW0 + 768 + np.arange(192)), 1)
    add('mlqk', 'fm', list(ML0 + np.arange(512)), 2)
    if layer > 0:
        add('vdown', 'fm', list(NIN + np.arange(32)), 0)
    add('attv', 'tm', list(640 + np.arange(128)), 0)
    add('rwrkv', 'tm', list(RW0 + np.arange(768)), 1)
    add('mlvog', 'tm', list(ML0 + 512 + np.arange(528)), 0)
    return np.array(src), cw, groups


_SBN = [0]


def sb(st, nc, name, shape, dt):
    _SBN[0] += 1
    return st.enter_context(nc.sbuf_tensor(f"{name}_u{_SBN[0]}", shape, dt))


def host_consts():
    c = {}
    c['ident_f'] = np.eye(128, dtype=np.float32)
    c['ident_b'] = np.eye(128).astype(ml_dtypes.bfloat16)
    t = np.arange(T)
    row = (t // 64).astype(np.float32)
    col = (t % 64).astype(np.float32)
    inv = (10000.0 ** (-np.arange(0, 32, 2, dtype=np.float32) / 32)).astype(np.float32)
    ang = np.concatenate([row[:, None] * inv, col[:, None] * inv], axis=-1).astype(np.float32)
    cos = np.cos(ang).astype(np.float32)
    sin = np.sin(ang).astype(np.float32)
    ct = np.ones((64, N), np.float32)
    stb = np.zeros((64, N), np.float32)
    for d in range(64):
        blk = d // 16
        f = (blk // 2) * 16 + d % 16
        ct[d, :T] = cos[:, f]
        stb[d, :T] = (-sin[:, f]) if blk % 2 == 0 else sin[:, f]
    c['rope_c'] = np.concatenate([ct, ct], 0)
    c['rope_s'] = np.concatenate([stb, stb], 0)
    c.update(att_consts())
    return c


class Prog:
    def __init__(self, nlayers=DEPTH, stop=None, dbg=()):
        self.nlayers = nlayers
        self.stop = stop
        self.dbg = dbg
        self.es = ExitStack()
        nc = self.nc = bass.Bass("TRN2", target_bir_lowering=False)
        self.k = K(nc, self.es)
        self.io = {}
        self.plans = [build_colplan(l) for l in range(DEPTH)]

    def din(self, name, shape, dt=F32):
        self.io[name] = self.nc.dram_tensor(name, list(shape), dt, kind="ExternalInput").ap()
        return self.io[name]

    def dscr(self, name, shape, dt=F32):
        kind = "ExternalOutput" if name in self.dbg else "Internal"
        self.io[name] = self.nc.dram_tensor(name, list(shape), dt, kind=kind).ap()
        return self.io[name]

    def declare(self):
        self.din('x0', [N, D])
        self.din('cc', [2, D])
        for nm in ('ident_f', 'rope_c', 'rope_s'):
            self.din(nm, {'ident_f': [128, 128], 'rope_c': [128, N], 'rope_s': [128, N]}[nm])
        self.din('ident_b', [128, 128], BF16)
        self.din('norm1_g', [DEPTH, D]); self.din('norm2_g', [DEPTH, D])
        self.din('ada_w', [DEPTH, D, 6 * D]); self.din('ada_b', [DEPTH, 6 * D])
        for l in range(DEPTH):
            ncw = len(self.plans[l][0])
            self.din(f'wx{l}', [D, ncw]); self.din(f'cs{l}', [1, ncw])
            self.dscr(f'modR{l}', [2, 6 * D])
        self.dscr('attq_d', [512, N]); self.dscr('attk_d', [128, N]); self.dscr('rwx_d', [192, N])
        self.dscr('mlqk_d', [512, N]); self.dscr('vdown_d', [32, N])
        self.dscr('attv_d', [N, 128]); self.dscr('rwrkv_d', [N, 768]); self.dscr('mlvog_d', [N, 528])
        self.dscr('xs', [N, D])
        self.dscr('mix_d', [N, D])
        self.din('mask_ge', [128, 512], BF16); self.din('mask_le', [128, 512], BF16)
        self.din('attn_sink', [DEPTH, 8])
        for nm, shp in (('rwkv_w0', [DEPTH, 2, 256]), ('rwkv_w_up', [DEPTH, 2, 32, 256]), ('rwkv_a0', [DEPTH, 2, 256]),
                        ('rwkv_a_up', [DEPTH, 2, 32, 256]), ('rwkv_g_up', [DEPTH, 64, 256]), ('rwkv_k_k', [DEPTH, 256]),
                        ('rwkv_k_a', [DEPTH, 256]), ('rwkv_r_k', [DEPTH, 4, 64]), ('rwkv_ln_w', [DEPTH, 256]),
                        ('rwkv_ln_b', [DEPTH, 256]), ('rwkv_v0', [1, 256]), ('rwkv_v_up', [1, 32, 256])):
            self.din(nm, shp)
        for nm, shp in (('vfirst_d', [N, 256]), ('rfm_d', [256, N]), ('rgate_d', [N, 256]),
                        ('rw_d', [2, 256, N]), ('rbonus_d', [2, N, 256]), ('ryscan_d', [2, N, 256])):
            self.dscr(nm, shp)
        for nm, shp in (('rv_d', [N, 256]), ('rnk_d', [N, 256]), ('rkka_d', [2, N, 256]), ('rkd_d', [2, N, 256])):
            self.dscr(nm, shp, BF16)
        for nm, shp in (('w_out', [DEPTH, D, D]), ('router_w', [DEPTH, D, NE]), ('router_b', [DEPTH, NE]), ('moe_w1', [DEPTH, NE, D, 2 * D]),
                        ('moe_b1', [DEPTH, NE, 2 * D]), ('moe_w2', [DEPTH, NE, D, D]), ('moe_b2', [DEPTH, NE, D]), ('final_norm_g', [D])):
            self.din(nm, shp)
        self.din('tri_le', [128, 128]); self.din('tri_ge', [128, 128])
        self.din('mlstm_gate_b', [DEPTH, 16]); self.din('mlstm_ln_w', [DEPTH, 256])

    def stage_M(self, l):
        k, nc, io = self.k, self.nc, self.io
        with ExitStack() as st:
            cT = sb(st, nc, 'cT', [128, 16], F32)
            adab = sb(st, nc, 'adab', [2, 6 * D], F32)
            gg = sb(st, nc, 'gg', [2, 2 * D], F32)
            mod = sb(st, nc, 'mod', [2, 6 * D], F32)
            R = sb(st, nc, 'R', [2, 6 * D], F32)
            wt = [sb(st, nc, f'adawt{i}', [128, 8, 512], F32) for i in range(2)]
            for r in range(2):
                k.dma(cT[:, r * 8:(r + 1) * 8], io['cc'][r, :].rearrange("(kc p) -> p kc", p=128),
                      allow_slow_non_contiguous=True)
            for r in range(2):
                k.dma(adab[r:r + 1, :], io['ada_b'][l:l + 1, :])
                k.dma(gg[r:r + 1, 0:D], io['norm1_g'][l:l + 1, :])
                k.dma(gg[r:r + 1, D:2 * D], io['norm2_g'][l:l + 1, :])
            k.act(cT[:], cT[:], AF.Silu)
            for ng in range(12):
                w = wt[ng % 2]
                k.dma(w[:], io['ada_w'][l, :, ng * 512:(ng + 1) * 512].rearrange("(kc p) n -> p kc n", p=128))
                ps = k.psum()
                for kc in range(8):
                    k.mm(ps[0:2, :], cT[:, kc:16:8], w[:, kc, :], kc == 0, kc == 7)
                k.tt(mod[:, ng * 512:(ng + 1) * 512], ps[0:2, :], adab[:, ng * 512:(ng + 1) * 512], ALU.add)
            m = lambda i: mod[:, i * D:(i + 1) * D]
            k.stt(R[:, 0:D], m(1), 1.0, gg[:, 0:D], ALU.add, ALU.mult)
            k.copy(R[:, D:2 * D], m(0))
            k.copy(R[:, 2 * D:3 * D], m(2))
            k.stt(R[:, 3 * D:4 * D], m(4), 1.0, gg[:, D:2 * D], ALU.add, ALU.mult)
            k.copy(R[:, 4 * D:5 * D], m(3))
            k.copy(R[:, 5 * D:6 * D], m(5))
            k.dma(io[f'modR{l}'], R[:], q='pool')
            k.barrier()

    def bcast_rows(self, st, l, idxs, tag):
        k, nc, io = self.k, self.nc, self.io
        out = {}
        for r in range(2):
            for i in idxs:
                t_ = sb(st, nc, f'bc_{tag}_{r}_{i}', [128, D], F32)
                k.dma(t_[:], io[f'modR{l}'][r, i * D:(i + 1) * D].partition_broadcast(128))
                out[(r, i)] = t_
        return out

    def norm_tile(self, xt, A, S, hb, tmp, junk, ssq, eps=1e-6):
        k = self.k
        k.act(junk[:], xt[:], AF.Square, accum_out=ssq[:, 0:1])
        k.act(ssq[:, 1:2], ssq[:, 0:1], AF.Sqrt, bias=eps, scale=1.0 / D)
        k.op('dve', lambda E: E.reciprocal(ssq[:, 2:3], ssq[:, 1:2]), [ssq[:]], [ssq[:]])
        k.stt(tmp[:], xt[:], ssq[:, 2:3], A[:], ALU.mult, ALU.mult)
        k.tt(hb[:], tmp[:], S[:], ALU.add, e='pool')

    def stage_A(self, l):
        k, nc, io = self.k, self.nc, self.io
        src, cw, groups = self.plans[l]
        ncw = len(src)
        xsrc = io['x0'] if l == 0 else io['xs']
        with ExitStack() as st:
            W = sb(st, nc, 'W_sb', [128, 8, ncw], BF16)
            identb = sb(st, nc, 'identb', [128, 128], BF16)
            k.dma(identb[:], io['ident_b'])
            with ExitStack() as st2:
                csb = sb(st2, nc, 'csb', [128, ncw], F32)
                stg = [sb(st2, nc, f'wstg{i}', [128, 512], F32) for i in range(3)]
                k.dma(csb[:], io[f'cs{l}'][0, :].partition_broadcast(128))
                i = 0
                for c0 in range(0, ncw, 512):
                    c1 = min(ncw, c0 + 512)
                    for kc in range(8):
                        s_ = stg[i % 3]
                        i += 1
                        k.dma(s_[:, 0:c1 - c0], io[f'wx{l}'][kc * 128:(kc + 1) * 128, c0:c1])
                        k.tt(W[:, kc, c0:c1], s_[:, 0:c1 - c0], csb[:, c0:c1], ALU.mult, e=('dve' if i % 2 else 'pool'))
                k.barrier()
            bc = self.bcast_rows(st, l, (0, 1), 'A')
            hT = [sb(st, nc, f'hT{i}', [128, 8, 514], BF16) for i in range(3)]
            xt = [sb(st, nc, f'xt{i}', [128, D], F32) for i in range(2)]
            tmp = sb(st, nc, 'ntmp', [128, D], F32)
            junk = sb(st, nc, 'njunk', [128, D], BF16)
            hb = [sb(st, nc, f'hb{i}', [128, D], BF16) for i in range(2)]
            ssq = [sb(st, nc, f'ssq{i}', [128, 4], F32) for i in range(2)]
            ropec = sb(st, nc, 'ropec', [128, 512], F32)
            ropes = sb(st, nc, 'ropes', [128, 512], F32)
            osb = [sb(st, nc, f'osb{i}', [128, 512], F32) for i in range(4)]
            rsw = sb(st, nc, 'rsw', [128, 512], F32)
            rt1 = sb(st, nc, 'rt1', [128, 512], F32)
            glist = [(g * 512, 512, 0) for g in range(16)] + [(T, 256, 1)]
            ocnt = [0]

            def nexto():
                ocnt[0] += 1
                return osb[ocnt[0] % 4]

            def make_hT(gi):
                t0, n, seg = glist[gi]
                buf = hT[gi % 3]
                for ti in range(n // 128):
                    x_ = xt[ti % 2]
                    k.dma(x_[:], xsrc[t0 + ti * 128:t0 + (ti + 1) * 128, :])
                    self.norm_tile(x_, bc[(seg, 0)], bc[(seg, 1)], hb[ti % 2], tmp, junk, ssq[ti % 2])
                    ps = k.psum()
                    pb = ps[:].bitcast(BF16)
                    for kc in range(8):
                        k.tr(pb[:, kc * 128:(kc + 1) * 128], hb[ti % 2][:, kc * 128:(kc + 1) * 128], identb[:])
                    k.copy(buf[:, :, 1 + ti * 128:1 + (ti + 1) * 128], pb[:, 0:1024].rearrange("p (kc t) -> p kc t", kc=8),
                           e='act')
                if gi > 0 and glist[gi - 1][2] == seg:
                    k.copy(buf[:, :, 0:1], hT[(gi - 1) % 3][:, :, 512:513], e='pool')
                    k.copy(hT[(gi - 1) % 3][:, :, 513:514], buf[:, :, 1:2], e='pool')
                else:
                    k.memset(buf[:, :, 0:1], 0.0)
                    if gi > 0:
                        k.memset(hT[(gi - 1) % 3][:, :, 513:514], 0.0)
                if gi == len(glist) - 1:
                    k.memset(buf[:, :, 1 + n:2 + n], 0.0)

            def proj(gi):
                t0, n, seg = glist[gi]
                buf = hT[gi % 3]
                k.dma(ropec[:, 0:n], io['rope_c'][:, t0:t0 + n])
                k.dma(ropes[:, 0:n], io['rope_s'][:, t0:t0 + n])
                G = {g['name']: g for g in groups}

                def fm_acc(g, c0, m):
                    ps = k.psum()
                    nt_ = g['nterm']
                    tot = nt_ * 8
                    ii = 0
                    for j in range(nt_):
                        sh = (j - 1) if nt_ == 3 else 0
                        for kc in range(8):
                            cb = g['base'] + j * g['n'] + c0
                            k.mm(ps[0:m, 0:n], W[:, kc, cb:cb + m], buf[:, kc, 1 + sh:1 + sh + n], ii == 0, ii == tot - 1)
                            ii += 1
                    return ps

                for (ga, gb, dst) in (('attq', 'attqs', 'attq_d'), ('attk', 'attks', 'attk_d')):
                    for c0 in range(0, G[ga]['n'], 128):
                        p1 = fm_acc(G[ga], c0, 128)
                        p2 = fm_acc(G[gb], c0, 128)
                        o = nexto()
                        k.tt(rt1[:, 0:n], p1[:, 0:n], ropec[:, 0:n], ALU.mult)
                        k.copy(rsw[:, 0:n], p2[:, 0:n], e='act')
                        k.tt(rsw[:, 0:n], rsw[:, 0:n], ropes[:, 0:n], ALU.mult, e='pool')
                        k.tt(o[:, 0:n], rt1[:, 0:n], rsw[:, 0:n], ALU.add)
                        k.dma(io[dst][c0:c0 + 128, t0:t0 + n], o[:, 0:n], q='pool')
                for (ga, dst, fn) in (('rwx', 'rwx_d', None), ('mlqk', 'mlqk_d', AF.Silu), ('vdown', 'vdown_d', None)):
                    if ga not in G:
                        continue
                    for c0 in range(0, G[ga]['n'], 128):
                        m = min(128, G[ga]['n'] - c0)
                        p1 = fm_acc(G[ga], c0, m)
                        o = nexto()
                        if fn is None:
                            k.copy(o[0:m, 0:n], p1[0:m, 0:n], e='act')
                        else:
                            k.act(o[0:m, 0:n], p1[0:m, 0:n], fn)
                        k.dma(io[dst][c0:c0 + m, t0:t0 + n], o[0:m, 0:n], q='pool')
                for ti in range(n // 128):
                    for (ga, dst) in (('attv', 'attv_d'), ('rwrkv', 'rwrkv_d'), ('mlvog', 'mlvog_d')):
                        g = G[ga]
                        for c0 in range(0, g['n'], 512):
                            w = min(512, g['n'] - c0)
                            ps = k.psum()
                            nt_ = g['nterm']
                            tot = nt_ * 8
                            ii = 0
                            for j in range(nt_):
                                sh = (j - 1) if nt_ == 3 else 0
                                for kc in range(8):
                                    cb = g['base'] + j * g['n'] + c0
                                    a = 1 + ti * 128 + sh
                                    k.mm(ps[:, 0:w], buf[:, kc, a:a + 128], W[:, kc, cb:cb + w], ii == 0, ii == tot - 1)
                                    ii += 1
                            o = nexto()
                            k.copy(o[:, 0:w], ps[:, 0:w], e=('act' if (ti + c0) % 2 else 'dve'))
                            k.dma(io[dst][t0 + ti * 128:t0 + (ti + 1) * 128, c0:c0 + w], o[:, 0:w], q='pool')

            make_hT(0)
            for gi in range(len(glist)):
                if gi + 1 < len(glist):
                    make_hT(gi + 1)
                proj(gi)
            k.barrier()


def host_inputs(inp, b, consts, plans):
    m = {}
    m['x0'] = np.concatenate([inp['x'][b], inp['ctx'][b]], axis=0)
    m['cc'] = np.stack([inp['c'][b], inp['c_ctx']], axis=0)
    m.update(consts)
    for nm in ('norm1_g', 'norm2_g', 'ada_w', 'ada_b', 'attn_sink', 'mlstm_gate_b', 'mlstm_ln_w', 'rwkv_w0', 'rwkv_w_up', 'rwkv_a0', 'rwkv_a_up', 'rwkv_g_up',
               'rwkv_k_k', 'rwkv_k_a', 'rwkv_r_k', 'rwkv_ln_w', 'rwkv_ln_b', 'rwkv_v0', 'rwkv_v_up',
               'w_out', 'router_w', 'router_b', 'moe_w1', 'moe_b1', 'moe_w2', 'moe_b2', 'final_norm_g'):
        m[nm] = inp[nm]
    ones = np.ones((1,), np.float32)
    for l in range(DEPTH):
        src, cw, groups = plans[l]
        wext = inp['w_in'][l] if l == 0 else np.concatenate([inp['w_in'][l], inp['rwkv_v_down'][l - 1]], axis=1)
        m[f'wx{l}'] = np.ascontiguousarray(wext[:, src])
        cs = np.empty((1, len(src)), np.float32)
        which = np.array([c[0] for c in cw]); jj = np.array([c[1] for c in cw]); cc_ = np.array([c[2] for c in cw])
        cs[0, which == 0] = ones[0]
        cs[0, which == 1] = inp['rwkv_conv'][l][jj[which == 1], cc_[which == 1]]
        cs[0, which == 2] = inp['mlstm_conv'][l][jj[which == 2], cc_[which == 2]]
        m[f'cs{l}'] = cs
    return m


def att_consts():
    j = np.arange(128)[:, None]
    i = np.arange(128)[None, :]
    ge = (j >= i).astype(np.float32)
    le = (j <= i).astype(np.float32)
    extra = {'tri_le': le, 'tri_ge': ge}
    return {**extra, 'mask_ge': np.tile(ge, (1, 4)).astype(ml_dtypes.bfloat16), 'mask_le': np.tile(le, (1, 4)).astype(ml_dtypes.bfloat16)}


def stage_ATT(self, l):
    k, nc, io = self.k, self.nc, self.io
    need_ctx = l < DEPTH - 1
    with ExitStack() as st:
        kTb = sb(st, nc, 'kTb', [128, N], BF16)
        vb = sb(st, nc, 'vb', [128, NT, 2, 65], BF16)
        mge = sb(st, nc, 'mge', [128, 512], BF16)
        mle = sb(st, nc, 'mle', [128, 512], BF16)
        esink = sb(st, nc, 'esink', [128, 8], F32)
        stg = [sb(st, nc, f'astg{i}', [128, 512], F32) for i in range(2)]
        qf = [sb(st, nc, f'qf{i}', [128, 512], F32) for i in range(2)]
        qb_ = [sb(st, nc, f'qb{i}', [128, 512], BF16) for i in range(2)]
        pt = [sb(st, nc, f'pt{i}', [128, 512], BF16) for i in range(6)]
        osb = [sb(st, nc, f'aosb{i}', [128, 512], F32) for i in range(2)]
        den = sb(st, nc, 'aden', [128, 8], F32)
        k.dma(mge[:], io['mask_ge'])
        k.dma(mle[:], io['mask_le'])
        k.dma(esink[:], io['attn_sink'][l, :].partition_broadcast(128))
        k.act(esink[:], esink[:], AF.Exp)
        k.memset(vb[:, :, :, 64:65], 1.0)
        for c in range(0, N, 512):
            w = min(512, N - c)
            s_ = stg[(c // 512) % 2]
            k.dma(s_[:, 0:w], io['attk_d'][:, c:c + w])
            k.copy(kTb[:, c:c + w], s_[:, 0:w], e='pool')
        for ti in range(NT):
            s_ = stg[ti % 2]
            k.dma(s_[:, 0:128], io['attv_d'][ti * 128:(ti + 1) * 128, :])
            k.copy(vb[:, ti, :, 0:64], s_[:, 0:128].rearrange("p (g d) -> p g d", g=2), e='dve')
        qblocks = list(range(64)) + ([64, 65] if need_ctx else [])
        pi = 0
        for n_, qb in enumerate(qblocks):
            if qb < 64:
                keys = [(kb, m) for kb, m in ((qb - 1, mge), (qb, None), (qb + 1, mle)) if 0 <= kb < 64] + [(64, None), (65, None)]
            else:
                keys = [(64, None), (65, None)]
            q_f, q_b, o_ = qf[n_ % 2], qb_[n_ % 2], osb[n_ % 2]
            for g in range(2):
                k.dma(q_f[g * 64:(g + 1) * 64, :].rearrange("p (h t) -> p h t", h=4),
                      io['attq_d'][g * 256:(g + 1) * 256, qb * 128:(qb + 1) * 128].rearrange("(h d) t -> d h t", d=64))
            k.copy(q_b[:], q_f[:], e='pool')
            for g in range(2):
                ptl = []
                for kb, m in keys:
                    ps = k.psum()
                    k.mm(ps[:, :], kTb[g * 64:(g + 1) * 64, kb * 128:(kb + 1) * 128], q_b[g * 64:(g + 1) * 64, :], True, True)
                    p_ = pt[pi % 6]
                    pi += 1
                    k.act(p_[:], ps[:, :], AF.Exp, scale=0.125)
                    if m is not None:
                        k.tt(p_[:], p_[:], m[:], ALU.mult, e='pool')
                    ptl.append((p_, kb))
                po = k.psum()
                for h in range(4):
                    for i_, (p_, kb) in enumerate(ptl):
                        k.mm(po[:, h * 65:(h + 1) * 65], p_[:, h * 128:(h + 1) * 128], vb[:, kb, g, :], i_ == 0, i_ == len(ptl) - 1)
                pv = po[:, 0:260].rearrange("p (h e) -> p h e", h=4)
                k.tt(den[:, g * 4:(g + 1) * 4], pv[:, :, 64], esink[:, g * 4:(g + 1) * 4], ALU.add)
                k.op('dve', lambda E: E.reciprocal(den[:, g * 4:(g + 1) * 4], den[:, g * 4:(g + 1) * 4]), [den[:]], [den[:]])
                for h in range(4):
                    hh = g * 4 + h
                    k.ts(o_[:, hh * 64:(hh + 1) * 64], po[:, h * 65:h * 65 + 64], den[:, hh:hh + 1], None, ALU.mult)
            k.dma(io['mix_d'][qb * 128:(qb + 1) * 128, 0:512], o_[:], q='pool')
        k.barrier()


Prog.stage_ATT = stage_ATT


def stage_ML(self, l):
    k, nc, io = self.k, self.nc, self.io
    need_ctx = l < DEPTH - 1
    with ExitStack() as st:
        ysum = sb(st, nc, 'ysum', [128, NT, 256], F32)
        tri = {0: sb(st, nc, 'tri_le', [128, 128], F32), 1: sb(st, nc, 'tri_ge', [128, 128], F32)}
        ones = sb(st, nc, 'ones_f', [128, 128], F32)
        identb = sb(st, nc, 'identb', [128, 128], BF16)
        gb = sb(st, nc, 'gb', [128, 16], F32)
        lnw = sb(st, nc, 'lnw', [128, 256], F32)
        C = sb(st, nc, 'Cst', [128, 2, 65], F32)
        Cb = sb(st, nc, 'Cstb', [128, 2, 65], BF16)
        qkf = [sb(st, nc, f'qkf{i}', [128, 4, 128], F32) for i in range(2)]
        qkb = [sb(st, nc, f'qkb{i}', [128, 4, 128], BF16) for i in range(2)]
        vf = [sb(st, nc, f'vf{i}', [128, 528], F32) for i in range(2)]
        va = [sb(st, nc, f'va{i}', [128, 4, 65], BF16) for i in range(2)]
        gt = [sb(st, nc, f'gt{i}', [128, 48], F32) for i in range(2)]
        kpp = [sb(st, nc, f'kpp{i}', [128, 128], BF16) for i in range(2)]
        pm = [sb(st, nc, f'pm{i}', [128, 128], BF16) for i in range(2)]
        sc = [sb(st, nc, f'sc{i}', [128, 8], F32) for i in range(2)]
        k.dma(tri[0][:], io['tri_le']); k.dma(tri[1][:], io['tri_ge']); k.dma(identb[:], io['ident_b'])
        k.memset(ones[:], 1.0)
        k.dma(gb[:], io['mlstm_gate_b'][l, :].partition_broadcast(128))
        k.dma(lnw[:], io['mlstm_ln_w'][l, :].partition_broadcast(128))
        it = 0
        for d in range(2):
            order = [64, 65] + list(range(64)) if d == 0 else [65, 64] + list(range(63, -1, -1))
            k.memset(C[:], 0.0)
            k.memset(Cb[:], 0.0)
            for ti in order:
                i2 = it % 2
                it += 1
                t0 = ti * 128
                qf_, qb_, vf_, va_, g_, s_ = qkf[i2], qkb[i2], vf[i2], va[i2], gt[i2], sc[i2]
                k.dma(qf_[:], io['mlqk_d'][:, t0:t0 + 128].rearrange("(s p) t -> p s t", p=128))
                k.dma(vf_[:], io['mlvog_d'][t0:t0 + 128, :])
                k.copy(qb_[:], qf_[:], e='pool')
                k.copy(va_[:, :, 0:64], vf_[:, 0:256].rearrange("p (h e) -> p h e", h=4), e='pool')
                k.memset(va_[:, :, 64:65], 1.0)
                k.tt(g_[:, 0:8], vf_[:, 512 + d * 8:512 + d * 8 + 8], gb[:, d * 8:d * 8 + 8], ALU.add)
                k.act(g_[:, 8:12], g_[:, 4:8], AF.Exp, scale=-1.0)
                k.act(g_[:, 8:12], g_[:, 8:12], AF.Ln, bias=1.0)
                k.ts(g_[:, 4:8], g_[:, 8:12], -1.0, None, ALU.mult)
                ps = k.psum()
                k.mm(ps[:, 0:4], tri[d][:], g_[:, 4:8], True, True)
                k.mm(ps[:, 4:8], ones[:], g_[:, 4:8], True, True)
                k.copy(g_[:, 12:20], ps[:, 0:8])
                k.tt(g_[:, 20:24], g_[:, 0:4], g_[:, 12:16], ALU.subtract)
                k.act(g_[:, 24:28], g_[:, 20:24], AF.Exp)
                k.ts(g_[:, 24:28], g_[:, 24:28], 0.125, None, ALU.mult)
                k.tt(g_[:, 28:32], g_[:, 20:24], g_[:, 16:20], ALU.add)
                k.act(g_[:, 28:32], g_[:, 28:32], AF.Exp)
                k.ts(g_[:, 28:32], g_[:, 28:32], 0.125, None, ALU.mult)
                k.act(g_[:, 32:36], g_[:, 12:16], AF.Exp)
                k.act(g_[:, 36:40], g_[:, 16:20], AF.Exp)
                for pr in range(2):
                    pT = k.psum()
                    pTb = pT[:].bitcast(BF16)
                    k.tr(pTb[:, 0:128], qb_[:, 2 + pr, :], identb[:])
                    kp = kpp[pr]
                    for hh in range(2):
                        h = pr * 2 + hh
                        k.ts(kp[:, hh * 64:(hh + 1) * 64], pTb[:, hh * 64:(hh + 1) * 64], g_[:, 28 + h:29 + h], None, ALU.mult,
                             e=('dve' if hh else 'pool') if False else 'dve')
                    for hh in range(2):
                        h = pr * 2 + hh
                        p0 = hh * 64
                        pS = k.psum()
                        k.mm(pS[:, 0:128], qb_[p0:p0 + 64, 2 + pr, :], qb_[p0:p0 + 64, pr, :], True, True)
                        pm_ = pm[hh]
                        k.stt(pm_[:], pS[:, 0:128], g_[:, 24 + h:25 + h], tri[d][:], ALU.mult, ALU.mult)
                        pO = k.psum()
                        k.mm(pO[:, 0:65], pm_[:], va_[:, h, :], True, False)
                        k.mm(pO[:, 0:65], qb_[p0:p0 + 64, pr, :], Cb[p0:p0 + 64, pr, :], False, True)
                        k.ts(s_[:, 0:1], pO[:, 64:65], g_[:, 32 + h:33 + h], None, ALU.mult)
                        k.act(s_[:, 0:1], s_[:, 0:1], AF.Abs)
                        k.ts(s_[:, 0:1], s_[:, 0:1], 1.0, None, ALU.max)
                        k.op('dve', lambda E: E.reciprocal(s_[:, 1:2], s_[:, 0:1]), [s_[:]], [s_[:]])
                        k.tt(s_[:, 2:3], s_[:, 1:2], g_[:, 32 + h:33 + h], ALU.mult)
                        yv = ysum[:, ti, h * 64:(h + 1) * 64]
                        if d == 0:
                            k.ts(yv, pO[:, 0:64], s_[:, 2:3], None, ALU.mult)
                        else:
                            k.stt(yv, pO[:, 0:64], s_[:, 2:3], yv, ALU.mult, ALU.add)
                    pC = k.psum()
                    k.mm(pC[:, 0:130], kp[:], va_[:, pr * 2:pr * 2 + 2, :], True, True)
                    for hh in range(2):
                        h = pr * 2 + hh
                        p0 = hh * 64
                        k.stt(C[p0:p0 + 64, pr, :], C[p0:p0 + 64, pr, :], g_[p0:p0 + 64, 36 + h:37 + h],
                              pC[p0:p0 + 64, hh * 65:(hh + 1) * 65], ALU.mult, ALU.add)
                    k.copy(Cb[:, pr, :], C[:, pr, :], e='pool')
        tiles = list(range(64)) + ([64, 65] if need_ctx else [])
        for n_, ti in enumerate(tiles):
            i2 = n_ % 2
            vf_, g_ = vf[i2], gt[i2]
            o_ = qkf[i2][:, 0:2, :]
            yc = qkf[i2][:, 2:4, :]
            k.dma(vf_[:, 0:256], io['mlvog_d'][ti * 128:(ti + 1) * 128, 256:512])
            k.act(vf_[:, 0:256], vf_[:, 0:256], AF.Sigmoid)
            k.op('dve', lambda E: E.reduce_sum(g_[:, 0:4], ysum[:, ti, :].rearrange("p (h e) -> p h e", h=4), AX.X),
                 [ysum[:]], [g_[:]])
            k.ts(g_[:, 0:4], g_[:, 0:4], -1.0 / 64, None, ALU.mult)
            for h in range(4):
                k.ts(yc[:, h // 2, (h % 2) * 64:(h % 2) * 64 + 64], ysum[:, ti, h * 64:(h + 1) * 64], g_[:, h:h + 1], None, ALU.add)
                k.act(o_[:, h // 2, (h % 2) * 64:(h % 2) * 64 + 64], yc[:, h // 2, (h % 2) * 64:(h % 2) * 64 + 64], AF.Square,
                      accum_out=g_[:, 4 + h:5 + h])
            k.act(g_[:, 8:12], g_[:, 4:8], AF.Sqrt, bias=1e-5, scale=1.0 / 64)
            k.op('dve', lambda E: E.reciprocal(g_[:, 12:16], g_[:, 8:12]), [g_[:]], [g_[:]])
            for h in range(4):
                k.stt(o_[:, h // 2, (h % 2) * 64:(h % 2) * 64 + 64], yc[:, h // 2, (h % 2) * 64:(h % 2) * 64 + 64], g_[:, 12 + h:13 + h],
                      lnw[:, h * 64:(h + 1) * 64], ALU.mult, ALU.mult)
            k.tt(vf_[:, 256:512], o_.rearrange("p a b -> p (a b)"), vf_[:, 0:256], ALU.mult, e='pool')
            k.dma(io['mix_d'][ti * 128:(ti + 1) * 128, 768:1024], vf_[:, 256:512], q='pool')
        k.barrier()


Prog.stage_ML = stage_ML


def stage_RW(self, l):
    k, nc, io = self.k, self.nc, self.io
    need_ctx = l < DEPTH - 1
    with ExitStack() as st:
        identf = sb(st, nc, 'identf', [128, 128], F32)
        k.dma(identf[:], io['ident_f'])
        bc = {}
        for nm, src in (('k_k', io['rwkv_k_k'][l, :]), ('k_a', io['rwkv_k_a'][l, :]), ('r_k', io['rwkv_r_k'][l].rearrange("h e -> (h e)")),
                        ('a0_0', io['rwkv_a0'][l, 0, :]), ('a0_1', io['rwkv_a0'][l, 1, :])) + \
                (() if l == 0 else (('v0', io['rwkv_v0'][l - 1, :]),)):
            bc[nm] = sb(st, nc, 'rbc_' + nm, [128, 256], F32)
            k.dma(bc[nm][:], src.partition_broadcast(128))
        up = sb(st, nc, 'rw_up', [128, 2, 256], F32)
        gup = sb(st, nc, 'rw_gup', [64, 256], F32)
        vup = sb(st, nc, 'rw_vup', [32, 256], F32)
        w0c = sb(st, nc, 'rw_w0c', [128, 4], F32)
        for d in range(2):
            k.dma(up[d * 32:(d + 1) * 32, 0, :], io['rwkv_w_up'][l, d])
            k.dma(up[d * 32:(d + 1) * 32, 1, :], io['rwkv_a_up'][l, d])
            k.dma(w0c[:, d * 2:d * 2 + 2], io['rwkv_w0'][l, d, :].rearrange("(c p) -> p c", p=128), allow_slow_non_contiguous=True)
        k.dma(gup[:], io['rwkv_g_up'][l])
        if l > 0:
            k.dma(vup[:], io['rwkv_v_up'][l - 1])
        NB = 2
        xr = [sb(st, nc, f'rxr{i}', [128, 768], F32) for i in range(NB)]
        xf = [sb(st, nc, f'rxf{i}', [64, 128], F32) for i in range(NB)]
        xaf = [sb(st, nc, f'rxaf{i}', [64, 128], F32) for i in range(NB)]
        gf = [sb(st, nc, f'rgf{i}', [64, 128], F32) for i in range(NB)]
        lvf = [sb(st, nc, f'rlv{i}', [32, 128], F32) for i in range(NB)]
        vfst = [sb(st, nc, f'rvf{i}', [128, 256], F32) for i in range(NB)]
        kk = [sb(st, nc, f'rkk{i}', [128, 256], F32) for i in range(NB)]
        t1 = [sb(st, nc, f'rt1{i}', [128, 256], F32) for i in range(4)]
        t2 = [sb(st, nc, f'rt2{i}', [128, 256], F32) for i in range(4)]
        sm = [sb(st, nc, f'rsm{i}', [128, 16], F32) for i in range(NB)]
        fo = [sb(st, nc, f'rfo{i}', [128, 128], F32) for i in range(4)]
        tb = [sb(st, nc, f'rtb{i}', [128, 256], BF16) for i in range(6)]
        cnt = [0]

        def T1():
            cnt[0] += 1
            return t1[cnt[0] % 4]

        def T2():
            cnt[0] += 1
            return t2[cnt[0] % 4]

        def FO():
            cnt[0] += 1
            return fo[cnt[0] % 4]

        def TB():
            cnt[0] += 1
            return tb[cnt[0] % 6]

        for ti in range(NT):
            i2 = ti % NB
            t0 = ti * 128
            x_, xf_, gf_, kk_, sm_ = xr[i2], xf[i2], gf[i2], kk[i2], sm[i2]
            k.dma(x_[:], io['rwrkv_d'][t0:t0 + 128, :])
            k.dma(xf_[:], io['rwx_d'][0:64, t0:t0 + 128])
            k.dma(xaf[i2][:], io['rwx_d'][64:128, t0:t0 + 128])
            k.dma(gf_[:], io['rwx_d'][128:192, t0:t0 + 128])
            r_, k_, v_ = x_[:, 0:256], x_[:, 256:512], x_[:, 512:768]
            if l == 0:
                k.dma(io['vfirst_d'][t0:t0 + 128, :], v_, q='pool')
            else:
                k.dma(lvf[i2][:], io['vdown_d'][:, t0:t0 + 128])
                k.dma(vfst[i2][:], io['vfirst_d'][t0:t0 + 128, :])
                ps = k.psum()
                k.mm(ps[:, 0:256], lvf[i2][:], vup[:], True, True)
                a = T1()
                k.tt(a[:], ps[:, 0:256], bc['v0'][:], ALU.add)
                k.act(a[:], a[:], AF.Sigmoid)
                b_ = T2()
                k.tt(b_[:], vfst[i2][:], v_, ALU.subtract)
                k.tt(b_[:], b_[:], a[:], ALU.mult, e='pool')
                k.tt(v_, v_, b_[:], ALU.add)
            vb_ = TB()
            k.copy(vb_[:], v_, e='pool')
            k.dma(io['rv_d'][t0:t0 + 128, :], vb_[:], q='pool')
            k.tt(kk_[:], k_, bc['k_k'][:], ALU.mult)
            a = T1()
            for h in range(4):
                k.act(a[:, h * 64:(h + 1) * 64], kk_[:, h * 64:(h + 1) * 64], AF.Square, accum_out=sm_[:, h:h + 1])
            k.act(sm_[:, 4:8], sm_[:, 0:4], AF.Sqrt)
            k.ts(sm_[:, 4:8], sm_[:, 4:8], 1e-12, None, ALU.max)
            k.op('dve', lambda E: E.reciprocal(sm_[:, 8:12], sm_[:, 4:8]), [sm_[:]], [sm_[:]])
            for h in range(4):
                k.ts(kk_[:, h * 64:(h + 1) * 64], kk_[:, h * 64:(h + 1) * 64], sm_[:, 8 + h:9 + h], None, ALU.mult)
            nk_ = TB()
            k.ts(nk_[:], kk_[:], -1.0, None, ALU.mult, e='pool')
            k.dma(io['rnk_d'][t0:t0 + 128, :], nk_[:], q='pool')
            for src_, dst in ((r_, 'rfm_d'),):
                for c in range(2):
                    ps = k.psum()
                    k.tr(ps[:, 0:128], src_[:, c * 128:(c + 1) * 128], identf[:])
                    f_ = FO()
                    k.copy(f_[:], ps[:, 0:128], e='act')
                    k.dma(io[dst][c * 128:(c + 1) * 128, t0:t0 + 128], f_[:], q='pool')
            k.act(gf_[:], gf_[:], AF.Sigmoid)
            ps = k.psum()
            k.mm(ps[:, 0:256], gf_[:], gup[:], True, True)
            a = T1()
            k.copy(a[:], ps[:, 0:256], e='act')
            k.dma(io['rgate_d'][t0:t0 + 128, :], a[:], q='pool')
            k.act(xf_[0:64, :], xf_[0:64, :], AF.Tanh)
            for d in range(2):
                for c in range(2):
                    ps = k.psum()
                    k.mm(ps[:, 0:128], up[d * 32:(d + 1) * 32, 0, c * 128:(c + 1) * 128], xf_[d * 32:(d + 1) * 32, :], True, True)
                    f_ = FO()
                    k.act(f_[:], ps[:, 0:128], AF.Sigmoid, bias=w0c[:, d * 2 + c:d * 2 + c + 1])
                    k.act(f_[:], f_[:], AF.Exp, scale=-0.6065306597126334)
                    k.dma(io['rw_d'][d, c * 128:(c + 1) * 128, t0:t0 + 128], f_[:], q='pool')
                ps = k.psum()
                k.mm(ps[:, 0:256], xaf[i2][d * 32:(d + 1) * 32, :], up[d * 32:(d + 1) * 32, 1, :], True, True)
                a = T1()
                k.tt(a[:], ps[:, 0:256], bc[f'a0_{d}'][:], ALU.add)
                k.act(a[:], a[:], AF.Sigmoid)
                b_ = TB()
                k.tt(b_[:], kk_[:], a[:], ALU.mult, e='pool')
                k.dma(io['rkka_d'][d, t0:t0 + 128, :], b_[:], q='pool')
                c_ = T2()
                k.stt(c_[:], a[:], -1.0, bc['k_a'][:], ALU.add, ALU.mult)
                k.stt(c_[:], c_[:], 1.0, k_, ALU.add, ALU.mult)
                cb_ = TB()
                k.copy(cb_[:], c_[:], e='pool')
                k.dma(io['rkd_d'][d, t0:t0 + 128, :], cb_[:], q='pool')
                e_ = T1()
                k.tt(e_[:], c_[:], bc['r_k'][:], ALU.mult, e='pool')
                k.tt(e_[:], e_[:], r_, ALU.mult)
                k.op('dve', lambda E: E.reduce_sum(sm_[:, 12:16], e_[:].rearrange("p (h e) -> p h e", h=4), AX.X), [e_[:]], [sm_[:]])
                f2 = T2()
                for h in range(4):
                    k.ts(f2[:, h * 64:(h + 1) * 64], v_[:, h * 64:(h + 1) * 64], sm_[:, 12 + h:13 + h], None, ALU.mult)
                k.dma(io['rbonus_d'][d, t0:t0 + 128, :], f2[:], q='pool')
        k.barrier()
    SB = 8
    with ExitStack() as st:
        sel = sb(st, nc, 'rsel', [128, 2], F32)
        k.memset(sel[:], 0.0)
        k.memset(sel[0:64, 0:1], 1.0)
        k.memset(sel[64:128, 1:2], 1.0)
        NBUF = 3
        mkf = lambda nm, shp, dt=F32: [[sb(st, nc, f'{nm}{g}_{i}', shp, dt) for i in range(NBUF)] for g in range(2)]
        Hist = mkf('rH', [128, SB, 2, 64])
        Sb = [[sb(st, nc, f'rSb{g}_{i}', [128, 2, 64], BF16) for i in range(2)] for g in range(2)]
        S0 = [sb(st, nc, f'rS0{g}', [128, 2, 64], F32) for g in range(2)]
        Ml = [[sb(st, nc, f'rMl{g}_{i}', [128, 2, 128], BF16) for i in range(3)] for g in range(2)]
        Rr = [mkf(f'rR{d}', [128, SB]) for d in range(2)]
        Wt = [mkf(f'Wt{d}', [128, SB]) for d in range(2)]
        NK = [mkf(f'NK{d}', [2, SB, 128], BF16) for d in range(2)]
        KA = [mkf(f'KA{d}', [2, SB, 128], BF16) for d in range(2)]
        KD = [mkf(f'KD{d}', [2, SB, 128], BF16) for d in range(2)]
        VV = [mkf(f'VV{d}', [2, SB, 64], BF16) for d in range(2)]
        YY = mkf('YY', [2, SB, 2, 64])
        ytmp = [sb(st, nc, f'rytmp{i}', [128, SB, 2, 64], F32) for i in range(2)]
        for g in range(2):
            k.memset(S0[g][:], 0.0)
            k.memset(Sb[g][0][:], 0.0)
            for d in range(2):
                for bi_ in range(NBUF):
                    for tl in (NK, KA, KD):
                        k.memset(tl[d][g][bi_][:], 0.0)
        import os as _os
        nblk = int(_os.environ.get('RW_NBLK', N // SB))

        def tokrange(blk, d):
            s0 = blk * SB
            if s0 < LC:
                return T + s0 if d == 0 else T + LC - s0 - SB
            return s0 - LC if d == 0 else T - (s0 - LC) - SB

        def load_block(blk, g):
            bi = blk % NBUF
            for d in range(2):
                tk = tokrange(blk, d)
                k.dma(Rr[d][g][bi][:], io['rfm_d'][g * 128:(g + 1) * 128, tk:tk + SB])
                k.dma(Wt[d][g][bi][:], io['rw_d'][d, g * 128:(g + 1) * 128, tk:tk + SB])
                for hh in range(2):
                    c0 = (g * 2 + hh) * 64
                    k.dma(NK[d][g][bi][hh:hh + 1, :, hh * 64:(hh + 1) * 64], io['rnk_d'][tk:tk + SB, c0:c0 + 64])
                    k.dma(KA[d][g][bi][hh:hh + 1, :, hh * 64:(hh + 1) * 64], io['rkka_d'][d, tk:tk + SB, c0:c0 + 64])
                    k.dma(KD[d][g][bi][hh:hh + 1, :, hh * 64:(hh + 1) * 64], io['rkd_d'][d, tk:tk + SB, c0:c0 + 64])
                    k.dma(VV[d][g][bi][hh:hh + 1, :, :], io['rv_d'][tk:tk + SB, c0:c0 + 64])

        nsteps = nblk * SB

        def cols(s):
            sl = s % SB
            return (sl, SB - 1 - sl)

        def build_M(s, g):
            bi = (s // SB) % NBUF
            col = cols(s)
            pM = k.psum()
            for d in range(2):
                k.mm(pM[:, d * 128:(d + 1) * 128], NK[d][g][bi][0:2, col[d], :], KA[d][g][bi][0:2, col[d], :], True, True)
            k.copy(Ml[g][s % 3][:].rearrange("p d c -> p (d c)"), pM[:, 0:256], e='act')

        def prev_state(s, g, d):
            if s == 0:
                return S0[g][:, d, :]
            ps_ = s - 1
            return Hist[g][(ps_ // SB) % NBUF][:, cols(ps_)[d], d, :]

        def block_out(blk, g):
            bi = blk % NBUF
            yt_ = ytmp[g]
            for d in range(2):
                k.tt(yt_[:, :, d, :], Hist[g][bi][:, :, d, :],
                     Rr[d][g][bi][:].unsqueeze(2).to_broadcast([128, SB, 64]), ALU.mult, e='pool')
            yv = yt_[:].rearrange("p s d v -> p (s d v)")
            yy = YY[g][bi]
            for hf in range(2):
                pY = k.psum()
                k.mm(pY[0:2, :], sel[:], yv[:, hf * 512:(hf + 1) * 512], True, True)
                k.copy(yy[:].rearrange("p s d v -> p (s d v)")[:, hf * 512:(hf + 1) * 512], pY[0:2, :], e='act')
            if not _os.environ.get('RW_NOSTORE'):
                for d in range(2):
                    tk = tokrange(blk, d)
                    for hh in range(2):
                        c0 = (g * 2 + hh) * 64
                        k.dma(io['ryscan_d'][d, tk:tk + SB, c0:c0 + 64], yy[hh:hh + 1, :, d, :], q='pool')

        for g in range(2):
            load_block(0, g)
            if nblk > 1:
                load_block(1, g)
        for g in range(2):
            build_M(0, g)
        for s in range(nsteps):
            blk, sl = s // SB, s % SB
            bi = blk % NBUF
            col = cols(s)
            if sl == 0 and blk + 2 < nblk and not _os.environ.get('RW_NOLOAD'):
                for g in range(2):
                    load_block(blk + 2, g)
            if s + 1 < nsteps:
                for g in range(2):
                    build_M(s + 1, g)
            pS = []
            for g in range(2):
                p_ = k.psum()
                so = Sb[g][s % 2]
                for d in range(2):
                    k.mm(p_[:, d * 64:(d + 1) * 64], Ml[g][s % 3][:, d, :], so[:, d, :], True, False)
                    k.mm(p_[:, d * 64:(d + 1) * 64], KD[d][g][bi][0:2, col[d], :], VV[d][g][bi][0:2, col[d], :], False, True)
                pS.append(p_)
            for g in range(2):
                sn = Sb[g][(s + 1) % 2]
                for d in range(2):
                    k.stt(sn[:, d, :], prev_state(s, g, d), Wt[d][g][bi][:, col[d]:col[d] + 1], pS[g][:, d * 64:(d + 1) * 64],
                          ALU.mult, ALU.add)
            for g in range(2):
                for d in range(2):
                    k.stt(Hist[g][bi][:, col[d], d, :], prev_state(s, g, d), Wt[d][g][bi][:, col[d]:col[d] + 1],
                          pS[g][:, d * 64:(d + 1) * 64], ALU.mult, ALU.add)
            if sl == SB - 1:
                for g in range(2):
                    block_out(blk, g)
        k.barrier()
    with ExitStack() as st:
        bcw = sb(st, nc, 'rlnw', [128, 256], F32)
        bcb = sb(st, nc, 'rlnb', [128, 256], F32)
        k.dma(bcw[:], io['rwkv_ln_w'][l, :].partition_broadcast(128))
        k.dma(bcb[:], io['rwkv_ln_b'][l, :].partition_broadcast(128))
        yt = [sb(st, nc, f'ryt{i}', [128, 2, 256], F32) for i in range(2)]
        bt = [sb(st, nc, f'rbt{i}', [128, 2, 256], F32) for i in range(2)]
        gtt = [sb(st, nc, f'rgtt{i}', [128, 256], F32) for i in range(2)]
        acc = [sb(st, nc, f'racc{i}', [128, 256], F32) for i in range(2)]
        jk = sb(st, nc, 'rjk', [128, 64], F32)
        sm = [sb(st, nc, f'rsm2{i}', [128, 32], F32) for i in range(2)]
        tiles = list(range(64)) + ([64, 65] if need_ctx else [])
        for n_, ti in enumerate(tiles):
            i2 = n_ % 2
            t0 = ti * 128
            y_, b_, g_, a_, s_ = yt[i2], bt[i2], gtt[i2], acc[i2], sm[i2]
            for d in range(2):
                k.dma(y_[:, d, :], io['ryscan_d'][d, t0:t0 + 128, :])
                k.dma(b_[:, d, :], io['rbonus_d'][d, t0:t0 + 128, :])
            k.dma(g_[:], io['rgate_d'][t0:t0 + 128, :])
            k.op('dve', lambda E: E.reduce_sum(s_[:, 0:8], y_[:].rearrange("p d (h e) -> p (d h) e", h=4), AX.X), [y_[:]], [s_[:]])
            k.ts(s_[:, 0:8], s_[:, 0:8], -1.0 / 64, None, ALU.mult)
            for j in range(8):
                d, h = j // 4, j % 4
                k.ts(y_[:, d, h * 64:(h + 1) * 64], y_[:, d, h * 64:(h + 1) * 64], s_[:, j:j + 1], None, ALU.add)
                k.act(jk[:], y_[:, d, h * 64:(h + 1) * 64], AF.Square, accum_out=s_[:, 8 + j:9 + j])
            k.act(s_[:, 16:24], s_[:, 8:16], AF.Sqrt, bias=64e-5, scale=1.0 / 64)
            k.op('dve', lambda E: E.reciprocal(s_[:, 24:32], s_[:, 16:24]), [s_[:]], [s_[:]])
            for j in range(8):
                d, h = j // 4, j % 4
                k.stt(y_[:, d, h * 64:(h + 1) * 64], y_[:, d, h * 64:(h + 1) * 64], s_[:, 24 + j:25 + j], bcw[:, h * 64:(h + 1) * 64],
                      ALU.mult, ALU.mult)
            k.tt(a_[:], y_[:, 0, :], y_[:, 1, :], ALU.add)
            k.tt(b_[:, 0, :], b_[:, 0, :], b_[:, 1, :], ALU.add, e='pool')
            k.stt(a_[:], bcb[:], 2.0, a_[:], ALU.mult, ALU.add)
            k.tt(a_[:], a_[:], b_[:, 0, :], ALU.add)
            k.tt(a_[:], a_[:], g_[:], ALU.mult, e='pool')
            k.dma(io['mix_d'][t0:t0 + 128, 512:768], a_[:], q='pool')
        k.barrier()


Prog.stage_RW = stage_RW


def build_full():
    P = Prog()
    P.declare()
    out = P.nc.dram_tensor('out', [T, D], F32, kind="ExternalOutput").ap()
    for l in range(DEPTH):
        P.stage_M(l)
        P.stage_A(l)
        P.stage_ATT(l)
        P.stage_ML(l)
        P.stage_RW(l)
        P.stage_C(l, out_ap=out)
    return P


def kernel(**inputs):
    inp = {k_: np.asarray(v) for k_, v in inputs.items()}
    P = build_full()
    consts = host_consts()
    in_maps = []
    for b in range(4):
        m = host_inputs(inp, b, consts, P.plans)
        in_maps.append({k_: np.ascontiguousarray(v) for k_, v in m.items() if k_ in P.io})
    res = run_bass_kernel_spmd(P.nc, in_maps, core_ids=list(range(4)))
    return np.stack([r['out'] for r in res.results], axis=0).astype(np.float32)


def stage_C(self, l, out_ap=None):
    k, nc, io = self.k, self.nc, self.io
    last = l == DEPTH - 1
    xsrc = io['x0'] if l == 0 else io['xs']
    groups = [(g * 1024, 1024, 0) for g in range(8)] + ([] if last else [(T, 256, 1)])
    import os as _os
    if 'C_NGROUPS' in _os.environ:
        groups = groups[:int(_os.environ['C_NGROUPS'])]
    with ExitStack() as st:
        identb = sb(st, nc, 'identb', [128, 128], BF16)
        identf = sb(st, nc, 'identf', [128, 128], F32)
        k.dma(identb[:], io['ident_b']); k.dma(identf[:], io['ident_f'])
        routw = sb(st, nc, 'routw', [128, 8, NE], F32)
        routb = sb(st, nc, 'routb', [128, NE], F32)
        b2 = sb(st, nc, 'b2', [NE, D], F32)
        b1c = [sb(st, nc, f'b1c{i}', [128, 16], F32) for i in range(2)]
        wstg = [sb(st, nc, f'cwstg{i}', [128, 1024], F32) for i in range(2)]
        k.dma(routw[:], io['router_w'][l].rearrange("(kc p) e -> p kc e", p=128))
        k.dma(routb[:], io['router_b'][l, :].partition_broadcast(128))
        k.dma(b2[:], io['moe_b2'][l])
        fng = None
        if last:
            fng = sb(st, nc, 'fng', [128, D], F32)
            k.dma(fng[:], io['final_norm_g'].partition_broadcast(128))
        bct = {i: sb(st, nc, f'cbc{i}', [128, D], F32) for i in (2, 3, 4, 5)}
        yacc = sb(st, nc, 'yacc', [128, 8, D], F32)
        h2T = sb(st, nc, 'h2T', [128, 8, 1024], BF16)
        gates = sb(st, nc, 'gates', [128, 8, NE], F32)
        W1s = [sb(st, nc, f'W1b{i}', [128, 8, 2048], BF16) for i in range(2)]
        W2s = [sb(st, nc, f'W2b{i}', [128, 8, D], BF16) for i in range(2)]
        actT = sb(st, nc, 'actT', [128, 8, 512], BF16)
        xt = [sb(st, nc, f'cxt{i}', [128, D], F32) for i in range(1)]
        mixT = sb(st, nc, 'cmixT', [128, 8, 128], BF16)
        tmp = sb(st, nc, 'ctmp', [128, D], F32)
        mixf = tmp
        hb = sb(st, nc, 'chb', [128, D], BF16)
        junk = hb
        mixb = hb
        h2fT = actT[:].rearrange("p a b -> p (a b)").bitcast(F32)[:, 0:1024].rearrange("p (kc t) -> p kc t", kc=8)
        ssq = sb(st, nc, 'cssq', [128, 4], F32)
        rt = [sb(st, nc, f'crt{i}', [128, 96], F32) for i in range(2)]
        gT = sb(st, nc, 'cgT', [NE, 128], F32)
        ev = [sb(st, nc, f'cev{i}', [128, 512], F32) for i in range(3)]
        ecnt = [0]
        evc = [0]

        def EV():
            evc[0] += 1
            return ev[evc[0] % 3]

        cur_seg = [None]
        wc = [0]
        for (g0, ntg, seg) in groups:
            if cur_seg[0] != seg:
                for i in (2, 3, 4, 5):
                    k.dma(bct[i][:], io[f'modR{l}'][seg, i * D:(i + 1) * D].partition_broadcast(128))
                cur_seg[0] = seg
            G2, A2, S2, G5 = bct[2], bct[3], bct[4], bct[5]
            nti = ntg // 128
            Wout = W2s[0]
            for kc in range(8):
                s_ = wstg[kc % 2]
                k.dma(s_[:], io['w_out'][l, kc * 128:(kc + 1) * 128, :])
                k.copy(Wout[:, kc, :], s_[:], e='pool')
            for ti in range(nti):
                t0 = g0 + ti * 128
                x_ = xt[0]
                k.dma(x_[:], xsrc[t0:t0 + 128, :])
                k.dma(mixf[:], io['mix_d'][t0:t0 + 128, :])
                k.copy(mixb[:], mixf[:], e='pool')
                ps = k.psum()
                pb = ps[:].bitcast(BF16)
                for kc in range(8):
                    k.tr(pb[:, kc * 128:(kc + 1) * 128], mixb[:, kc * 128:(kc + 1) * 128], identb[:])
                k.copy(mixT[:], pb[:, 0:1024].rearrange("p (kc t) -> p kc t", kc=8), e='act')
                for half in range(2):
                    ps = k.psum()
                    for kc in range(8):
                        k.mm(ps[:, :], mixT[:, kc, :], Wout[:, kc, half * 512:(half + 1) * 512], kc == 0, kc == 7)
                    k.tt(tmp[:, half * 512:(half + 1) * 512], ps[:, :], G2[:, half * 512:(half + 1) * 512], ALU.mult)
                k.tt(x_[:], x_[:], tmp[:], ALU.add, e='pool')
                k.dma(io['xs'][t0:t0 + 128, :], x_[:], q='pool')
                k.act(junk[:], x_[:], AF.Square, accum_out=ssq[:, 0:1])
                k.act(ssq[:, 1:2], ssq[:, 0:1], AF.Sqrt, bias=1e-6, scale=1.0 / D)
                k.op('dve', lambda E: E.reciprocal(ssq[:, 2:3], ssq[:, 1:2]), [ssq[:]], [ssq[:]])
                k.stt(tmp[:], x_[:], ssq[:, 2:3], A2[:], ALU.mult, ALU.mult)
                k.tt(tmp[:], tmp[:], S2[:], ALU.add, e='pool')
                k.copy(hb[:], tmp[:], e='pool')
                ps = k.psum()
                pb = ps[:].bitcast(BF16)
                for kc in range(8):
                    k.tr(pb[:, kc * 128:(kc + 1) * 128], hb[:, kc * 128:(kc + 1) * 128], identb[:])
                k.copy(h2T[:, :, ti * 128:(ti + 1) * 128], pb[:, 0:1024].rearrange("p (kc t) -> p kc t", kc=8), e='act')
                for hh in range(2):
                    ps = k.psum()
                    for q_ in range(4):
                        kc = hh * 4 + q_
                        k.tr(ps[:, q_ * 128:(q_ + 1) * 128], tmp[:, kc * 128:(kc + 1) * 128], identf[:])
                    k.copy(h2fT[:, hh * 4:(hh + 1) * 4, :], ps[:, :].rearrange("p (kc t) -> p kc t", kc=4), e='act')
                ps = k.psum()
                for kc in range(8):
                    k.mm(ps[:, 0:NE], h2fT[:, kc, :], routw[:, kc, :], kc == 0, kc == 7)
                r_ = rt[ti % 2]
                k.tt(r_[:, 0:32], ps[:, 0:NE], routb[:], ALU.add)
                k.op('dve', lambda E: E.max(r_[:, 32:40], r_[:, 0:32]), [r_[:]], [r_[:]])
                k.ts(r_[:, 40:41], r_[:, 32:33], -1.0, None, ALU.mult)
                k.ts(r_[:, 64:96], r_[:, 0:32], r_[:, 35:36], None, ALU.is_ge)
                k.act(r_[:, 0:32], r_[:, 0:32], AF.Exp, bias=r_[:, 40:41])
                k.tt(r_[:, 0:32], r_[:, 0:32], r_[:, 64:96], ALU.mult)
                k.op('dve', lambda E: E.reduce_sum(r_[:, 41:42], r_[:, 0:32], AX.X), [r_[:]], [r_[:]])
                k.op('dve', lambda E: E.reciprocal(r_[:, 42:43], r_[:, 41:42]), [r_[:]], [r_[:]])
                k.ts(gates[:, ti, :], r_[:, 0:32], r_[:, 42:43], None, ALU.mult)
                ps = k.psum()
                k.tr(ps[0:NE, 0:128], gates[:, ti, :], identf[:])
                k.copy(gT[:], ps[0:NE, 0:128], e='act')
                for half in range(2):
                    ps = k.psum()
                    k.mm(ps[:, :], gT[:], b2[:, half * 512:(half + 1) * 512], True, True)
                    k.copy(yacc[:, ti, half * 512:(half + 1) * 512], ps[:, :], e='act')
            def weight_tasks(e):
                W1b, W2b = W1s[(e + 1) % 2], W2s[(e + 1) % 2]
                tasks = []

                def t_b1():
                    k.dma(b1c[e % 2][:], io['moe_b1'][l, e, :].rearrange("(c p) -> p c", p=128), allow_slow_non_contiguous=True)
                tasks.append(t_b1)
                for kc in range(8):
                    for half in range(2):
                        def t1(kc=kc, half=half):
                            s_ = wstg[wc[0] % 2]
                            wc[0] += 1
                            k.dma(s_[:], io['moe_w1'][l, e, kc * 128:(kc + 1) * 128, half * 1024:(half + 1) * 1024])
                            k.copy(W1b[:, kc, half * 1024:(half + 1) * 1024], s_[:], e='act')
                        tasks.append(t1)
                for fc in range(8):
                    def t2(fc=fc):
                        s_ = wstg[wc[0] % 2]
                        wc[0] += 1
                        k.dma(s_[:], io['moe_w2'][l, e, fc * 128:(fc + 1) * 128, :])
                        k.copy(W2b[:, fc, :], s_[:], e='act')
                    tasks.append(t2)
                return tasks

            for t_ in weight_tasks(0):
                t_()
            for e in range(NE):
                b1 = b1c[e % 2]
                W1b, W2b = W1s[(e + 1) % 2], W2s[(e + 1) % 2]
                pend = weight_tasks(e + 1) if e + 1 < NE else []
                hsz = min(512, ntg)
                nslots = (ntg // hsz) * (8 + (hsz // 128) * 2)
                per = -(-len(pend) // nslots) if pend else 0

                def drain():
                    for _ in range(per):
                        if pend:
                            pend.pop(0)()

                for hg in range(ntg // hsz):
                    c0 = hg * hsz
                    for i in range(8):
                        pg = k.psum()
                        for kc in range(8):
                            k.mm(pg[:, 0:hsz], W1b[:, kc, i * 128:(i + 1) * 128], h2T[:, kc, c0:c0 + hsz], kc == 0, kc == 7)
                        pl = k.psum()
                        for kc in range(8):
                            k.mm(pl[:, 0:hsz], W1b[:, kc, 1024 + i * 128:1024 + (i + 1) * 128], h2T[:, kc, c0:c0 + hsz], kc == 0, kc == 7)
                        g_, sg, lt = EV(), EV(), EV()
                        k.ts(g_[:, 0:hsz], pg[:, 0:hsz], b1[:, i:i + 1], 7.0, ALU.add, ALU.min)
                        k.act(sg[:, 0:hsz], g_[:, 0:hsz], AF.Silu, scale=1.702)
                        k.ts(lt[:, 0:hsz], pl[:, 0:hsz], b1[:, 8 + i:9 + i], 7.0, ALU.add, ALU.min)
                        k.ts(lt[:, 0:hsz], lt[:, 0:hsz], -7.0, 1.0, ALU.max, ALU.add)
                        k.stt(actT[:, i, 0:hsz], sg[:, 0:hsz], 1.0 / 1.702, lt[:, 0:hsz], ALU.mult, ALU.mult)
                        drain()
                    for tt_ in range(hsz // 128):
                        ti = (c0 // 128) + tt_
                        for half in range(2):
                            po = k.psum()
                            for fc in range(8):
                                k.mm(po[:, :], actT[:, fc, tt_ * 128:(tt_ + 1) * 128], W2b[:, fc, half * 512:(half + 1) * 512], fc == 0, fc == 7)
                            ys = yacc[:, ti, half * 512:(half + 1) * 512]
                            k.stt(ys, po[:, :], gates[:, ti, e:e + 1], ys, ALU.mult, ALU.add)
                            drain()
                while pend:
                    pend.pop(0)()
            for ti in range(nti):
                t0 = g0 + ti * 128
                x_ = xt[0]
                k.dma(x_[:], io['xs'][t0:t0 + 128, :])
                k.tt(tmp[:], yacc[:, ti, :], G5[:], ALU.mult)
                k.tt(x_[:], x_[:], tmp[:], ALU.add, e='pool')
                if not last:
                    k.dma(io['xs'][t0:t0 + 128, :], x_[:], q='pool')
                else:
                    k.act(junk[:], x_[:], AF.Square, accum_out=ssq[:, 0:1])
                    k.act(ssq[:, 1:2], ssq[:, 0:1], AF.Sqrt, bias=1e-6, scale=1.0 / D)
                    k.op('dve', lambda E: E.reciprocal(ssq[:, 2:3], ssq[:, 1:2]), [ssq[:]], [ssq[:]])
                    k.stt(tmp[:], x_[:], ssq[:, 2:3], fng[:], ALU.mult, ALU.mult)
                    k.dma(out_ap[t0:t0 + 128, :], tmp[:], q='pool')
        k.barrier()


Prog.stage_C = stage_C
```

```python
import numpy as np
import ml_dtypes
from contextlib import ExitStack
import concourse.bass as bass
import concourse.mybir as mybir
from concourse.bass_utils import run_bass_kernel_spmd

F32 = mybir.dt.float32
BF16 = mybir.dt.bfloat16
AF = mybir.ActivationFunctionType
ALU = mybir.AluOpType
AX = mybir.AxisListType

D = 1024
T = 8192
LC = 256
N = T + LC
NT = N // 128
DEPTH = 2
HD = 64
NE = 32
import os as _os0
SEM_ROT = int(_os0.environ.get('SEM_ROT', 20000))


class K:
    def __init__(self, nc, es):
        self.nc = nc
        self.es = es
        self.eng = {'pe': nc.tensor, 'act': nc.scalar, 'dve': nc.vector, 'pool': nc.gpsimd, 'sp': nc.sync}
        self.esems = {e: [es.enter_context(nc.semaphore(f"s_{e}_0"))] for e in ('pe', 'act', 'dve', 'pool')}
        self.ecnt = {e: 0 for e in self.esems}
        self.dsems = [es.enter_context(nc.semaphore(f"s_dma_{i}")) for i in range(44)]
        self.dcnt = [0] * len(self.dsems)
        self.drr = 0
        self.seen = {e: {} for e in self.eng}
        self.res = {}
        self.semobj = {}
        for e, l in self.esems.items():
            self.semobj[id(l[0])] = l[0]
        for s in self.dsems:
            self.semobj[id(s)] = s
        self.psb = [es.enter_context(nc.psum_tensor(f"psb{i}", [128, 512], F32)) for i in range(8)]
        self.psi = 0
        self.ninst = 0
        self.E1 = es.enter_context(nc.semaphore("s_bar1"))
        self.E2 = es.enter_context(nc.semaphore("s_bar2"))
        self.epoch = 0

    def psum(self):
        p = self.psb[self.psi % 8]
        self.psi += 1
        return p

    def _wait(self, e, sem, cnt):
        sid = id(sem)
        if self.seen[e].get(sid, 0) >= cnt:
            return
        self.eng[e].wait_ge(sem, cnt)
        self.seen[e][sid] = cnt
        self.ninst += 1

    def _deps(self, e, reads, writes, acc):
        for ap in reads:
            r = self.res.get(ap.name)
            if r and r['w']:
                self._wait(e, *r['w'])
        for ap in writes:
            r = self.res.get(ap.name)
            if not r:
                continue
            if r['w'] and not (acc and e == 'pe' and r.get('we') == 'pe'):
                self._wait(e, *r['w'])
            for sid, (sem, cnt) in r['r'].items():
                self._wait(e, sem, cnt)

    def _record(self, e, sem, cnt, reads, writes):
        for ap in reads:
            r = self.res.setdefault(ap.name, {'w': None, 'r': {}})
            r['r'][id(sem)] = (sem, cnt)
        for ap in writes:
            r = self.res.setdefault(ap.name, {'w': None, 'r': {}})
            r['w'] = (sem, cnt)
            r['we'] = e
            r['r'] = {}

    def op(self, e, fn, reads, writes, acc=False):
        self._deps(e, reads, writes, acc)
        ins = fn(self.eng[e])
        if self.ecnt[e] >= SEM_ROT:
            s = self.es.enter_context(self.nc.semaphore(f"s_{e}_{len(self.esems[e])}"))
            self.esems[e].append(s)
            self.semobj[id(s)] = s
            self.ecnt[e] = 0
        sem = self.esems[e][-1]
        self.ecnt[e] += 1
        ins.then_inc(sem, 1)
        self._record(e, sem, self.ecnt[e], reads, writes)
        self.ninst += 1
        return ins

    def dma(self, out, in_, q='sp', **kw):
        self._deps(q, [in_], [out], False)
        i = self.drr % len(self.dsems)
        self.drr += 1
        sem = self.dsems[i]
        if self.dcnt[i]:
            self._wait(q, sem, self.dcnt[i])
        ins = self.eng[q].dma_start(out=out, in_=in_, **kw)
        self.dcnt[i] += 16
        ins.then_inc(sem, 16)
        self._record(q, sem, self.dcnt[i], [in_], [out])
        self.ninst += 1

    def barrier(self):
        for e in self.eng:
            for e2, l in self.esems.items():
                if self.ecnt[e2]:
                    self._wait(e, l[-1], self.ecnt[e2])
            for i, s in enumerate(self.dsems):
                if self.dcnt[i]:
                    self._wait(e, s, self.dcnt[i])
        self.res = {}

    def mm(self, out, lhsT, rhs, start, stop):
        return self.op('pe', lambda E: E.matmul(out, lhsT, rhs, start=start, stop=stop), [lhsT, rhs], [out],
                       acc=not start)

    def tr(self, out, in_, ident):
        return self.op('pe', lambda E: E.transpose(out, in_, ident), [in_, ident], [out])

    def act(self, out, in_, func, bias=None, scale=None, accum_out=None, e='act'):
        kw = {}
        rd = [in_]
        wr = [out]
        if bias is not None:
            kw['bias'] = bias
            if not isinstance(bias, (int, float)):
                rd.append(bias)
        if scale is not None:
            kw['scale'] = scale
            if not isinstance(scale, (int, float)):
                rd.append(scale)
        if accum_out is not None:
            kw['accum_out'] = accum_out
            wr.append(accum_out)
        return self.op(e, lambda E: E.activation(out, in_, func, **kw), rd, wr)

    def tt(self, out, a, b, op, e='dve'):
        return self.op(e, lambda E: E.tensor_tensor(out, a, b, op), [a, b], [out])

    def ts(self, out, a, s1, s2, op0, op1=None, e='dve', accum_out=None):
        rd = [a] + [s for s in (s1, s2) if s is not None and not isinstance(s, (int, float))]
        wr = [out] + ([accum_out] if accum_out is not None else [])
        kw = {}
        if op1 is not None:
            kw['op1'] = op1
        if accum_out is not None:
            kw['accum_out'] = accum_out
        return self.op(e, lambda E: E.tensor_scalar(out, a, s1, s2, op0, **kw), rd, wr)

    def stt(self, out, a, s, b, op0, op1, e='dve'):
        rd = [a, b] + ([] if isinstance(s, (int, float)) else [s])
        return self.op(e, lambda E: E.scalar_tensor_tensor(out, a, s, b, op0, op1), rd, [out])

    def copy(self, out, in_, e='dve'):
        if e == 'act':
            return self.op(e, lambda E: E.copy(out, in_), [in_], [out])
        return self.op(e, lambda E: E.tensor_copy(out, in_), [in_], [out])

    def memset(self, out, v, e='pool'):
        return self.op(e, lambda E: E.memset(out, v), [], [out])


ATT_IN = 768
RW0 = 768
ML0 = 1728
NIN = 2768


def _rope_swap_idx():
    i = np.arange(64)
    blk = i // 16
    return np.where(blk % 2 == 0, i + 16, i - 16)


def build_colplan(layer):
    src = []
    cw = []
    groups = []

    def add(name, kind, cols, conv):
        base = len(src)
        nterm = 3 if conv else 1
        for j in range(nterm):
            for c in cols:
                src.append(c)
                if conv == 1:
                    cw.append((1, j, c - RW0))
                elif conv == 2:
                    cw.append((2, j, c - ML0))
                else:
                    cw.append((0, 0, 0))
        groups.append(dict(name=name, kind=kind, base=base, n=len(cols), nterm=nterm))

    sw = _rope_swap_idx()
    q = np.arange(512)
    qs = (q // 64) * 64 + sw[q % 64]
    kk = 512 + np.arange(128)
    ks = 512 + (np.arange(128) // 64) * 64 + sw[np.arange(128) % 64]
    add('attq', 'fm', list(q), 0)
    add('attqs', 'fm', list(qs), 0)
    add('attk', 'fm', list(kk), 0)
    add('attks', 'fm', list(ks), 0)
    add('rwx', 'fm', list(RW0 + 768 + np.arange(192)), 1)
    add('mlqk', 'fm', list(ML0 + np.arange(512)), 2)
    if layer > 0:
        add('vdown', 'fm', list(NIN + np.arange(32)), 0)
    add('attv', 'tm', list(640 + np.arange(128)), 0)
    add('rwrkv', 'tm', list(RW0 + np.arange(768)), 1)
    add('mlvog', 'tm', list(ML0 + 512 + np.arange(528)), 0)
    return np.array(src), cw, groups


_SBN = [0]


def sb(st, nc, name, shape, dt):
    _SBN[0] += 1
    return st.enter_context(nc.sbuf_tensor(f"{name}_u{_SBN[0]}", shape, dt))


def host_consts():
    c = {}
    c['ident_f'] = np.eye(128, dtype=np.float32)
    c['ident_b'] = np.eye(128).astype(ml_dtypes.bfloat16)
    t = np.arange(T)
    row = (t // 64).astype(np.float32)
    col = (t % 64).astype(np.float32)
    inv = (10000.0 ** (-np.arange(0, 32, 2, dtype=np.float32) / 32)).astype(np.float32)
    ang = np.concatenate([row[:, None] * inv, col[:, None] * inv], axis=-1).astype(np.float32)
    cos = np.cos(ang).astype(np.float32)
    sin = np.sin(ang).astype(np.float32)
    ct = np.ones((64, N), np.float32)
    stb = np.zeros((64, N), np.float32)
    for d in range(64):
        blk = d // 16
        f = (blk // 2) * 16 + d % 16
        ct[d, :T] = cos[:, f]
        stb[d, :T] = (-sin[:, f]) if blk % 2 == 0 else sin[:, f]
    c['rope_c'] = np.concatenate([ct, ct], 0)
    c['rope_s'] = np.concatenate([stb, stb], 0)
    c.update(att_consts())
    return c


class Prog:
    def __init__(self, nlayers=DEPTH, stop=None, dbg=()):
        self.nlayers = nlayers
        self.stop = stop
        self.dbg = dbg
        self.es = ExitStack()
        nc = self.nc = bass.Bass("TRN2", target_bir_lowering=False)
        self.k = K(nc, self.es)
        self.io = {}
        self.plans = [build_colplan(l) for l in range(DEPTH)]

    def din(self, name, shape, dt=F32):
        self.io[name] = self.nc.dram_tensor(name, list(shape), dt, kind="ExternalInput").ap()
        return self.io[name]

    def dscr(self, name, shape, dt=F32):
        kind = "ExternalOutput" if name in self.dbg else "Internal"
        self.io[name] = self.nc.dram_tensor(name, list(shape), dt, kind=kind).ap()
        return self.io[name]

    def declare(self):
        self.din('x0', [N, D])
        self.din('cc', [2, D])
        for nm in ('ident_f', 'rope_c', 'rope_s'):
            self.din(nm, {'ident_f': [128, 128], 'rope_c': [128, N], 'rope_s': [128, N]}[nm])
        self.din('ident_b', [128, 128], BF16)
        self.din('norm1_g', [DEPTH, D]); self.din('norm2_g', [DEPTH, D])
        self.din('ada_w', [DEPTH, D, 6 * D]); self.din('ada_b', [DEPTH, 6 * D])
        for l in range(DEPTH):
            ncw = len(self.plans[l][0])
            self.din(f'wx{l}', [D, ncw]); self.din(f'cs{l}', [1, ncw])
            self.dscr(f'modR{l}', [2, 6 * D])
        self.dscr('attq_d', [512, N]); self.dscr('attk_d', [128, N]); self.dscr('rwx_d', [192, N])
        self.dscr('mlqk_d', [512, N]); self.dscr('vdown_d', [32, N])
        self.dscr('attv_d', [N, 128]); self.dscr('rwrkv_d', [N, 768]); self.dscr('mlvog_d', [N, 528])
        self.dscr('xs', [N, D])
        self.dscr('mix_d', [N, D])
        self.din('mask_ge', [128, 512], BF16); self.din('mask_le', [128, 512], BF16)
        self.din('attn_sink', [DEPTH, 8])
        for nm, shp in (('rwkv_w0', [DEPTH, 2, 256]), ('rwkv_w_up', [DEPTH, 2, 32, 256]), ('rwkv_a0', [DEPTH, 2, 256]),
                        ('rwkv_a_up', [DEPTH, 2, 32, 256]), ('rwkv_g_up', [DEPTH, 64, 256]), ('rwkv_k_k', [DEPTH, 256]),
                        ('rwkv_k_a', [DEPTH, 256]), ('rwkv_r_k', [DEPTH, 4, 64]), ('rwkv_ln_w', [DEPTH, 256]),
                        ('rwkv_ln_b', [DEPTH, 256]), ('rwkv_v0', [1, 256]), ('rwkv_v_up', [1, 32, 256])):
            self.din(nm, shp)
        for nm, shp in (('vfirst_d', [N, 256]), ('rfm_d', [256, N]), ('rgate_d', [N, 256]),
                        ('rw_d', [2, 256, N]), ('rbonus_d', [2, N, 256]), ('ryscan_d', [2, N, 256])):
            self.dscr(nm, shp)
        for nm, shp in (('rv_d', [N, 256]), ('rnk_d', [N, 256]), ('rkka_d', [2, N, 256]), ('rkd_d', [2, N, 256])):
            self.dscr(nm, shp, BF16)
        for nm, shp in (('w_out', [DEPTH, D, D]), ('router_w', [DEPTH, D, NE]), ('router_b', [DEPTH, NE]), ('moe_w1', [DEPTH, NE, D, 2 * D]),
                        ('moe_b1', [DEPTH, NE, 2 * D]), ('moe_w2', [DEPTH, NE, D, D]), ('moe_b2', [DEPTH, NE, D]), ('final_norm_g', [D])):
            self.din(nm, shp)
        self.din('tri_le', [128, 128]); self.din('tri_ge', [128, 128])
        self.din('mlstm_gate_b', [DEPTH, 16]); self.din('mlstm_ln_w', [DEPTH, 256])

    def stage_M(self, l):
        k, nc, io = self.k, self.nc, self.io
        with ExitStack() as st:
            cT = sb(st, nc, 'cT', [128, 16], F32)
            adab = sb(st, nc, 'adab', [2, 6 * D], F32)
            gg = sb(st, nc, 'gg', [2, 2 * D], F32)
            mod = sb(st, nc, 'mod', [2, 6 * D], F32)
            R = sb(st, nc, 'R', [2, 6 * D], F32)
            wt = [sb(st, nc, f'adawt{i}', [128, 8, 512], F32) for i in range(2)]
            for r in range(2):
                k.dma(cT[:, r * 8:(r + 1) * 8], io['cc'][r, :].rearrange("(kc p) -> p kc", p=128),
                      allow_slow_non_contiguous=True)
            for r in range(2):
                k.dma(adab[r:r + 1, :], io['ada_b'][l:l + 1, :])
                k.dma(gg[r:r + 1, 0:D], io['norm1_g'][l:l + 1, :])
                k.dma(gg[r:r + 1, D:2 * D], io['norm2_g'][l:l + 1, :])
            k.act(cT[:], cT[:], AF.Silu)
            for ng in range(12):
                w = wt[ng % 2]
                k.dma(w[:], io['ada_w'][l, :, ng * 512:(ng + 1) * 512].rearrange("(kc p) n -> p kc n", p=128))
                ps = k.psum()
                for kc in range(8):
                    k.mm(ps[0:2, :], cT[:, kc:16:8], w[:, kc, :], kc == 0, kc == 7)
                k.tt(mod[:, ng * 512:(ng + 1) * 512], ps[0:2, :], adab[:, ng * 512:(ng + 1) * 512], ALU.add)
            m = lambda i: mod[:, i * D:(i + 1) * D]
            k.stt(R[:, 0:D], m(1), 1.0, gg[:, 0:D], ALU.add, ALU.mult)
            k.copy(R[:, D:2 * D], m(0))
            k.copy(R[:, 2 * D:3 * D], m(2))
            k.stt(R[:, 3 * D:4 * D], m(4), 1.0, gg[:, D:2 * D], ALU.add, ALU.mult)
            k.copy(R[:, 4 * D:5 * D], m(3))
            k.copy(R[:, 5 * D:6 * D], m(5))
            k.dma(io[f'modR{l}'], R[:], q='pool')
            k.barrier()

    def bcast_rows(self, st, l, idxs, tag):
        k, nc, io = self.k, self.nc, self.io
        out = {}
        for r in range(2):
            for i in idxs:
                t_ = sb(st, nc, f'bc_{tag}_{r}_{i}', [128, D], F32)
                k.dma(t_[:], io[f'modR{l}'][r, i * D:(i + 1) * D].partition_broadcast(128))
                out[(r, i)] = t_
        return out

    def norm_tile(self, xt, A, S, hb, tmp, junk, ssq, eps=1e-6):
        k = self.k
        k.act(junk[:], xt[:], AF.Square, accum_out=ssq[:, 0:1])
        k.act(ssq[:, 1:2], ssq[:, 0:1], AF.Sqrt, bias=eps, scale=1.0 / D)
        k.op('dve', lambda E: E.reciprocal(ssq[:, 2:3], ssq[:, 1:2]), [ssq[:]], [ssq[:]])
        k.stt(tmp[:], xt[:], ssq[:, 2:3], A[:], ALU.mult, ALU.mult)
        k.tt(hb[:], tmp[:], S[:], ALU.add, e='pool')

    def stage_A(self, l):
        k, nc, io = self.k, self.nc, self.io
        src, cw, groups = self.plans[l]
        ncw = len(src)
        xsrc = io['x0'] if l == 0 else io['xs']
        with ExitStack() as st:
            W = sb(st, nc, 'W_sb', [128, 8, ncw], BF16)
            identb = sb(st, nc, 'identb', [128, 128], BF16)
            k.dma(identb[:], io['ident_b'])
            with ExitStack() as st2:
                csb = sb(st2, nc, 'csb', [128, ncw], F32)
                stg = [sb(st2, nc, f'wstg{i}', [128, 512], F32) for i in range(3)]
                k.dma(csb[:], io[f'cs{l}'][0, :].partition_broadcast(128))
                i = 0
                for c0 in range(0, ncw, 512):
                    c1 = min(ncw, c0 + 512)
                    for kc in range(8):
                        s_ = stg[i % 3]
                        i += 1
                        k.dma(s_[:, 0:c1 - c0], io[f'wx{l}'][kc * 128:(kc + 1) * 128, c0:c1])
                        k.tt(W[:, kc, c0:c1], s_[:, 0:c1 - c0], csb[:, c0:c1], ALU.mult, e=('dve' if i % 2 else 'pool'))
                k.barrier()
            bc = self.bcast_rows(st, l, (0, 1), 'A')
            hT = [sb(st, nc, f'hT{i}', [128, 8, 514], BF16) for i in range(3)]
            xt = [sb(st, nc, f'xt{i}', [128, D], F32) for i in range(2)]
            tmp = sb(st, nc, 'ntmp', [128, D], F32)
            junk = sb(st, nc, 'njunk', [128, D], BF16)
            hb = [sb(st, nc, f'hb{i}', [128, D], BF16) for i in range(2)]
            ssq = [sb(st, nc, f'ssq{i}', [128, 4], F32) for i in range(2)]
            ropec = sb(st, nc, 'ropec', [128, 512], F32)
            ropes = sb(st, nc, 'ropes', [128, 512], F32)
            osb = [sb(st, nc, f'osb{i}', [128, 512], F32) for i in range(4)]
            rsw = sb(st, nc, 'rsw', [128, 512], F32)
            rt1 = sb(st, nc, 'rt1', [128, 512], F32)
            glist = [(g * 512, 512, 0) for g in range(16)] + [(T, 256, 1)]
            ocnt = [0]

            def nexto():
                ocnt[0] += 1
                return osb[ocnt[0] % 4]

            def make_hT(gi):
                t0, n, seg = glist[gi]
                buf = hT[gi % 3]
                for ti in range(n // 128):
                    x_ = xt[ti % 2]
                    k.dma(x_[:], xsrc[t0 + ti * 128:t0 + (ti + 1) * 128, :])
                    self.norm_tile(x_, bc[(seg, 0)], bc[(seg, 1)], hb[ti % 2], tmp, junk, ssq[ti % 2])
                    ps = k.psum()
                    pb = ps[:].bitcast(BF16)
                    for kc in range(8):
                        k.tr(pb[:, kc * 128:(kc + 1) * 128], hb[ti % 2][:, kc * 128:(kc + 1) * 128], identb[:])
                    k.copy(buf[:, :, 1 + ti * 128:1 + (ti + 1) * 128], pb[:, 0:1024].rearrange("p (kc t) -> p kc t", kc=8),
                           e='act')
                if gi > 0 and glist[gi - 1][2] == seg:
                    k.copy(buf[:, :, 0:1], hT[(gi - 1) % 3][:, :, 512:513], e='pool')
                    k.copy(hT[(gi - 1) % 3][:, :, 513:514], buf[:, :, 1:2], e='pool')
                else:
                    k.memset(buf[:, :, 0:1], 0.0)
                    if gi > 0:
                        k.memset(hT[(gi - 1) % 3][:, :, 513:514], 0.0)
                if gi == len(glist) - 1:
                    k.memset(buf[:, :, 1 + n:2 + n], 0.0)

            def proj(gi):
                t0, n, seg = glist[gi]
                buf = hT[gi % 3]
                k.dma(ropec[:, 0:n], io['rope_c'][:, t0:t0 + n])
                k.dma(ropes[:, 0:n], io['rope_s'][:, t0:t0 + n])
                G = {g['name']: g for g in groups}

                def fm_acc(g, c0, m):
                    ps = k.psum()
                    nt_ = g['nterm']
                    tot = nt_ * 8
                    ii = 0
                    for j in range(nt_):
                        sh = (j - 1) if nt_ == 3 else 0
                        for kc in range(8):
                            cb = g['base'] + j * g['n'] + c0
                            k.mm(ps[0:m, 0:n], W[:, kc, cb:cb + m], buf[:, kc, 1 + sh:1 + sh + n], ii == 0, ii == tot - 1)
                            ii += 1
                    return ps

                for (ga, gb, dst) in (('attq', 'attqs', 'attq_d'), ('attk', 'attks', 'attk_d')):
                    for c0 in range(0, G[ga]['n'], 128):
                        p1 = fm_acc(G[ga], c0, 128)
                        p2 = fm_acc(G[gb], c0, 128)
                        o = nexto()
                        k.tt(rt1[:, 0:n], p1[:, 0:n], ropec[:, 0:n], ALU.mult)
                        k.copy(rsw[:, 0:n], p2[:, 0:n], e='act')
                        k.tt(rsw[:, 0:n], rsw[:, 0:n], ropes[:, 0:n], ALU.mult, e='pool')
                        k.tt(o[:, 0:n], rt1[:, 0:n], rsw[:, 0:n], ALU.add)
                        k.dma(io[dst][c0:c0 + 128, t0:t0 + n], o[:, 0:n], q='pool')
                for (ga, dst, fn) in (('rwx', 'rwx_d', None), ('mlqk', 'mlqk_d', AF.Silu), ('vdown', 'vdown_d', None)):
                    if ga not in G:
                        continue
                    for c0 in range(0, G[ga]['n'], 128):
                        m = min(128, G[ga]['n'] - c0)
                        p1 = fm_acc(G[ga], c0, m)
                        o = nexto()
                        if fn is None:
                            k.copy(o[0:m, 0:n], p1[0:m, 0:n], e='act')
                        else:
                            k.act(o[0:m, 0:n], p1[0:m, 0:n], fn)
                        k.dma(io[dst][c0:c0 + m, t0:t0 + n], o[0:m, 0:n], q='pool')
                for ti in range(n // 128):
                    for (ga, dst) in (('attv', 'attv_d'), ('rwrkv', 'rwrkv_d'), ('mlvog', 'mlvog_d')):
                        g = G[ga]
                        for c0 in range(0, g['n'], 512):
                            w = min(512, g['n'] - c0)
                            ps = k.psum()
                            nt_ = g['nterm']
                            tot = nt_ * 8
                            ii = 0
                            for j in range(nt_):
                                sh = (j - 1) if nt_ == 3 else 0
                                for kc in range(8):
                                    cb = g['base'] + j * g['n'] + c0
                                    a = 1 + ti * 128 + sh
                                    k.mm(ps[:, 0:w], buf[:, kc, a:a + 128], W[:, kc, cb:cb + w], ii == 0, ii == tot - 1)
                                    ii += 1
                            o = nexto()
                            k.copy(o[:, 0:w], ps[:, 0:w], e=('act' if (ti + c0) % 2 else 'dve'))
                            k.dma(io[dst][t0 + ti * 128:t0 + (ti + 1) * 128, c0:c0 + w], o[:, 0:w], q='pool')

            make_hT(0)
            for gi in range(len(glist)):
                if gi + 1 < len(glist):
                    make_hT(gi + 1)
                proj(gi)
            k.barrier()


def host_inputs(inp, b, consts, plans):
    m = {}
    m['x0'] = np.concatenate([inp['x'][b], inp['ctx'][b]], axis=0)
    m['cc'] = np.stack([inp['c'][b], inp['c_ctx']], axis=0)
    m.update(consts)
    for nm in ('norm1_g', 'norm2_g', 'ada_w', 'ada_b', 'attn_sink', 'mlstm_gate_b', 'mlstm_ln_w', 'rwkv_w0', 'rwkv_w_up', 'rwkv_a0', 'rwkv_a_up', 'rwkv_g_up',
               'rwkv_k_k', 'rwkv_k_a', 'rwkv_r_k', 'rwkv_ln_w', 'rwkv_ln_b', 'rwkv_v0', 'rwkv_v_up',
               'w_out', 'router_w', 'router_b', 'moe_w1', 'moe_b1', 'moe_w2', 'moe_b2', 'final_norm_g'):
        m[nm] = inp[nm]
    ones = np.ones((1,), np.float32)
    for l in range(DEPTH):
        src, cw, groups = plans[l]
        wext = inp['w_in'][l] if l == 0 else np.concatenate([inp['w_in'][l], inp['rwkv_v_down'][l - 1]], axis=1)
        m[f'wx{l}'] = np.ascontiguousarray(wext[:, src])
        cs = np.empty((1, len(src)), np.float32)
        which = np.array([c[0] for c in cw]); jj = np.array([c[1] for c in cw]); cc_ = np.array([c[2] for c in cw])
        cs[0, which == 0] = ones[0]
        cs[0, which == 1] = inp['rwkv_conv'][l][jj[which == 1], cc_[which == 1]]
        cs[0, which == 2] = inp['mlstm_conv'][l][jj[which == 2], cc_[which == 2]]
        m[f'cs{l}'] = cs
    return m


def att_consts():
    j = np.arange(128)[:, None]
    i = np.arange(128)[None, :]
    ge = (j >= i).astype(np.float32)
    le = (j <= i).astype(np.float32)
    extra = {'tri_le': le, 'tri_ge': ge}
    return {**extra, 'mask_ge': np.tile(ge, (1, 4)).astype(ml_dtypes.bfloat16), 'mask_le': np.tile(le, (1, 4)).astype(ml_dtypes.bfloat16)}


def stage_ATT(self, l):
    k, nc, io = self.k, self.nc, self.io
    need_ctx = l < DEPTH - 1
    with ExitStack() as st:
        kTb = sb(st, nc, 'kTb', [128, N], BF16)
        vb = sb(st, nc, 'vb', [128, NT, 2, 65], BF16)
        mge = sb(st, nc, 'mge', [128, 512], BF16)
        mle = sb(st, nc, 'mle', [128, 512], BF16)
        esink = sb(st, nc, 'esink', [128, 8], F32)
        stg = [sb(st, nc, f'astg{i}', [128, 512], F32) for i in range(2)]
        qf = [sb(st, nc, f'qf{i}', [128, 512], F32) for i in range(2)]
        qb_ = [sb(st, nc, f'qb{i}', [128, 512], BF16) for i in range(2)]
        pt = [sb(st, nc, f'pt{i}', [128, 512], BF16) for i in range(6)]
        osb = [sb(st, nc, f'aosb{i}', [128, 512], F32) for i in range(2)]
        den = sb(st, nc, 'aden', [128, 8], F32)
        k.dma(mge[:], io['mask_ge'])
        k.dma(mle[:], io['mask_le'])
        k.dma(esink[:], io['attn_sink'][l, :].partition_broadcast(128))
        k.act(esink[:], esink[:], AF.Exp)
        k.memset(vb[:, :, :, 64:65], 1.0)
        for c in range(0, N, 512):
            w = min(512, N - c)
            s_ = stg[(c // 512) % 2]
            k.dma(s_[:, 0:w], io['attk_d'][:, c:c + w])
            k.copy(kTb[:, c:c + w], s_[:, 0:w], e='pool')
        for ti in range(NT):
            s_ = stg[ti % 2]
            k.dma(s_[:, 0:128], io['attv_d'][ti * 128:(ti + 1) * 128, :])
            k.copy(vb[:, ti, :, 0:64], s_[:, 0:128].rearrange("p (g d) -> p g d", g=2), e='dve')
        qblocks = list(range(64)) + ([64, 65] if need_ctx else [])
        pi = 0
        for n_, qb in enumerate(qblocks):
            if qb < 64:
                keys = [(kb, m) for kb, m in ((qb - 1, mge), (qb, None), (qb + 1, mle)) if 0 <= kb < 64] + [(64, None), (65, None)]
            else:
                keys = [(64, None), (65, None)]
            q_f, q_b, o_ = qf[n_ % 2], qb_[n_ % 2], osb[n_ % 2]
            for g in range(2):
                k.dma(q_f[g * 64:(g + 1) * 64, :].rearrange("p (h t) -> p h t", h=4),
                      io['attq_d'][g * 256:(g + 1) * 256, qb * 128:(qb + 1) * 128].rearrange("(h d) t -> d h t", d=64))
            k.copy(q_b[:], q_f[:], e='pool')
            for g in range(2):
                ptl = []
                for kb, m in keys:
                    ps = k.psum()
                    k.mm(ps[:, :], kTb[g * 64:(g + 1) * 64, kb * 128:(kb + 1) * 128], q_b[g * 64:(g + 1) * 64, :], True, True)
                    p_ = pt[pi % 6]
                    pi += 1
                    k.act(p_[:], ps[:, :], AF.Exp, scale=0.125)
                    if m is not None:
                        k.tt(p_[:], p_[:], m[:], ALU.mult, e='pool')
                    ptl.append((p_, kb))
                po = k.psum()
                for h in range(4):
                    for i_, (p_, kb) in enumerate(ptl):
                        k.mm(po[:, h * 65:(h + 1) * 65], p_[:, h * 128:(h + 1) * 128], vb[:, kb, g, :], i_ == 0, i_ == len(ptl) - 1)
                pv = po[:, 0:260].rearrange("p (h e) -> p h e", h=4)
                k.tt(den[:, g * 4:(g + 1) * 4], pv[:, :, 64], esink[:, g * 4:(g + 1) * 4], ALU.add)
                k.op('dve', lambda E: E.reciprocal(den[:, g * 4:(g + 1) * 4], den[:, g * 4:(g + 1) * 4]), [den[:]], [den[:]])
                for h in range(4):
                    hh = g * 4 + h
                    k.ts(o_[:, hh * 64:(hh + 1) * 64], po[:, h * 65:h * 65 + 64], den[:, hh:hh + 1], None, ALU.mult)
            k.dma(io['mix_d'][qb * 128:(qb + 1) * 128, 0:512], o_[:], q='pool')
        k.barrier()


Prog.stage_ATT = stage_ATT


def stage_ML(self, l):
    k, nc, io = self.k, self.nc, self.io
    need_ctx = l < DEPTH - 1
    with ExitStack() as st:
        ysum = sb(st, nc, 'ysum', [128, NT, 256], F32)
        tri = {0: sb(st, nc, 'tri_le', [128, 128], F32), 1: sb(st, nc, 'tri_ge', [128, 128], F32)}
        ones = sb(st, nc, 'ones_f', [128, 128], F32)
        identb = sb(st, nc, 'identb', [128, 128], BF16)
        gb = sb(st, nc, 'gb', [128, 16], F32)
        lnw = sb(st, nc, 'lnw', [128, 256], F32)
        C = sb(st, nc, 'Cst', [128, 2, 65], F32)
        Cb = sb(st, nc, 'Cstb', [128, 2, 65], BF16)
        qkf = [sb(st, nc, f'qkf{i}', [128, 4, 128], F32) for i in range(2)]
        qkb = [sb(st, nc, f'qkb{i}', [128, 4, 128], BF16) for i in range(2)]
        vf = [sb(st, nc, f'vf{i}', [128, 528], F32) for i in range(2)]
        va = [sb(st, nc, f'va{i}', [128, 4, 65], BF16) for i in range(2)]
        gt = [sb(st, nc, f'gt{i}', [128, 48], F32) for i in range(2)]
        kpp = [sb(st, nc, f'kpp{i}', [128, 128], BF16) for i in range(2)]
        pm = [sb(st, nc, f'pm{i}', [128, 128], BF16) for i in range(2)]
        sc = [sb(st, nc, f'sc{i}', [128, 8], F32) for i in range(2)]
        k.dma(tri[0][:], io['tri_le']); k.dma(tri[1][:], io['tri_ge']); k.dma(identb[:], io['ident_b'])
        k.memset(ones[:], 1.0)
        k.dma(gb[:], io['mlstm_gate_b'][l, :].partition_broadcast(128))
        k.dma(lnw[:], io['mlstm_ln_w'][l, :].partition_broadcast(128))
        it = 0
        for d in range(2):
            order = [64, 65] + list(range(64)) if d == 0 else [65, 64] + list(range(63, -1, -1))
            k.memset(C[:], 0.0)
            k.memset(Cb[:], 0.0)
            for ti in order:
                i2 = it % 2
                it += 1
                t0 = ti * 128
                qf_, qb_, vf_, va_, g_, s_ = qkf[i2], qkb[i2], vf[i2], va[i2], gt[i2], sc[i2]
                k.dma(qf_[:], io['mlqk_d'][:, t0:t0 + 128].rearrange("(s p) t -> p s t", p=128))
                k.dma(vf_[:], io['mlvog_d'][t0:t0 + 128, :])
                k.copy(qb_[:], qf_[:], e='pool')
                k.copy(va_[:, :, 0:64], vf_[:, 0:256].rearrange("p (h e) -> p h e", h=4), e='pool')
                k.memset(va_[:, :, 64:65], 1.0)
                k.tt(g_[:, 0:8], vf_[:, 512 + d * 8:512 + d * 8 + 8], gb[:, d * 8:d * 8 + 8], ALU.add)
                k.act(g_[:, 8:12], g_[:, 4:8], AF.Exp, scale=-1.0)
                k.act(g_[:, 8:12], g_[:, 8:12], AF.Ln, bias=1.0)
                k.ts(g_[:, 4:8], g_[:, 8:12], -1.0, None, ALU.mult)
                ps = k.psum()
                k.mm(ps[:, 0:4], tri[d][:], g_[:, 4:8], True, True)
                k.mm(ps[:, 4:8], ones[:], g_[:, 4:8], True, True)
                k.copy(g_[:, 12:20], ps[:, 0:8])
                k.tt(g_[:, 20:24], g_[:, 0:4], g_[:, 12:16], ALU.subtract)
                k.act(g_[:, 24:28], g_[:, 20:24], AF.Exp)
                k.ts(g_[:, 24:28], g_[:, 24:28], 0.125, None, ALU.mult)
                k.tt(g_[:, 28:32], g_[:, 20:24], g_[:, 16:20], ALU.add)
                k.act(g_[:, 28:32], g_[:, 28:32], AF.Exp)
                k.ts(g_[:, 28:32], g_[:, 28:32], 0.125, None, ALU.mult)
                k.act(g_[:, 32:36], g_[:, 12:16], AF.Exp)
                k.act(g_[:, 36:40], g_[:, 16:20], AF.Exp)
                for pr in range(2):
                    pT = k.psum()
                    pTb = pT[:].bitcast(BF16)
                    k.tr(pTb[:, 0:128], qb_[:, 2 + pr, :], identb[:])
                    kp = kpp[pr]
                    for hh in range(2):
                        h = pr * 2 + hh
                        k.ts(kp[:, hh * 64:(hh + 1) * 64], pTb[:, hh * 64:(hh + 1) * 64], g_[:, 28 + h:29 + h], None, ALU.mult,
                             e=('dve' if hh else 'pool') if False else 'dve')
                    for hh in range(2):
                        h = pr * 2 + hh
                        p0 = hh * 64
                        pS = k.psum()
                        k.mm(pS[:, 0:128], qb_[p0:p0 + 64, 2 + pr, :], qb_[p0:p0 + 64, pr, :], True, True)
                        pm_ = pm[hh]
                        k.stt(pm_[:], pS[:, 0:128], g_[:, 24 + h:25 + h], tri[d][:], ALU.mult, ALU.mult)
                        pO = k.psum()
                        k.mm(pO[:, 0:65], pm_[:], va_[:, h, :], True, False)
                        k.mm(pO[:, 0:65], qb_[p0:p0 + 64, pr, :], Cb[p0:p0 + 64, pr, :], False, True)
                        k.ts(s_[:, 0:1], pO[:, 64:65], g_[:, 32 + h:33 + h], None, ALU.mult)
                        k.act(s_[:, 0:1], s_[:, 0:1], AF.Abs)
                        k.ts(s_[:, 0:1], s_[:, 0:1], 1.0, None, ALU.max)
                        k.op('dve', lambda E: E.reciprocal(s_[:, 1:2], s_[:, 0:1]), [s_[:]], [s_[:]])
                        k.tt(s_[:, 2:3], s_[:, 1:2], g_[:, 32 + h:33 + h], ALU.mult)
                        yv = ysum[:, ti, h * 64:(h + 1) * 64]
                        if d == 0:
                            k.ts(yv, pO[:, 0:64], s_[:, 2:3], None, ALU.mult)
                        else:
                            k.stt(yv, pO[:, 0:64], s_[:, 2:3], yv, ALU.mult, ALU.add)
                    pC = k.psum()
                    k.mm(pC[:, 0:130], kp[:], va_[:, pr * 2:pr * 2 + 2, :], True, True)
                    for hh in range(2):
                        h = pr * 2 + hh
                        p0 = hh * 64
                        k.stt(C[p0:p0 + 64, pr, :], C[p0:p0 + 64, pr, :], g_[p0:p0 + 64, 36 + h:37 + h],
                              pC[p0:p0 + 64, hh * 65:(hh + 1) * 65], ALU.mult, ALU.add)
                    k.copy(Cb[:, pr, :], C[:, pr, :], e='pool')
        tiles = list(range(64)) + ([64, 65] if need_ctx else [])
        for n_, ti in enumerate(tiles):
            i2 = n_ % 2
            vf_, g_ = vf[i2], gt[i2]
            o_ = qkf[i2][:, 0:2, :]
            yc = qkf[i2][:, 2:4, :]
            k.dma(vf_[:, 0:256], io['mlvog_d'][ti * 128:(ti + 1) * 128, 256:512])
            k.act(vf_[:, 0:256], vf_[:, 0:256], AF.Sigmoid)
            k.op('dve', lambda E: E.reduce_sum(g_[:, 0:4], ysum[:, ti, :].rearrange("p (h e) -> p h e", h=4), AX.X),
                 [ysum[:]], [g_[:]])
            k.ts(g_[:, 0:4], g_[:, 0:4], -1.0 / 64, None, ALU.mult)
            for h in range(4):
                k.ts(yc[:, h // 2, (h % 2) * 64:(h % 2) * 64 + 64], ysum[:, ti, h * 64:(h + 1) * 64], g_[:, h:h + 1], None, ALU.add)
                k.act(o_[:, h // 2, (h % 2) * 64:(h % 2) * 64 + 64], yc[:, h // 2, (h % 2) * 64:(h % 2) * 64 + 64], AF.Square,
                      accum_out=g_[:, 4 + h:5 + h])
            k.act(g_[:, 8:12], g_[:, 4:8], AF.Sqrt, bias=1e-5, scale=1.0 / 64)
            k.op('dve', lambda E: E.reciprocal(g_[:, 12:16], g_[:, 8:12]), [g_[:]], [g_[:]])
            for h in range(4):
                k.stt(o_[:, h // 2, (h % 2) * 64:(h % 2) * 64 + 64], yc[:, h // 2, (h % 2) * 64:(h % 2) * 64 + 64], g_[:, 12 + h:13 + h],
                      lnw[:, h * 64:(h + 1) * 64], ALU.mult, ALU.mult)
            k.tt(vf_[:, 256:512], o_.rearrange("p a b -> p (a b)"), vf_[:, 0:256], ALU.mult, e='pool')
            k.dma(io['mix_d'][ti * 128:(ti + 1) * 128, 768:1024], vf_[:, 256:512], q='pool')
        k.barrier()


Prog.stage_ML = stage_ML


def stage_RW(self, l):
    k, nc, io = self.k, self.nc, self.io
    need_ctx = l < DEPTH - 1
    with ExitStack() as st:
        identf = sb(st, nc, 'identf', [128, 128], F32)
        k.dma(identf[:], io['ident_f'])
        bc = {}
        for nm, src in (('k_k', io['rwkv_k_k'][l, :]), ('k_a', io['rwkv_k_a'][l, :]), ('r_k', io['rwkv_r_k'][l].rearrange("h e -> (h e)")),
                        ('a0_0', io['rwkv_a0'][l, 0, :]), ('a0_1', io['rwkv_a0'][l, 1, :])) + \
                (() if l == 0 else (('v0', io['rwkv_v0'][l - 1, :]),)):
            bc[nm] = sb(st, nc, 'rbc_' + nm, [128, 256], F32)
            k.dma(bc[nm][:], src.partition_broadcast(128))
        up = sb(st, nc, 'rw_up', [128, 2, 256], F32)
        gup = sb(st, nc, 'rw_gup', [64, 256], F32)
        vup = sb(st, nc, 'rw_vup', [32, 256], F32)
        w0c = sb(st, nc, 'rw_w0c', [128, 4], F32)
        for d in range(2):
            k.dma(up[d * 32:(d + 1) * 32, 0, :], io['rwkv_w_up'][l, d])
            k.dma(up[d * 32:(d + 1) * 32, 1, :], io['rwkv_a_up'][l, d])
            k.dma(w0c[:, d * 2:d * 2 + 2], io['rwkv_w0'][l, d, :].rearrange("(c p) -> p c", p=128), allow_slow_non_contiguous=True)
        k.dma(gup[:], io['rwkv_g_up'][l])
        if l > 0:
            k.dma(vup[:], io['rwkv_v_up'][l - 1])
        NB = 2
        xr = [sb(st, nc, f'rxr{i}', [128, 768], F32) for i in range(NB)]
        xf = [sb(st, nc, f'rxf{i}', [64, 128], F32) for i in range(NB)]
        xaf = [sb(st, nc, f'rxaf{i}', [64, 128], F32) for i in range(NB)]
        gf = [sb(st, nc, f'rgf{i}', [64, 128], F32) for i in range(NB)]
        lvf = [sb(st, nc, f'rlv{i}', [32, 128], F32) for i in range(NB)]
        vfst = [sb(st, nc, f'rvf{i}', [128, 256], F32) for i in range(NB)]
        kk = [sb(st, nc, f'rkk{i}', [128, 256], F32) for i in range(NB)]
        t1 = [sb(st, nc, f'rt1{i}', [128, 256], F32) for i in range(4)]
        t2 = [sb(st, nc, f'rt2{i}', [128, 256], F32) for i in range(4)]
        sm = [sb(st, nc, f'rsm{i}', [128, 16], F32) for i in range(NB)]
        fo = [sb(st, nc, f'rfo{i}', [128, 128], F32) for i in range(4)]
        tb = [sb(st, nc, f'rtb{i}', [128, 256], BF16) for i in range(6)]
        cnt = [0]

        def T1():
            cnt[0] += 1
            return t1[cnt[0] % 4]

        def T2():
            cnt[0] += 1
            return t2[cnt[0] % 4]

        def FO():
            cnt[0] += 1
            return fo[cnt[0] % 4]

        def TB():
            cnt[0] += 1
            return tb[cnt[0] % 6]

        for ti in range(NT):
            i2 = ti % NB
            t0 = ti * 128
            x_, xf_, gf_, kk_, sm_ = xr[i2], xf[i2], gf[i2], kk[i2], sm[i2]
            k.dma(x_[:], io['rwrkv_d'][t0:t0 + 128, :])
            k.dma(xf_[:], io['rwx_d'][0:64, t0:t0 + 128])
            k.dma(xaf[i2][:], io['rwx_d'][64:128, t0:t0 + 128])
            k.dma(gf_[:], io['rwx_d'][128:192, t0:t0 + 128])
            r_, k_, v_ = x_[:, 0:256], x_[:, 256:512], x_[:, 512:768]
            if l == 0:
                k.dma(io['vfirst_d'][t0:t0 + 128, :], v_, q='pool')
            else:
                k.dma(lvf[i2][:], io['vdown_d'][:, t0:t0 + 128])
                k.dma(vfst[i2][:], io['vfirst_d'][t0:t0 + 128, :])
                ps = k.psum()
                k.mm(ps[:, 0:256], lvf[i2][:], vup[:], True, True)
                a = T1()
                k.tt(a[:], ps[:, 0:256], bc['v0'][:], ALU.add)
                k.act(a[:], a[:], AF.Sigmoid)
                b_ = T2()
                k.tt(b_[:], vfst[i2][:], v_, ALU.subtract)
                k.tt(b_[:], b_[:], a[:], ALU.mult, e='pool')
                k.tt(v_, v_, b_[:], ALU.add)
            vb_ = TB()
            k.copy(vb_[:], v_, e='pool')
            k.dma(io['rv_d'][t0:t0 + 128, :], vb_[:], q='pool')
            k.tt(kk_[:], k_, bc['k_k'][:], ALU.mult)
            a = T1()
            for h in range(4):
                k.act(a[:, h * 64:(h + 1) * 64], kk_[:, h * 64:(h + 1) * 64], AF.Square, accum_out=sm_[:, h:h + 1])
            k.act(sm_[:, 4:8], sm_[:, 0:4], AF.Sqrt)
            k.ts(sm_[:, 4:8], sm_[:, 4:8], 1e-12, None, ALU.max)
            k.op('dve', lambda E: E.reciprocal(sm_[:, 8:12], sm_[:, 4:8]), [sm_[:]], [sm_[:]])
            for h in range(4):
                k.ts(kk_[:, h * 64:(h + 1) * 64], kk_[:, h * 64:(h + 1) * 64], sm_[:, 8 + h:9 + h], None, ALU.mult)
            nk_ = TB()
            k.ts(nk_[:], kk_[:], -1.0, None, ALU.mult, e='pool')
            k.dma(io['rnk_d'][t0:t0 + 128, :], nk_[:], q='pool')
            for src_, dst in ((r_, 'rfm_d'),):
                for c in range(2):
                    ps = k.psum()
                    k.tr(ps[:, 0:128], src_[:, c * 128:(c + 1) * 128], identf[:])
                    f_ = FO()
                    k.copy(f_[:], ps[:, 0:128], e='act')
                    k.dma(io[dst][c * 128:(c + 1) * 128, t0:t0 + 128], f_[:], q='pool')
            k.act(gf_[:], gf_[:], AF.Sigmoid)
            ps = k.psum()
            k.mm(ps[:, 0:256], gf_[:], gup[:], True, True)
            a = T1()
            k.copy(a[:], ps[:, 0:256], e='act')
            k.dma(io['rgate_d'][t0:t0 + 128, :], a[:], q='pool')
            k.act(xf_[0:64, :], xf_[0:64, :], AF.Tanh)
            for d in range(2):
                for c in range(2):
                    ps = k.psum()
                    k.mm(ps[:, 0:128], up[d * 32:(d + 1) * 32, 0, c * 128:(c + 1) * 128], xf_[d * 32:(d + 1) * 32, :], True, True)
                    f_ = FO()
                    k.act(f_[:], ps[:, 0:128], AF.Sigmoid, bias=w0c[:, d * 2 + c:d * 2 + c + 1])
                    k.act(f_[:], f_[:], AF.Exp, scale=-0.6065306597126334)
                    k.dma(io['rw_d'][d, c * 128:(c + 1) * 128, t0:t0 + 128], f_[:], q='pool')
                ps = k.psum()
                k.mm(ps[:, 0:256], xaf[i2][d * 32:(d + 1) * 32, :], up[d * 32:(d + 1) * 32, 1, :], True, True)
                a = T1()
                k.tt(a[:], ps[:, 0:256], bc[f'a0_{d}'][:], ALU.add)
                k.act(a[:], a[:], AF.Sigmoid)
                b_ = TB()
                k.tt(b_[:], kk_[:], a[:], ALU.mult, e='pool')
                k.dma(io['rkka_d'][d, t0:t0 + 128, :], b_[:], q='pool')
                c_ = T2()
                k.stt(c_[:], a[:], -1.0, bc['k_a'][:], ALU.add, ALU.mult)
                k.stt(c_[:], c_[:], 1.0, k_, ALU.add, ALU.mult)
                cb_ = TB()
                k.copy(cb_[:], c_[:], e='pool')
                k.dma(io['rkd_d'][d, t0:t0 + 128, :], cb_[:], q='pool')
                e_ = T1()
                k.tt(e_[:], c_[:], bc['r_k'][:], ALU.mult, e='pool')
                k.tt(e_[:], e_[:], r_, ALU.mult)
                k.op('dve', lambda E: E.reduce_sum(sm_[:, 12:16], e_[:].rearrange("p (h e) -> p h e", h=4), AX.X), [e_[:]], [sm_[:]])
                f2 = T2()
                for h in range(4):
                    k.ts(f2[:, h * 64:(h + 1) * 64], v_[:, h * 64:(h + 1) * 64], sm_[:, 12 + h:13 + h], None, ALU.mult)
                k.dma(io['rbonus_d'][d, t0:t0 + 128, :], f2[:], q='pool')
        k.barrier()
    SB = 8
    with ExitStack() as st:
        sel = sb(st, nc, 'rsel', [128, 2], F32)
        k.memset(sel[:], 0.0)
        k.memset(sel[0:64, 0:1], 1.0)
        k.memset(sel[64:128, 1:2], 1.0)
        NBUF = 3
        mkf = lambda nm, shp, dt=F32: [[sb(st, nc, f'{nm}{g}_{i}', shp, dt) for i in range(NBUF)] for g in range(2)]
        Hist = mkf('rH', [128, SB, 2, 64])
        Sb = [[sb(st, nc, f'rSb{g}_{i}', [128, 2, 64], BF16) for i in range(2)] for g in range(2)]
        S0 = [sb(st, nc, f'rS0{g}', [128, 2, 64], F32) for g in range(2)]
        Ml = [[sb(st, nc, f'rMl{g}_{i}', [128, 2, 128], BF16) for i in range(3)] for g in range(2)]
        Rr = [mkf(f'rR{d}', [128, SB]) for d in range(2)]
        Wt = [mkf(f'Wt{d}', [128, SB]) for d in range(2)]
        NK = [mkf(f'NK{d}', [2, SB, 128], BF16) for d in range(2)]
        KA = [mkf(f'KA{d}', [2, SB, 128], BF16) for d in range(2)]
        KD = [mkf(f'KD{d}', [2, SB, 128], BF16) for d in range(2)]
        VV = [mkf(f'VV{d}', [2, SB, 64], BF16) for d in range(2)]
        YY = mkf('YY', [2, SB, 2, 64])
        ytmp = [[sb(st, nc, f'rytmp{g}_{i}', [128, SB, 2, 64], F32) for i in range(2)] for g in range(2)]
        for g in range(2):
            k.memset(S0[g][:], 0.0)
            k.memset(Sb[g][0][:], 0.0)
            for d in range(2):
                for bi_ in range(NBUF):
                    for tl in (NK, KA, KD):
                        k.memset(tl[d][g][bi_][:], 0.0)
        import os as _os
        nblk = int(_os.environ.get('RW_NBLK', N // SB))

        def tokrange(blk, d):
            s0 = blk * SB
            if s0 < LC:
                return T + s0 if d == 0 else T + LC - s0 - SB
            return s0 - LC if d == 0 else T - (s0 - LC) - SB

        def load_block(blk, g):
            bi = blk % NBUF
            for d in range(2):
                tk = tokrange(blk, d)
                k.dma(Rr[d][g][bi][:], io['rfm_d'][g * 128:(g + 1) * 128, tk:tk + SB])
                k.dma(Wt[d][g][bi][:], io['rw_d'][d, g * 128:(g + 1) * 128, tk:tk + SB])
                for hh in range(2):
                    c0 = (g * 2 + hh) * 64
                    k.dma(NK[d][g][bi][hh:hh + 1, :, hh * 64:(hh + 1) * 64], io['rnk_d'][tk:tk + SB, c0:c0 + 64])
                    k.dma(KA[d][g][bi][hh:hh + 1, :, hh * 64:(hh + 1) * 64], io['rkka_d'][d, tk:tk + SB, c0:c0 + 64])
                    k.dma(KD[d][g][bi][hh:hh + 1, :, hh * 64:(hh + 1) * 64], io['rkd_d'][d, tk:tk + SB, c0:c0 + 64])
                    k.dma(VV[d][g][bi][hh:hh + 1, :, :], io['rv_d'][tk:tk + SB, c0:c0 + 64])

        nsteps = nblk * SB

        def cols(s):
            sl = s % SB
            return (sl, SB - 1 - sl)

        def build_M(s, g):
            bi = (s // SB) % NBUF
            col = cols(s)
            pM = k.psum()
            for d in range(2):
                k.mm(pM[:, d * 128:(d + 1) * 128], NK[d][g][bi][0:2, col[d], :], KA[d][g][bi][0:2, col[d], :], True, True)
            k.copy(Ml[g][s % 3][:].rearrange("p d c -> p (d c)"), pM[:, 0:256], e='act')

        def prev_state(s, g, d):
            if s == 0:
                return S0[g][:, d, :]
            ps_ = s - 1
            return Hist[g][(ps_ // SB) % NBUF][:, cols(ps_)[d], d, :]

        def block_mul(blk, g):
            bi = blk % NBUF
            yt_ = ytmp[g][blk % 2]
            for d in range(2):
                k.tt(yt_[:, :, d, :], Hist[g][bi][:, :, d, :],
                     Rr[d][g][bi][:].unsqueeze(2).to_broadcast([128, SB, 64]), ALU.mult, e='pool')

        def block_out(blk, g):
            bi = blk % NBUF
            yt_ = ytmp[g][blk % 2]
            yv = yt_[:].rearrange("p s d v -> p (s d v)")
            yy = YY[g][bi]
            for hf in range(2):
                pY = k.psum()
                k.mm(pY[0:2, :], sel[:], yv[:, hf * 512:(hf + 1) * 512], True, True)
                k.copy(yy[:].rearrange("p s d v -> p (s d v)")[:, hf * 512:(hf + 1) * 512], pY[0:2, :], e='act')
            if not _os.environ.get('RW_NOSTORE'):
                for d in range(2):
                    tk = tokrange(blk, d)
                    for hh in range(2):
                        c0 = (g * 2 + hh) * 64
                        k.dma(io['ryscan_d'][d, tk:tk + SB, c0:c0 + 64], yy[hh:hh + 1, :, d, :], q='pool')

        for g in range(2):
            load_block(0, g)
            if nblk > 1:
                load_block(1, g)
        for g in range(2):
            build_M(0, g)
        for s in range(nsteps):
            blk, sl = s // SB, s % SB
            bi = blk % NBUF
            col = cols(s)
            if sl == 0 and blk + 2 < nblk and not _os.environ.get('RW_NOLOAD'):
                for g in range(2):
                    load_block(blk + 2, g)
            if s + 1 < nsteps:
                for g in range(2):
                    build_M(s + 1, g)
            pS = []
            for g in range(2):
                p_ = k.psum()
                so = Sb[g][s % 2]
                for d in range(2):
                    k.mm(p_[:, d * 64:(d + 1) * 64], Ml[g][s % 3][:, d, :], so[:, d, :], True, False)
                    k.mm(p_[:, d * 64:(d + 1) * 64], KD[d][g][bi][0:2, col[d], :], VV[d][g][bi][0:2, col[d], :], False, True)
                pS.append(p_)
            for g in range(2):
                sn = Sb[g][(s + 1) % 2]
                for d in range(2):
                    k.stt(sn[:, d, :], prev_state(s, g, d), Wt[d][g][bi][:, col[d]:col[d] + 1], pS[g][:, d * 64:(d + 1) * 64],
                          ALU.mult, ALU.add)
            for g in range(2):
                for d in range(2):
                    k.stt(Hist[g][bi][:, col[d], d, :], prev_state(s, g, d), Wt[d][g][bi][:, col[d]:col[d] + 1],
                          pS[g][:, d * 64:(d + 1) * 64], ALU.mult, ALU.add)
            if sl == SB - 1:
                for g in range(2):
                    block_mul(blk, g)
                if blk > 0:
                    for g in range(2):
                        block_out(blk - 1, g)
        for g in range(2):
            block_out(nblk - 1, g)
        k.barrier()
    with ExitStack() as st:
        bcw = sb(st, nc, 'rlnw', [128, 256], F32)
        bcb = sb(st, nc, 'rlnb', [128, 256], F32)
        k.dma(bcw[:], io['rwkv_ln_w'][l, :].partition_broadcast(128))
        k.dma(bcb[:], io['rwkv_ln_b'][l, :].partition_broadcast(128))
        yt = [sb(st, nc, f'ryt{i}', [128, 2, 256], F32) for i in range(2)]
        bt = [sb(st, nc, f'rbt{i}', [128, 2, 256], F32) for i in range(2)]
        gtt = [sb(st, nc, f'rgtt{i}', [128, 256], F32) for i in range(2)]
        acc = [sb(st, nc, f'racc{i}', [128, 256], F32) for i in range(2)]
        jk = sb(st, nc, 'rjk', [128, 64], F32)
        sm = [sb(st, nc, f'rsm2{i}', [128, 32], F32) for i in range(2)]
        tiles = list(range(64)) + ([64, 65] if need_ctx else [])
        for n_, ti in enumerate(tiles):
            i2 = n_ % 2
            t0 = ti * 128
            y_, b_, g_, a_, s_ = yt[i2], bt[i2], gtt[i2], acc[i2], sm[i2]
            for d in range(2):
                k.dma(y_[:, d, :], io['ryscan_d'][d, t0:t0 + 128, :])
                k.dma(b_[:, d, :], io['rbonus_d'][d, t0:t0 + 128, :])
            k.dma(g_[:], io['rgate_d'][t0:t0 + 128, :])
            k.op('dve', lambda E: E.reduce_sum(s_[:, 0:8], y_[:].rearrange("p d (h e) -> p (d h) e", h=4), AX.X), [y_[:]], [s_[:]])
            k.ts(s_[:, 0:8], s_[:, 0:8], -1.0 / 64, None, ALU.mult)
            for j in range(8):
                d, h = j // 4, j % 4
                k.ts(y_[:, d, h * 64:(h + 1) * 64], y_[:, d, h * 64:(h + 1) * 64], s_[:, j:j + 1], None, ALU.add)
                k.act(jk[:], y_[:, d, h * 64:(h + 1) * 64], AF.Square, accum_out=s_[:, 8 + j:9 + j])
            k.act(s_[:, 16:24], s_[:, 8:16], AF.Sqrt, bias=64e-5, scale=1.0 / 64)
            k.op('dve', lambda E: E.reciprocal(s_[:, 24:32], s_[:, 16:24]), [s_[:]], [s_[:]])
            for j in range(8):
                d, h = j // 4, j % 4
                k.stt(y_[:, d, h * 64:(h + 1) * 64], y_[:, d, h * 64:(h + 1) * 64], s_[:, 24 + j:25 + j], bcw[:, h * 64:(h + 1) * 64],
                      ALU.mult, ALU.mult)
            k.tt(a_[:], y_[:, 0, :], y_[:, 1, :], ALU.add)
            k.tt(b_[:, 0, :], b_[:, 0, :], b_[:, 1, :], ALU.add, e='pool')
            k.stt(a_[:], bcb[:], 2.0, a_[:], ALU.mult, ALU.add)
            k.tt(a_[:], a_[:], b_[:, 0, :], ALU.add)
            k.tt(a_[:], a_[:], g_[:], ALU.mult, e='pool')
            k.dma(io['mix_d'][t0:t0 + 128, 512:768], a_[:], q='pool')
        k.barrier()


Prog.stage_RW = stage_RW


def build_full():
    P = Prog()
    P.declare()
    out = P.nc.dram_tensor('out', [T, D], F32, kind="ExternalOutput").ap()
    for l in range(DEPTH):
        P.stage_M(l)
        P.stage_A(l)
        P.stage_ATT(l)
        P.stage_ML(l)
        P.stage_RW(l)
        P.stage_C(l, out_ap=out)
    return P


def kernel(**inputs):
    inp = {k_: np.asarray(v) for k_, v in inputs.items()}
    P = build_full()
    consts = host_consts()
    in_maps = []
    for b in range(4):
        m = host_inputs(inp, b, consts, P.plans)
        in_maps.append({k_: np.ascontiguousarray(v) for k_, v in m.items() if k_ in P.io})
    res = run_bass_kernel_spmd(P.nc, in_maps, core_ids=list(range(4)))
    return np.stack([r['out'] for r in res.results], axis=0).astype(np.float32)


def stage_C(self, l, out_ap=None):
    k, nc, io = self.k, self.nc, self.io
    last = l == DEPTH - 1
    xsrc = io['x0'] if l == 0 else io['xs']
    groups = [(g * 1024, 1024, 0) for g in range(8)] + ([] if last else [(T, 256, 1)])
    import os as _os
    if 'C_NGROUPS' in _os.environ:
        groups = groups[:int(_os.environ['C_NGROUPS'])]
    with ExitStack() as st:
        identb = sb(st, nc, 'identb', [128, 128], BF16)
        identf = sb(st, nc, 'identf', [128, 128], F32)
        k.dma(identb[:], io['ident_b']); k.dma(identf[:], io['ident_f'])
        routw = sb(st, nc, 'routw', [128, 8, NE], F32)
        routb = sb(st, nc, 'routb', [128, NE], F32)
        b2 = sb(st, nc, 'b2', [NE, D], F32)
        b1c = [sb(st, nc, f'b1c{i}', [128, 16], F32) for i in range(2)]
        wstg = [sb(st, nc, f'cwstg{i}', [128, 1024], F32) for i in range(2)]
        k.dma(routw[:], io['router_w'][l].rearrange("(kc p) e -> p kc e", p=128))
        k.dma(routb[:], io['router_b'][l, :].partition_broadcast(128))
        k.dma(b2[:], io['moe_b2'][l])
        fng = None
        if last:
            fng = sb(st, nc, 'fng', [128, D], F32)
            k.dma(fng[:], io['final_norm_g'].partition_broadcast(128))
        bct = {i: sb(st, nc, f'cbc{i}', [128, D], F32) for i in (2, 3, 4, 5)}
        yacc = sb(st, nc, 'yacc', [128, 8, D], F32)
        h2T = sb(st, nc, 'h2T', [128, 8, 1024], BF16)
        gates = sb(st, nc, 'gates', [128, 8, NE], F32)
        W1s = [sb(st, nc, f'W1b{i}', [128, 8, 2048], BF16) for i in range(2)]
        W2s = [sb(st, nc, f'W2b{i}', [128, 8, D], BF16) for i in range(2)]
        actT = sb(st, nc, 'actT', [128, 8, 512], BF16)
        xt = [sb(st, nc, f'cxt{i}', [128, D], F32) for i in range(1)]
        mixT = sb(st, nc, 'cmixT', [128, 8, 128], BF16)
        tmp = sb(st, nc, 'ctmp', [128, D], F32)
        mixf = tmp
        hb = sb(st, nc, 'chb', [128, D], BF16)
        junk = hb
        mixb = hb
        h2fT = actT[:].rearrange("p a b -> p (a b)").bitcast(F32)[:, 0:1024].rearrange("p (kc t) -> p kc t", kc=8)
        ssq = sb(st, nc, 'cssq', [128, 4], F32)
        rt = [sb(st, nc, f'crt{i}', [128, 96], F32) for i in range(2)]
        gT = sb(st, nc, 'cgT', [NE, 128], F32)
        ev = [sb(st, nc, f'cev{i}', [128, 512], F32) for i in range(3)]
        ecnt = [0]
        evc = [0]

        def EV():
            evc[0] += 1
            return ev[evc[0] % 3]

        cur_seg = [None]
        wc = [0]
        for (g0, ntg, seg) in groups:
            if cur_seg[0] != seg:
                for i in (2, 3, 4, 5):
                    k.dma(bct[i][:], io[f'modR{l}'][seg, i * D:(i + 1) * D].partition_broadcast(128))
                cur_seg[0] = seg
            G2, A2, S2, G5 = bct[2], bct[3], bct[4], bct[5]
            nti = ntg // 128
            Wout = W2s[0]
            for kc in range(8):
                s_ = wstg[kc % 2]
                k.dma(s_[:], io['w_out'][l, kc * 128:(kc + 1) * 128, :])
                k.copy(Wout[:, kc, :], s_[:], e='pool')
            for ti in range(nti):
                t0 = g0 + ti * 128
                x_ = xt[0]
                k.dma(x_[:], xsrc[t0:t0 + 128, :])
                k.dma(mixf[:], io['mix_d'][t0:t0 + 128, :])
                k.copy(mixb[:], mixf[:], e='pool')
                ps = k.psum()
                pb = ps[:].bitcast(BF16)
                for kc in range(8):
                    k.tr(pb[:, kc * 128:(kc + 1) * 128], mixb[:, kc * 128:(kc + 1) * 128], identb[:])
                k.copy(mixT[:], pb[:, 0:1024].rearrange("p (kc t) -> p kc t", kc=8), e='act')
                for half in range(2):
                    ps = k.psum()
                    for kc in range(8):
                        k.mm(ps[:, :], mixT[:, kc, :], Wout[:, kc, half * 512:(half + 1) * 512], kc == 0, kc == 7)
                    k.tt(tmp[:, half * 512:(half + 1) * 512], ps[:, :], G2[:, half * 512:(half + 1) * 512], ALU.mult)
                k.tt(x_[:], x_[:], tmp[:], ALU.add, e='pool')
                k.dma(io['xs'][t0:t0 + 128, :], x_[:], q='pool')
                k.act(junk[:], x_[:], AF.Square, accum_out=ssq[:, 0:1])
                k.act(ssq[:, 1:2], ssq[:, 0:1], AF.Sqrt, bias=1e-6, scale=1.0 / D)
                k.op('dve', lambda E: E.reciprocal(ssq[:, 2:3], ssq[:, 1:2]), [ssq[:]], [ssq[:]])
                k.stt(tmp[:], x_[:], ssq[:, 2:3], A2[:], ALU.mult, ALU.mult)
                k.tt(tmp[:], tmp[:], S2[:], ALU.add, e='pool')
                k.copy(hb[:], tmp[:], e='pool')
                ps = k.psum()
                pb = ps[:].bitcast(BF16)
                for kc in range(8):
                    k.tr(pb[:, kc * 128:(kc + 1) * 128], hb[:, kc * 128:(kc + 1) * 128], identb[:])
                k.copy(h2T[:, :, ti * 128:(ti + 1) * 128], pb[:, 0:1024].rearrange("p (kc t) -> p kc t", kc=8), e='act')
                for hh in range(2):
                    ps = k.psum()
                    for q_ in range(4):
                        kc = hh * 4 + q_
                        k.tr(ps[:, q_ * 128:(q_ + 1) * 128], tmp[:, kc * 128:(kc + 1) * 128], identf[:])
                    k.copy(h2fT[:, hh * 4:(hh + 1) * 4, :], ps[:, :].rearrange("p (kc t) -> p kc t", kc=4), e='act')
                ps = k.psum()
                for kc in range(8):
                    k.mm(ps[:, 0:NE], h2fT[:, kc, :], routw[:, kc, :], kc == 0, kc == 7)
                r_ = rt[ti % 2]
                k.tt(r_[:, 0:32], ps[:, 0:NE], routb[:], ALU.add)
                k.op('dve', lambda E: E.max(r_[:, 32:40], r_[:, 0:32]), [r_[:]], [r_[:]])
                k.ts(r_[:, 40:41], r_[:, 32:33], -1.0, None, ALU.mult)
                k.ts(r_[:, 64:96], r_[:, 0:32], r_[:, 35:36], None, ALU.is_ge)
                k.act(r_[:, 0:32], r_[:, 0:32], AF.Exp, bias=r_[:, 40:41])
                k.tt(r_[:, 0:32], r_[:, 0:32], r_[:, 64:96], ALU.mult)
                k.op('dve', lambda E: E.reduce_sum(r_[:, 41:42], r_[:, 0:32], AX.X), [r_[:]], [r_[:]])
                k.op('dve', lambda E: E.reciprocal(r_[:, 42:43], r_[:, 41:42]), [r_[:]], [r_[:]])
                k.ts(gates[:, ti, :], r_[:, 0:32], r_[:, 42:43], None, ALU.mult)
                ps = k.psum()
                k.tr(ps[0:NE, 0:128], gates[:, ti, :], identf[:])
                k.copy(gT[:], ps[0:NE, 0:128], e='act')
                for half in range(2):
                    ps = k.psum()
                    k.mm(ps[:, :], gT[:], b2[:, half * 512:(half + 1) * 512], True, True)
                    k.copy(yacc[:, ti, half * 512:(half + 1) * 512], ps[:, :], e='act')
            def weight_tasks(e):
                W1b, W2b = W1s[(e + 1) % 2], W2s[(e + 1) % 2]
                tasks = []

                def t_b1():
                    k.dma(b1c[e % 2][:], io['moe_b1'][l, e, :].rearrange("(c p) -> p c", p=128), allow_slow_non_contiguous=True)
                tasks.append(t_b1)
                for kc in range(8):
                    for half in range(2):
                        def t1(kc=kc, half=half):
                            s_ = wstg[wc[0] % 2]
                            wc[0] += 1
                            k.dma(s_[:], io['moe_w1'][l, e, kc * 128:(kc + 1) * 128, half * 1024:(half + 1) * 1024])
                            k.copy(W1b[:, kc, half * 1024:(half + 1) * 1024], s_[:], e='act')
                        tasks.append(t1)
                for fc in range(8):
                    def t2(fc=fc):
                        s_ = wstg[wc[0] % 2]
                        wc[0] += 1
                        k.dma(s_[:], io['moe_w2'][l, e, fc * 128:(fc + 1) * 128, :])
                        k.copy(W2b[:, fc, :], s_[:], e='act')
                    tasks.append(t2)
                return tasks

            for t_ in weight_tasks(0):
                t_()
            for e in range(NE):
                b1 = b1c[e % 2]
                W1b, W2b = W1s[(e + 1) % 2], W2s[(e + 1) % 2]
                pend = weight_tasks(e + 1) if e + 1 < NE else []
                hsz = min(512, ntg)
                nslots = (ntg // hsz) * (8 + (hsz // 128) * 2)
                per = -(-len(pend) // nslots) if pend else 0

                def drain():
                    for _ in range(per):
                        if pend:
                            pend.pop(0)()

                for hg in range(ntg // hsz):
                    c0 = hg * hsz
                    for i in range(8):
                        pg = k.psum()
                        for kc in range(8):
                            k.mm(pg[:, 0:hsz], W1b[:, kc, i * 128:(i + 1) * 128], h2T[:, kc, c0:c0 + hsz], kc == 0, kc == 7)
                        pl = k.psum()
                        for kc in range(8):
                            k.mm(pl[:, 0:hsz], W1b[:, kc, 1024 + i * 128:1024 + (i + 1) * 128], h2T[:, kc, c0:c0 + hsz], kc == 0, kc == 7)
                        g_, sg, lt = EV(), EV(), EV()
                        k.ts(g_[:, 0:hsz], pg[:, 0:hsz], b1[:, i:i + 1], 7.0, ALU.add, ALU.min)
                        k.act(sg[:, 0:hsz], g_[:, 0:hsz], AF.Silu, scale=1.702)
                        k.ts(lt[:, 0:hsz], pl[:, 0:hsz], b1[:, 8 + i:9 + i], 7.0, ALU.add, ALU.min)
                        k.ts(lt[:, 0:hsz], lt[:, 0:hsz], -7.0, 1.0, ALU.max, ALU.add)
                        k.stt(actT[:, i, 0:hsz], sg[:, 0:hsz], 1.0 / 1.702, lt[:, 0:hsz], ALU.mult, ALU.mult)
                        drain()
                    for tt_ in range(hsz // 128):
                        ti = (c0 // 128) + tt_
                        for half in range(2):
                            po = k.psum()
                            for fc in range(8):
                                k.mm(po[:, :], actT[:, fc, tt_ * 128:(tt_ + 1) * 128], W2b[:, fc, half * 512:(half + 1) * 512], fc == 0, fc == 7)
                            ys = yacc[:, ti, half * 512:(half + 1) * 512]
                            k.stt(ys, po[:, :], gates[:, ti, e:e + 1], ys, ALU.mult, ALU.add)
                            drain()
                while pend:
                    pend.pop(0)()
            for ti in range(nti):
                t0 = g0 + ti * 128
                x_ = xt[0]
                k.dma(x_[:], io['xs'][t0:t0 + 128, :])
                k.tt(tmp[:], yacc[:, ti, :], G5[:], ALU.mult)
                k.tt(x_[:], x_[:], tmp[:], ALU.add, e='pool')
                if not last:
                    k.dma(io['xs'][t0:t0 + 128, :], x_[:], q='pool')
                else:
                    k.act(junk[:], x_[:], AF.Square, accum_out=ssq[:, 0:1])
                    k.act(ssq[:, 1:2], ssq[:, 0:1], AF.Sqrt, bias=1e-6, scale=1.0 / D)
                    k.op('dve', lambda E: E.reciprocal(ssq[:, 2:3], ssq[:, 1:2]), [ssq[:]], [ssq[:]])
                    k.stt(tmp[:], x_[:], ssq[:, 2:3], fng[:], ALU.mult, ALU.mult)
                    k.dma(out_ap[t0:t0 + 128, :], tmp[:], q='pool')
        k.barrier()


Prog.stage_C = stage_C
```

```python
import numpy as np
import ml_dtypes
from contextlib import ExitStack
import concourse.bass as bass
import concourse.mybir as mybir
from concourse.bass_utils import run_bass_kernel_spmd

F32 = mybir.dt.float32
BF16 = mybir.dt.bfloat16
AF = mybir.ActivationFunctionType
ALU = mybir.AluOpType
AX = mybir.AxisListType

D = 1024
T = 8192
LC = 256
N = T + LC
NT = N // 128
DEPTH = 2
HD = 64
NE = 32
import os as _os0
SEM_ROT = int(_os0.environ.get('SEM_ROT', 20000))


class K:
    def __init__(self, nc, es):
        self.nc = nc
        self.es = es
        self.eng = {'pe': nc.tensor, 'act': nc.scalar, 'dve': nc.vector, 'pool': nc.gpsimd, 'sp': nc.sync}
        self.esems = {e: [es.enter_context(nc.semaphore(f"s_{e}_0"))] for e in ('pe', 'act', 'dve', 'pool')}
        self.ecnt = {e: 0 for e in self.esems}
        self.dsems = [es.enter_context(nc.semaphore(f"s_dma_{i}")) for i in range(44)]
        self.dcnt = [0] * len(self.dsems)
        self.drr = 0
        self.seen = {e: {} for e in self.eng}
        self.res = {}
        self.semobj = {}
        for e, l in self.esems.items():
            self.semobj[id(l[0])] = l[0]
        for s in self.dsems:
            self.semobj[id(s)] = s
        self.psb = [es.enter_context(nc.psum_tensor(f"psb{i}", [128, 512], F32)) for i in range(8)]
        self.psi = 0
        self.ninst = 0
        self.E1 = es.enter_context(nc.semaphore("s_bar1"))
        self.E2 = es.enter_context(nc.semaphore("s_bar2"))
        self.epoch = 0

    def psum(self):
        p = self.psb[self.psi % 8]
        self.psi += 1
        return p

    def _wait(self, e, sem, cnt):
        sid = id(sem)
        if self.seen[e].get(sid, 0) >= cnt:
            return
        self.eng[e].wait_ge(sem, cnt)
        self.seen[e][sid] = cnt
        self.ninst += 1

    def _deps(self, e, reads, writes, acc):
        for ap in reads:
            r = self.res.get(ap.name)
            if r and r['w']:
                self._wait(e, *r['w'])
        for ap in writes:
            r = self.res.get(ap.name)
            if not r:
                continue
            if r['w'] and not (acc and e == 'pe' and r.get('we') == 'pe'):
                self._wait(e, *r['w'])
            for sid, (sem, cnt) in r['r'].items():
                self._wait(e, sem, cnt)

    def _record(self, e, sem, cnt, reads, writes):
        for ap in reads:
            r = self.res.setdefault(ap.name, {'w': None, 'r': {}})
            r['r'][id(sem)] = (sem, cnt)
        for ap in writes:
            r = self.res.setdefault(ap.name, {'w': None, 'r': {}})
            r['w'] = (sem, cnt)
            r['we'] = e
            r['r'] = {}

    def op(self, e, fn, reads, writes, acc=False):
        self._deps(e, reads, writes, acc)
        ins = fn(self.eng[e])
        if self.ecnt[e] >= SEM_ROT:
            s = self.es.enter_context(self.nc.semaphore(f"s_{e}_{len(self.esems[e])}"))
            self.esems[e].append(s)
            self.semobj[id(s)] = s
            self.ecnt[e] = 0
        sem = self.esems[e][-1]
        self.ecnt[e] += 1
        ins.then_inc(sem, 1)
        self._record(e, sem, self.ecnt[e], reads, writes)
        self.ninst += 1
        return ins

    def dma(self, out, in_, q='sp', **kw):
        self._deps(q, [in_], [out], False)
        i = self.drr % len(self.dsems)
        self.drr += 1
        sem = self.dsems[i]
        if self.dcnt[i]:
            self._wait(q, sem, self.dcnt[i])
        ins = self.eng[q].dma_start(out=out, in_=in_, **kw)
        self.dcnt[i] += 16
        ins.then_inc(sem, 16)
        self._record(q, sem, self.dcnt[i], [in_], [out])
        self.ninst += 1

    def barrier(self):
        for e in self.eng:
            for e2, l in self.esems.items():
                if self.ecnt[e2]:
                    self._wait(e, l[-1], self.ecnt[e2])
            for i, s in enumerate(self.dsems):
                if self.dcnt[i]:
                    self._wait(e, s, self.dcnt[i])
        self.res = {}

    def mm(self, out, lhsT, rhs, start, stop):
        return self.op('pe', lambda E: E.matmul(out, lhsT, rhs, start=start, stop=stop), [lhsT, rhs], [out],
                       acc=not start)

    def tr(self, out, in_, ident):
        return self.op('pe', lambda E: E.transpose(out, in_, ident), [in_, ident], [out])

    def act(self, out, in_, func, bias=None, scale=None, accum_out=None, e='act'):
        kw = {}
        rd = [in_]
        wr = [out]
        if bias is not None:
            kw['bias'] = bias
            if not isinstance(bias, (int, float)):
                rd.append(bias)
        if scale is not None:
            kw['scale'] = scale
            if not isinstance(scale, (int, float)):
                rd.append(scale)
        if accum_out is not None:
            kw['accum_out'] = accum_out
            wr.append(accum_out)
        return self.op(e, lambda E: E.activation(out, in_, func, **kw), rd, wr)

    def tt(self, out, a, b, op, e='dve'):
        return self.op(e, lambda E: E.tensor_tensor(out, a, b, op), [a, b], [out])

    def ts(self, out, a, s1, s2, op0, op1=None, e='dve', accum_out=None):
        rd = [a] + [s for s in (s1, s2) if s is not None and not isinstance(s, (int, float))]
        wr = [out] + ([accum_out] if accum_out is not None else [])
        kw = {}
        if op1 is not None:
            kw['op1'] = op1
        if accum_out is not None:
            kw['accum_out'] = accum_out
        return self.op(e, lambda E: E.tensor_scalar(out, a, s1, s2, op0, **kw), rd, wr)

    def stt(self, out, a, s, b, op0, op1, e='dve'):
        rd = [a, b] + ([] if isinstance(s, (int, float)) else [s])
        return self.op(e, lambda E: E.scalar_tensor_tensor(out, a, s, b, op0, op1), rd, [out])

    def copy(self, out, in_, e='dve'):
        if e == 'act':
            return self.op(e, lambda E: E.copy(out, in_), [in_], [out])
        return self.op(e, lambda E: E.tensor_copy(out, in_), [in_], [out])

    def memset(self, out, v, e='pool'):
        return self.op(e, lambda E: E.memset(out, v), [], [out])


ATT_IN = 768
RW0 = 768
ML0 = 1728
NIN = 2768


def _rope_swap_idx():
    i = np.arange(64)
    blk = i // 16
    return np.where(blk % 2 == 0, i + 16, i - 16)


def build_colplan(layer):
    src = []
    cw = []
    groups = []

    def add(name, kind, cols, conv):
        base = len(src)
        nterm = 3 if conv else 1
        for j in range(nterm):
            for c in cols:
                src.append(c)
                if conv == 1:
                    cw.append((1, j, c - RW0))
                elif conv == 2:
                    cw.append((2, j, c - ML0))
                else:
                    cw.append((0, 0, 0))
        groups.append(dict(name=name, kind=kind, base=base, n=len(cols), nterm=nterm))

    sw = _rope_swap_idx()
    q = np.arange(512)
    qs = (q // 64) * 64 + sw[q % 64]
    kk = 512 + np.arange(128)
    ks = 512 + (np.arange(128) // 64) * 64 + sw[np.arange(128) % 64]
    add('attq', 'fm', list(q), 0)
    add('attqs', 'fm', list(qs), 0)
    add('attk', 'fm', list(kk), 0)
    add('attks', 'fm', list(ks), 0)
    add('rwx', 'fm', list(RW0 + 768 + np.arange(192)), 1)
    add('mlqk', 'fm', list(ML0 + np.arange(512)), 2)
    if layer > 0:
        add('vdown', 'fm', list(NIN + np.arange(32)), 0)
    add('attv', 'tm', list(640 + np.arange(128)), 0)
    add('rwrkv', 'tm', list(RW0 + np.arange(768)), 1)
    add('mlvog', 'tm', list(ML0 + 512 + np.arange(528)), 0)
    return np.array(src), cw, groups


_SBN = [0]


def sb(st, nc, name, shape, dt):
    _SBN[0] += 1
    return st.enter_context(nc.sbuf_tensor(f"{name}_u{_SBN[0]}", shape, dt))


def host_consts():
    c = {}
    c['ident_f'] = np.eye(128, dtype=np.float32)
    c['ident_b'] = np.eye(128).astype(ml_dtypes.bfloat16)
    t = np.arange(T)
    row = (t // 64).astype(np.float32)
    col = (t % 64).astype(np.float32)
    inv = (10000.0 ** (-np.arange(0, 32, 2, dtype=np.float32) / 32)).astype(np.float32)
    ang = np.concatenate([row[:, None] * inv, col[:, None] * inv], axis=-1).astype(np.float32)
    cos = np.cos(ang).astype(np.float32)
    sin = np.sin(ang).astype(np.float32)
    ct = np.ones((64, N), np.float32)
    stb = np.zeros((64, N), np.float32)
    for d in range(64):
        blk = d // 16
        f = (blk // 2) * 16 + d % 16
        ct[d, :T] = cos[:, f]
        stb[d, :T] = (-sin[:, f]) if blk % 2 == 0 else sin[:, f]
    c['rope_c'] = np.concatenate([ct, ct], 0)
    c['rope_s'] = np.concatenate([stb, stb], 0)
    c.update(att_consts())
    return c


class Prog:
    def __init__(self, nlayers=DEPTH, stop=None, dbg=()):
        self.nlayers = nlayers
        self.stop = stop
        self.dbg = dbg
        self.es = ExitStack()
        nc = self.nc = bass.Bass("TRN2", target_bir_lowering=False)
        self.k = K(nc, self.es)
        self.io = {}
        self.plans = [build_colplan(l) for l in range(DEPTH)]

    def din(self, name, shape, dt=F32):
        self.io[name] = self.nc.dram_tensor(name, list(shape), dt, kind="ExternalInput").ap()
        return self.io[name]

    def dscr(self, name, shape, dt=F32):
        kind = "ExternalOutput" if name in self.dbg else "Internal"
        self.io[name] = self.nc.dram_tensor(name, list(shape), dt, kind=kind).ap()
        return self.io[name]

    def declare(self):
        self.din('x0', [N, D])
        self.din('cc', [2, D])
        for nm in ('ident_f', 'rope_c', 'rope_s'):
            self.din(nm, {'ident_f': [128, 128], 'rope_c': [128, N], 'rope_s': [128, N]}[nm])
        self.din('ident_b', [128, 128], BF16)
        self.din('norm1_g', [DEPTH, D]); self.din('norm2_g', [DEPTH, D])
        self.din('ada_w', [DEPTH, D, 6 * D]); self.din('ada_b', [DEPTH, 6 * D])
        for l in range(DEPTH):
            ncw = len(self.plans[l][0])
            self.din(f'wx{l}', [D, ncw]); self.din(f'cs{l}', [1, ncw])
            self.dscr(f'modR{l}', [2, 6 * D])
        self.dscr('attq_d', [512, N]); self.dscr('attk_d', [128, N]); self.dscr('rwx_d', [192, N])
        self.dscr('mlqk_d', [512, N]); self.dscr('vdown_d', [32, N])
        self.dscr('attv_d', [N, 128]); self.dscr('rwrkv_d', [N, 768]); self.dscr('mlvog_d', [N, 528])
        self.dscr('xs', [N, D])
        self.dscr('mix_d', [N, D])
        self.din('mask_ge', [128, 512], BF16); self.din('mask_le', [128, 512], BF16)
        self.din('attn_sink', [DEPTH, 8])
        for nm, shp in (('rwkv_w0', [DEPTH, 2, 256]), ('rwkv_w_up', [DEPTH, 2, 32, 256]), ('rwkv_a0', [DEPTH, 2, 256]),
                        ('rwkv_a_up', [DEPTH, 2, 32, 256]), ('rwkv_g_up', [DEPTH, 64, 256]), ('rwkv_k_k', [DEPTH, 256]),
                        ('rwkv_k_a', [DEPTH, 256]), ('rwkv_r_k', [DEPTH, 4, 64]), ('rwkv_ln_w', [DEPTH, 256]),
                        ('rwkv_ln_b', [DEPTH, 256]), ('rwkv_v0', [1, 256]), ('rwkv_v_up', [1, 32, 256])):
            self.din(nm, shp)
        for nm, shp in (('vfirst_d', [N, 256]), ('rfm_d', [256, N]), ('rgate_d', [N, 256]),
                        ('rw_d', [2, 256, N]), ('rbonus_d', [2, N, 256]), ('ryscan_d', [2, N, 256])):
            self.dscr(nm, shp)
        for nm, shp in (('rv_d', [N, 256]), ('rnk_d', [N, 256]), ('rkka_d', [2, N, 256]), ('rkd_d', [2, N, 256])):
            self.dscr(nm, shp, BF16)
        for nm, shp in (('w_out', [DEPTH, D, D]), ('router_w', [DEPTH, D, NE]), ('router_b', [DEPTH, NE]), ('moe_w1', [DEPTH, NE, D, 2 * D]),
                        ('moe_b1', [DEPTH, NE, 2 * D]), ('moe_w2', [DEPTH, NE, D, D]), ('moe_b2', [DEPTH, NE, D]), ('final_norm_g', [D])):
            self.din(nm, shp)
        self.din('tri_le', [128, 128]); self.din('tri_ge', [128, 128])
        self.din('mlstm_gate_b', [DEPTH, 16]); self.din('mlstm_ln_w', [DEPTH, 256])

    def stage_M(self, l):
        k, nc, io = self.k, self.nc, self.io
        with ExitStack() as st:
            cT = sb(st, nc, 'cT', [128, 16], F32)
            adab = sb(st, nc, 'adab', [2, 6 * D], F32)
            gg = sb(st, nc, 'gg', [2, 2 * D], F32)
            mod = sb(st, nc, 'mod', [2, 6 * D], F32)
            R = sb(st, nc, 'R', [2, 6 * D], F32)
            wt = [sb(st, nc, f'adawt{i}', [128, 8, 512], F32) for i in range(2)]
            for r in range(2):
                k.dma(cT[:, r * 8:(r + 1) * 8], io['cc'][r, :].rearrange("(kc p) -> p kc", p=128),
                      allow_slow_non_contiguous=True)
            for r in range(2):
                k.dma(adab[r:r + 1, :], io['ada_b'][l:l + 1, :])
                k.dma(gg[r:r + 1, 0:D], io['norm1_g'][l:l + 1, :])
                k.dma(gg[r:r + 1, D:2 * D], io['norm2_g'][l:l + 1, :])
            k.act(cT[:], cT[:], AF.Silu)
            for ng in range(12):
                w = wt[ng % 2]
                k.dma(w[:], io['ada_w'][l, :, ng * 512:(ng + 1) * 512].rearrange("(kc p) n -> p kc n", p=128))
                ps = k.psum()
                for kc in range(8):
                    k.mm(ps[0:2, :], cT[:, kc:16:8], w[:, kc, :], kc == 0, kc == 7)
                k.tt(mod[:, ng * 512:(ng + 1) * 512], ps[0:2, :], adab[:, ng * 512:(ng + 1) * 512], ALU.add)
            m = lambda i: mod[:, i * D:(i + 1) * D]
            k.stt(R[:, 0:D], m(1), 1.0, gg[:, 0:D], ALU.add, ALU.mult)
            k.copy(R[:, D:2 * D], m(0))
            k.copy(R[:, 2 * D:3 * D], m(2))
            k.stt(R[:, 3 * D:4 * D], m(4), 1.0, gg[:, D:2 * D], ALU.add, ALU.mult)
            k.copy(R[:, 4 * D:5 * D], m(3))
            k.copy(R[:, 5 * D:6 * D], m(5))
            k.dma(io[f'modR{l}'], R[:], q='pool')
            k.barrier()

    def bcast_rows(self, st, l, idxs, tag):
        k, nc, io = self.k, self.nc, self.io
        out = {}
        for r in range(2):
            for i in idxs:
                t_ = sb(st, nc, f'bc_{tag}_{r}_{i}', [128, D], F32)
                k.dma(t_[:], io[f'modR{l}'][r, i * D:(i + 1) * D].partition_broadcast(128))
                out[(r, i)] = t_
        return out

    def norm_tile(self, xt, A, S, hb, tmp, junk, ssq, eps=1e-6):
        k = self.k
        k.act(junk[:], xt[:], AF.Square, accum_out=ssq[:, 0:1])
        k.act(ssq[:, 1:2], ssq[:, 0:1], AF.Sqrt, bias=eps, scale=1.0 / D)
        k.op('dve', lambda E: E.reciprocal(ssq[:, 2:3], ssq[:, 1:2]), [ssq[:]], [ssq[:]])
        k.stt(tmp[:], xt[:], ssq[:, 2:3], A[:], ALU.mult, ALU.mult)
        k.tt(hb[:], tmp[:], S[:], ALU.add, e='pool')

    def stage_A(self, l):
        k, nc, io = self.k, self.nc, self.io
        src, cw, groups = self.plans[l]
        ncw = len(src)
        xsrc = io['x0'] if l == 0 else io['xs']
        with ExitStack() as st:
            W = sb(st, nc, 'W_sb', [128, 8, ncw], BF16)
            identb = sb(st, nc, 'identb', [128, 128], BF16)
            k.dma(identb[:], io['ident_b'])
            with ExitStack() as st2:
                csb = sb(st2, nc, 'csb', [128, ncw], F32)
                stg = [sb(st2, nc, f'wstg{i}', [128, 512], F32) for i in range(3)]
                k.dma(csb[:], io[f'cs{l}'][0, :].partition_broadcast(128))
                i = 0
                for c0 in range(0, ncw, 512):
                    c1 = min(ncw, c0 + 512)
                    for kc in range(8):
                        s_ = stg[i % 3]
                        i += 1
                        k.dma(s_[:, 0:c1 - c0], io[f'wx{l}'][kc * 128:(kc + 1) * 128, c0:c1])
                        k.tt(W[:, kc, c0:c1], s_[:, 0:c1 - c0], csb[:, c0:c1], ALU.mult, e=('dve' if i % 2 else 'pool'))
                k.barrier()
            bc = self.bcast_rows(st, l, (0, 1), 'A')
            hT = [sb(st, nc, f'hT{i}', [128, 8, 514], BF16) for i in range(3)]
            xt = [sb(st, nc, f'xt{i}', [128, D], F32) for i in range(2)]
            tmp = sb(st, nc, 'ntmp', [128, D], F32)
            junk = sb(st, nc, 'njunk', [128, D], BF16)
            hb = [sb(st, nc, f'hb{i}', [128, D], BF16) for i in range(2)]
            ssq = [sb(st, nc, f'ssq{i}', [128, 4], F32) for i in range(2)]
            ropec = sb(st, nc, 'ropec', [128, 512], F32)
            ropes = sb(st, nc, 'ropes', [128, 512], F32)
            osb = [sb(st, nc, f'osb{i}', [128, 512], F32) for i in range(4)]
            rsw = sb(st, nc, 'rsw', [128, 512], F32)
            rt1 = sb(st, nc, 'rt1', [128, 512], F32)
            glist = [(g * 512, 512, 0) for g in range(16)] + [(T, 256, 1)]
            ocnt = [0]

            def nexto():
                ocnt[0] += 1
                return osb[ocnt[0] % 4]

            def make_hT(gi):
                t0, n, seg = glist[gi]
                buf = hT[gi % 3]
                for ti in range(n // 128):
                    x_ = xt[ti % 2]
                    k.dma(x_[:], xsrc[t0 + ti * 128:t0 + (ti + 1) * 128, :])
                    self.norm_tile(x_, bc[(seg, 0)], bc[(seg, 1)], hb[ti % 2], tmp, junk, ssq[ti % 2])
                    ps = k.psum()
                    pb = ps[:].bitcast(BF16)
                    for kc in range(8):
                        k.tr(pb[:, kc * 128:(kc + 1) * 128], hb[ti % 2][:, kc * 128:(kc + 1) * 128], identb[:])
                    k.copy(buf[:, :, 1 + ti * 128:1 + (ti + 1) * 128], pb[:, 0:1024].rearrange("p (kc t) -> p kc t", kc=8),
                           e='act')
                if gi > 0 and glist[gi - 1][2] == seg:
                    k.copy(buf[:, :, 0:1], hT[(gi - 1) % 3][:, :, 512:513], e='pool')
                    k.copy(hT[(gi - 1) % 3][:, :, 513:514], buf[:, :, 1:2], e='pool')
                else:
                    k.memset(buf[:, :, 0:1], 0.0)
                    if gi > 0:
                        k.memset(hT[(gi - 1) % 3][:, :, 513:514], 0.0)
                if gi == len(glist) - 1:
                    k.memset(buf[:, :, 1 + n:2 + n], 0.0)

            def proj(gi):
                t0, n, seg = glist[gi]
                buf = hT[gi % 3]
                k.dma(ropec[:, 0:n], io['rope_c'][:, t0:t0 + n])
                k.dma(ropes[:, 0:n], io['rope_s'][:, t0:t0 + n])
                G = {g['name']: g for g in groups}

                def fm_acc(g, c0, m):
                    ps = k.psum()
                    nt_ = g['nterm']
                    tot = nt_ * 8
                    ii = 0
                    for j in range(nt_):
                        sh = (j - 1) if nt_ == 3 else 0
                        for kc in range(8):
                            cb = g['base'] + j * g['n'] + c0
                            k.mm(ps[0:m, 0:n], W[:, kc, cb:cb + m], buf[:, kc, 1 + sh:1 + sh + n], ii == 0, ii == tot - 1)
                            ii += 1
                    return ps

                for (ga, gb, dst) in (('attq', 'attqs', 'attq_d'), ('attk', 'attks', 'attk_d')):
                    for c0 in range(0, G[ga]['n'], 128):
                        p1 = fm_acc(G[ga], c0, 128)
                        p2 = fm_acc(G[gb], c0, 128)
                        o = nexto()
                        k.tt(rt1[:, 0:n], p1[:, 0:n], ropec[:, 0:n], ALU.mult)
                        k.copy(rsw[:, 0:n], p2[:, 0:n], e='act')
                        k.tt(rsw[:, 0:n], rsw[:, 0:n], ropes[:, 0:n], ALU.mult, e='pool')
                        k.tt(o[:, 0:n], rt1[:, 0:n], rsw[:, 0:n], ALU.add)
                        k.dma(io[dst][c0:c0 + 128, t0:t0 + n], o[:, 0:n], q='pool')
                for (ga, dst, fn) in (('rwx', 'rwx_d', None), ('mlqk', 'mlqk_d', AF.Silu), ('vdown', 'vdown_d', None)):
                    if ga not in G:
                        continue
                    for c0 in range(0, G[ga]['n'], 128):
                        m = min(128, G[ga]['n'] - c0)
                        p1 = fm_acc(G[ga], c0, m)
                        o = nexto()
                        if fn is None:
                            k.copy(o[0:m, 0:n], p1[0:m, 0:n], e='act')
                        else:
                            k.act(o[0:m, 0:n], p1[0:m, 0:n], fn)
                        k.dma(io[dst][c0:c0 + m, t0:t0 + n], o[0:m, 0:n], q='pool')
                for ti in range(n // 128):
                    for (ga, dst) in (('attv', 'attv_d'), ('rwrkv', 'rwrkv_d'), ('mlvog', 'mlvog_d')):
                        g = G[ga]
                        for c0 in range(0, g['n'], 512):
                            w = min(512, g['n'] - c0)
                            ps = k.psum()
                            nt_ = g['nterm']
                            tot = nt_ * 8
                            ii = 0
                            for j in range(nt_):
                                sh = (j - 1) if nt_ == 3 else 0
                                for kc in range(8):
                                    cb = g['base'] + j * g['n'] + c0
                                    a = 1 + ti * 128 + sh
                                    k.mm(ps[:, 0:w], buf[:, kc, a:a + 128], W[:, kc, cb:cb + w], ii == 0, ii == tot - 1)
                                    ii += 1
                            o = nexto()
                            k.copy(o[:, 0:w], ps[:, 0:w], e=('act' if (ti + c0) % 2 else 'dve'))
                            k.dma(io[dst][t0 + ti * 128:t0 + (ti + 1) * 128, c0:c0 + w], o[:, 0:w], q='pool')

            make_hT(0)
            for gi in range(len(glist)):
                if gi + 1 < len(glist):
                    make_hT(gi + 1)
                proj(gi)
            k.barrier()


def host_inputs(inp, b, consts, plans):
    m = {}
    m['x0'] = np.concatenate([inp['x'][b], inp['ctx'][b]], axis=0)
    m['cc'] = np.stack([inp['c'][b], inp['c_ctx']], axis=0)
    m.update(consts)
    for nm in ('norm1_g', 'norm2_g', 'ada_w', 'ada_b', 'attn_sink', 'mlstm_gate_b', 'mlstm_ln_w', 'rwkv_w0', 'rwkv_w_up', 'rwkv_a0', 'rwkv_a_up', 'rwkv_g_up',
               'rwkv_k_k', 'rwkv_k_a', 'rwkv_r_k', 'rwkv_ln_w', 'rwkv_ln_b', 'rwkv_v0', 'rwkv_v_up',
               'w_out', 'router_w', 'router_b', 'moe_w1', 'moe_b1', 'moe_w2', 'moe_b2', 'final_norm_g'):
        m[nm] = inp[nm]
    ones = np.ones((1,), np.float32)
    for l in range(DEPTH):
        src, cw, groups = plans[l]
        wext = inp['w_in'][l] if l == 0 else np.concatenate([inp['w_in'][l], inp['rwkv_v_down'][l - 1]], axis=1)
        m[f'wx{l}'] = np.ascontiguousarray(wext[:, src])
        cs = np.empty((1, len(src)), np.float32)
        which = np.array([c[0] for c in cw]); jj = np.array([c[1] for c in cw]); cc_ = np.array([c[2] for c in cw])
        cs[0, which == 0] = ones[0]
        cs[0, which == 1] = inp['rwkv_conv'][l][jj[which == 1], cc_[which == 1]]
        cs[0, which == 2] = inp['mlstm_conv'][l][jj[which == 2], cc_[which == 2]]
        m[f'cs{l}'] = cs
    return m


def att_consts():
    j = np.arange(128)[:, None]
    i = np.arange(128)[None, :]
    ge = (j >= i).astype(np.float32)
    le = (j <= i).astype(np.float32)
    extra = {'tri_le': le, 'tri_ge': ge}
    return {**extra, 'mask_ge': np.tile(ge, (1, 4)).astype(ml_dtypes.bfloat16), 'mask_le': np.tile(le, (1, 4)).astype(ml_dtypes.bfloat16)}


def stage_ATT(self, l):
    k, nc, io = self.k, self.nc, self.io
    need_ctx = l < DEPTH - 1
    with ExitStack() as st:
        kTb = sb(st, nc, 'kTb', [128, N], BF16)
        vb = sb(st, nc, 'vb', [128, NT, 2, 65], BF16)
        mge = sb(st, nc, 'mge', [128, 512], BF16)
        mle = sb(st, nc, 'mle', [128, 512], BF16)
        esink = sb(st, nc, 'esink', [128, 8], F32)
        stg = [sb(st, nc, f'astg{i}', [128, 512], F32) for i in range(2)]
        qf = [sb(st, nc, f'qf{i}', [128, 512], F32) for i in range(2)]
        qb_ = [sb(st, nc, f'qb{i}', [128, 512], BF16) for i in range(2)]
        pt = [sb(st, nc, f'pt{i}', [128, 512], BF16) for i in range(6)]
        osb = [sb(st, nc, f'aosb{i}', [128, 512], F32) for i in range(2)]
        den = sb(st, nc, 'aden', [128, 8], F32)
        k.dma(mge[:], io['mask_ge'])
        k.dma(mle[:], io['mask_le'])
        k.dma(esink[:], io['attn_sink'][l, :].partition_broadcast(128))
        k.act(esink[:], esink[:], AF.Exp)
        k.memset(vb[:, :, :, 64:65], 1.0)
        for c in range(0, N, 512):
            w = min(512, N - c)
            s_ = stg[(c // 512) % 2]
            k.dma(s_[:, 0:w], io['attk_d'][:, c:c + w])
            k.copy(kTb[:, c:c + w], s_[:, 0:w], e='pool')
        for ti in range(NT):
            s_ = stg[ti % 2]
            k.dma(s_[:, 0:128], io['attv_d'][ti * 128:(ti + 1) * 128, :])
            k.copy(vb[:, ti, :, 0:64], s_[:, 0:128].rearrange("p (g d) -> p g d", g=2), e='dve')
        qblocks = list(range(64)) + ([64, 65] if need_ctx else [])
        pi = 0
        for n_, qb in enumerate(qblocks):
            if qb < 64:
                keys = [(kb, m) for kb, m in ((qb - 1, mge), (qb, None), (qb + 1, mle)) if 0 <= kb < 64] + [(64, None), (65, None)]
            else:
                keys = [(64, None), (65, None)]
            q_f, q_b, o_ = qf[n_ % 2], qb_[n_ % 2], osb[n_ % 2]
            for g in range(2):
                k.dma(q_f[g * 64:(g + 1) * 64, :].rearrange("p (h t) -> p h t", h=4),
                      io['attq_d'][g * 256:(g + 1) * 256, qb * 128:(qb + 1) * 128].rearrange("(h d) t -> d h t", d=64))
            k.copy(q_b[:], q_f[:], e='pool')
            for g in range(2):
                ptl = []
                for kb, m in keys:
                    ps = k.psum()
                    k.mm(ps[:, :], kTb[g * 64:(g + 1) * 64, kb * 128:(kb + 1) * 128], q_b[g * 64:(g + 1) * 64, :], True, True)
                    p_ = pt[pi % 6]
                    pi += 1
                    k.act(p_[:], ps[:, :], AF.Exp, scale=0.125)
                    if m is not None:
                        k.tt(p_[:], p_[:], m[:], ALU.mult, e='pool')
                    ptl.append((p_, kb))
                po = k.psum()
                for h in range(4):
                    for i_, (p_, kb) in enumerate(ptl):
                        k.mm(po[:, h * 65:(h + 1) * 65], p_[:, h * 128:(h + 1) * 128], vb[:, kb, g, :], i_ == 0, i_ == len(ptl) - 1)
                pv = po[:, 0:260].rearrange("p (h e) -> p h e", h=4)
                k.tt(den[:, g * 4:(g + 1) * 4], pv[:, :, 64], esink[:, g * 4:(g + 1) * 4], ALU.add)
                k.op('dve', lambda E: E.reciprocal(den[:, g * 4:(g + 1) * 4], den[:, g * 4:(g + 1) * 4]), [den[:]], [den[:]])
                for h in range(4):
                    hh = g * 4 + h
                    k.ts(o_[:, hh * 64:(hh + 1) * 64], po[:, h * 65:h * 65 + 64], den[:, hh:hh + 1], None, ALU.mult)
            k.dma(io['mix_d'][qb * 128:(qb + 1) * 128, 0:512], o_[:], q='pool')
        k.barrier()


Prog.stage_ATT = stage_ATT


def stage_ML(self, l):
    k, nc, io = self.k, self.nc, self.io
    need_ctx = l < DEPTH - 1
    with ExitStack() as st:
        ysum = sb(st, nc, 'ysum', [128, NT, 256], F32)
        tri = {0: sb(st, nc, 'tri_le', [128, 128], F32), 1: sb(st, nc, 'tri_ge', [128, 128], F32)}
        ones = sb(st, nc, 'ones_f', [128, 128], F32)
        identb = sb(st, nc, 'identb', [128, 128], BF16)
        gb = sb(st, nc, 'gb', [128, 16], F32)
        lnw = sb(st, nc, 'lnw', [128, 256], F32)
        C = sb(st, nc, 'Cst', [128, 2, 65], F32)
        Cb = sb(st, nc, 'Cstb', [128, 2, 65], BF16)
        qkf = [sb(st, nc, f'qkf{i}', [128, 4, 128], F32) for i in range(2)]
        qkb = [sb(st, nc, f'qkb{i}', [128, 4, 128], BF16) for i in range(2)]
        vf = [sb(st, nc, f'vf{i}', [128, 528], F32) for i in range(2)]
        va = [sb(st, nc, f'va{i}', [128, 4, 65], BF16) for i in range(2)]
        gt = [sb(st, nc, f'gt{i}', [128, 48], F32) for i in range(2)]
        kpp = [sb(st, nc, f'kpp{i}', [128, 128], BF16) for i in range(2)]
        pm = [sb(st, nc, f'pm{i}', [128, 128], BF16) for i in range(2)]
        sc = [sb(st, nc, f'sc{i}', [128, 8], F32) for i in range(2)]
        k.dma(tri[0][:], io['tri_le']); k.dma(tri[1][:], io['tri_ge']); k.dma(identb[:], io['ident_b'])
        k.memset(ones[:], 1.0)
        k.dma(gb[:], io['mlstm_gate_b'][l, :].partition_broadcast(128))
        k.dma(lnw[:], io['mlstm_ln_w'][l, :].partition_broadcast(128))
        it = 0
        for d in range(2):
            order = [64, 65] + list(range(64)) if d == 0 else [65, 64] + list(range(63, -1, -1))
            k.memset(C[:], 0.0)
            k.memset(Cb[:], 0.0)
            for ti in order:
                i2 = it % 2
                it += 1
                t0 = ti * 128
                qf_, qb_, vf_, va_, g_, s_ = qkf[i2], qkb[i2], vf[i2], va[i2], gt[i2], sc[i2]
                k.dma(qf_[:], io['mlqk_d'][:, t0:t0 + 128].rearrange("(s p) t -> p s t", p=128))
                k.dma(vf_[:], io['mlvog_d'][t0:t0 + 128, :])
                k.copy(qb_[:], qf_[:], e='pool')
                k.copy(va_[:, :, 0:64], vf_[:, 0:256].rearrange("p (h e) -> p h e", h=4), e='pool')
                k.memset(va_[:, :, 64:65], 1.0)
                k.tt(g_[:, 0:8], vf_[:, 512 + d * 8:512 + d * 8 + 8], gb[:, d * 8:d * 8 + 8], ALU.add)
                k.act(g_[:, 8:12], g_[:, 4:8], AF.Exp, scale=-1.0)
                k.act(g_[:, 8:12], g_[:, 8:12], AF.Ln, bias=1.0)
                k.ts(g_[:, 4:8], g_[:, 8:12], -1.0, None, ALU.mult)
                ps = k.psum()
                k.mm(ps[:, 0:4], tri[d][:], g_[:, 4:8], True, True)
                k.mm(ps[:, 4:8], ones[:], g_[:, 4:8], True, True)
                k.copy(g_[:, 12:20], ps[:, 0:8])
                k.tt(g_[:, 20:24], g_[:, 0:4], g_[:, 12:16], ALU.subtract)
                k.act(g_[:, 24:28], g_[:, 20:24], AF.Exp)
                k.ts(g_[:, 24:28], g_[:, 24:28], 0.125, None, ALU.mult)
                k.tt(g_[:, 28:32], g_[:, 20:24], g_[:, 16:20], ALU.add)
                k.act(g_[:, 28:32], g_[:, 28:32], AF.Exp)
                k.ts(g_[:, 28:32], g_[:, 28:32], 0.125, None, ALU.mult)
                k.act(g_[:, 32:36], g_[:, 12:16], AF.Exp)
                k.act(g_[:, 36:40], g_[:, 16:20], AF.Exp)
                for pr in range(2):
                    pT = k.psum()
                    pTb = pT[:].bitcast(BF16)
                    k.tr(pTb[:, 0:128], qb_[:, 2 + pr, :], identb[:])
                    kp = kpp[pr]
                    for hh in range(2):
                        h = pr * 2 + hh
                        k.ts(kp[:, hh * 64:(hh + 1) * 64], pTb[:, hh * 64:(hh + 1) * 64], g_[:, 28 + h:29 + h], None, ALU.mult,
                             e=('dve' if hh else 'pool') if False else 'dve')
                    for hh in range(2):
                        h = pr * 2 + hh
                        p0 = hh * 64
                        pS = k.psum()
                        k.mm(pS[:, 0:128], qb_[p0:p0 + 64, 2 + pr, :], qb_[p0:p0 + 64, pr, :], True, True)
                        pm_ = pm[hh]
                        k.stt(pm_[:], pS[:, 0:128], g_[:, 24 + h:25 + h], tri[d][:], ALU.mult, ALU.mult)
                        pO = k.psum()
                        k.mm(pO[:, 0:65], pm_[:], va_[:, h, :], True, False)
                        k.mm(pO[:, 0:65], qb_[p0:p0 + 64, pr, :], Cb[p0:p0 + 64, pr, :], False, True)
                        k.ts(s_[:, 0:1], pO[:, 64:65], g_[:, 32 + h:33 + h], None, ALU.mult)
                        k.act(s_[:, 0:1], s_[:, 0:1], AF.Abs)
                        k.ts(s_[:, 0:1], s_[:, 0:1], 1.0, None, ALU.max)
                        k.op('dve', lambda E: E.reciprocal(s_[:, 1:2], s_[:, 0:1]), [s_[:]], [s_[:]])
                        k.tt(s_[:, 2:3], s_[:, 1:2], g_[:, 32 + h:33 + h], ALU.mult)
                        yv = ysum[:, ti, h * 64:(h + 1) * 64]
                        if d == 0:
                            k.ts(yv, pO[:, 0:64], s_[:, 2:3], None, ALU.mult)
                        else:
                            k.stt(yv, pO[:, 0:64], s_[:, 2:3], yv, ALU.mult, ALU.add)
                    pC = k.psum()
                    k.mm(pC[:, 0:130], kp[:], va_[:, pr * 2:pr * 2 + 2, :], True, True)
                    for hh in range(2):
                        h = pr * 2 + hh
                        p0 = hh * 64
                        k.stt(C[p0:p0 + 64, pr, :], C[p0:p0 + 64, pr, :], g_[p0:p0 + 64, 36 + h:37 + h],
                              pC[p0:p0 + 64, hh * 65:(hh + 1) * 65], ALU.mult, ALU.add)
                    k.copy(Cb[:, pr, :], C[:, pr, :], e='pool')
        tiles = list(range(64)) + ([64, 65] if need_ctx else [])
        for n_, ti in enumerate(tiles):
            i2 = n_ % 2
            vf_, g_ = vf[i2], gt[i2]
            o_ = qkf[i2][:, 0:2, :]
            yc = qkf[i2][:, 2:4, :]
            k.dma(vf_[:, 0:256], io['mlvog_d'][ti * 128:(ti + 1) * 128, 256:512])
            k.act(vf_[:, 0:256], vf_[:, 0:256], AF.Sigmoid)
            k.op('dve', lambda E: E.reduce_sum(g_[:, 0:4], ysum[:, ti, :].rearrange("p (h e) -> p h e", h=4), AX.X),
                 [ysum[:]], [g_[:]])
            k.ts(g_[:, 0:4], g_[:, 0:4], -1.0 / 64, None, ALU.mult)
            for h in range(4):
                k.ts(yc[:, h // 2, (h % 2) * 64:(h % 2) * 64 + 64], ysum[:, ti, h * 64:(h + 1) * 64], g_[:, h:h + 1], None, ALU.add)
                k.act(o_[:, h // 2, (h % 2) * 64:(h % 2) * 64 + 64], yc[:, h // 2, (h % 2) * 64:(h % 2) * 64 + 64], AF.Square,
                      accum_out=g_[:, 4 + h:5 + h])
            k.act(g_[:, 8:12], g_[:, 4:8], AF.Sqrt, bias=1e-5, scale=1.0 / 64)
            k.op('dve', lambda E: E.reciprocal(g_[:, 12:16], g_[:, 8:12]), [g_[:]], [g_[:]])
            for h in range(4):
                k.stt(o_[:, h // 2, (h % 2) * 64:(h % 2) * 64 + 64], yc[:, h // 2, (h % 2) * 64:(h % 2) * 64 + 64], g_[:, 12 + h:13 + h],
                      lnw[:, h * 64:(h + 1) * 64], ALU.mult, ALU.mult)
            k.tt(vf_[:, 256:512], o_.rearrange("p a b -> p (a b)"), vf_[:, 0:256], ALU.mult, e='pool')
            k.dma(io['mix_d'][ti * 128:(ti + 1) * 128, 768:1024], vf_[:, 256:512], q='pool')
        k.barrier()


Prog.stage_ML = stage_ML


def stage_RW(self, l):
    k, nc, io = self.k, self.nc, self.io
    need_ctx = l < DEPTH - 1
    with ExitStack() as st:
        identf = sb(st, nc, 'identf', [128, 128], F32)
        k.dma(identf[:], io['ident_f'])
        bc = {}
        for nm, src in (('k_k', io['rwkv_k_k'][l, :]), ('k_a', io['rwkv_k_a'][l, :]), ('r_k', io['rwkv_r_k'][l].rearrange("h e -> (h e)")),
                        ('a0_0', io['rwkv_a0'][l, 0, :]), ('a0_1', io['rwkv_a0'][l, 1, :])) + \
                (() if l == 0 else (('v0', io['rwkv_v0'][l - 1, :]),)):
            bc[nm] = sb(st, nc, 'rbc_' + nm, [128, 256], F32)
            k.dma(bc[nm][:], src.partition_broadcast(128))
        up = sb(st, nc, 'rw_up', [128, 2, 256], F32)
        gup = sb(st, nc, 'rw_gup', [64, 256], F32)
        vup = sb(st, nc, 'rw_vup', [32, 256], F32)
        w0c = sb(st, nc, 'rw_w0c', [128, 4], F32)
        for d in range(2):
            k.dma(up[d * 32:(d + 1) * 32, 0, :], io['rwkv_w_up'][l, d])
            k.dma(up[d * 32:(d + 1) * 32, 1, :], io['rwkv_a_up'][l, d])
            k.dma(w0c[:, d * 2:d * 2 + 2], io['rwkv_w0'][l, d, :].rearrange("(c p) -> p c", p=128), allow_slow_non_contiguous=True)
        k.dma(gup[:], io['rwkv_g_up'][l])
        if l > 0:
            k.dma(vup[:], io['rwkv_v_up'][l - 1])
        NB = 2
        xr = [sb(st, nc, f'rxr{i}', [128, 768], F32) for i in range(NB)]
        xf = [sb(st, nc, f'rxf{i}', [64, 128], F32) for i in range(NB)]
        xaf = [sb(st, nc, f'rxaf{i}', [64, 128], F32) for i in range(NB)]
        gf = [sb(st, nc, f'rgf{i}', [64, 128], F32) for i in range(NB)]
        lvf = [sb(st, nc, f'rlv{i}', [32, 128], F32) for i in range(NB)]
        vfst = [sb(st, nc, f'rvf{i}', [128, 256], F32) for i in range(NB)]
        kk = [sb(st, nc, f'rkk{i}', [128, 256], F32) for i in range(NB)]
        t1 = [sb(st, nc, f'rt1{i}', [128, 256], F32) for i in range(4)]
        t2 = [sb(st, nc, f'rt2{i}', [128, 256], F32) for i in range(4)]
        sm = [sb(st, nc, f'rsm{i}', [128, 16], F32) for i in range(NB)]
        fo = [sb(st, nc, f'rfo{i}', [128, 128], F32) for i in range(4)]
        tb = [sb(st, nc, f'rtb{i}', [128, 256], BF16) for i in range(6)]
        cnt = [0]

        def T1():
            cnt[0] += 1
            return t1[cnt[0] % 4]

        def T2():
            cnt[0] += 1
            return t2[cnt[0] % 4]

        def FO():
            cnt[0] += 1
            return fo[cnt[0] % 4]

        def TB():
            cnt[0] += 1
            return tb[cnt[0] % 6]

        for ti in range(NT):
            i2 = ti % NB
            t0 = ti * 128
            x_, xf_, gf_, kk_, sm_ = xr[i2], xf[i2], gf[i2], kk[i2], sm[i2]
            k.dma(x_[:], io['rwrkv_d'][t0:t0 + 128, :])
            k.dma(xf_[:], io['rwx_d'][0:64, t0:t0 + 128])
            k.dma(xaf[i2][:], io['rwx_d'][64:128, t0:t0 + 128])
            k.dma(gf_[:], io['rwx_d'][128:192, t0:t0 + 128])
            r_, k_, v_ = x_[:, 0:256], x_[:, 256:512], x_[:, 512:768]
            if l == 0:
                k.dma(io['vfirst_d'][t0:t0 + 128, :], v_, q='pool')
            else:
                k.dma(lvf[i2][:], io['vdown_d'][:, t0:t0 + 128])
                k.dma(vfst[i2][:], io['vfirst_d'][t0:t0 + 128, :])
                ps = k.psum()
                k.mm(ps[:, 0:256], lvf[i2][:], vup[:], True, True)
                a = T1()
                k.tt(a[:], ps[:, 0:256], bc['v0'][:], ALU.add)
                k.act(a[:], a[:], AF.Sigmoid)
                b_ = T2()
                k.tt(b_[:], vfst[i2][:], v_, ALU.subtract)
                k.tt(b_[:], b_[:], a[:], ALU.mult, e='pool')
                k.tt(v_, v_, b_[:], ALU.add)
            vb_ = TB()
            k.copy(vb_[:], v_, e='pool')
            k.dma(io['rv_d'][t0:t0 + 128, :], vb_[:], q='pool')
            k.tt(kk_[:], k_, bc['k_k'][:], ALU.mult)
            a = T1()
            for h in range(4):
                k.act(a[:, h * 64:(h + 1) * 64], kk_[:, h * 64:(h + 1) * 64], AF.Square, accum_out=sm_[:, h:h + 1])
            k.act(sm_[:, 4:8], sm_[:, 0:4], AF.Sqrt)
            k.ts(sm_[:, 4:8], sm_[:, 4:8], 1e-12, None, ALU.max)
            k.op('dve', lambda E: E.reciprocal(sm_[:, 8:12], sm_[:, 4:8]), [sm_[:]], [sm_[:]])
            for h in range(4):
                k.ts(kk_[:, h * 64:(h + 1) * 64], kk_[:, h * 64:(h + 1) * 64], sm_[:, 8 + h:9 + h], None, ALU.mult)
            nk_ = TB()
            k.ts(nk_[:], kk_[:], -1.0, None, ALU.mult, e='pool')
            k.dma(io['rnk_d'][t0:t0 + 128, :], nk_[:], q='pool')
            for src_, dst in ((r_, 'rfm_d'),):
                for c in range(2):
                    ps = k.psum()
                    k.tr(ps[:, 0:128], src_[:, c * 128:(c + 1) * 128], identf[:])
                    f_ = FO()
                    k.copy(f_[:], ps[:, 0:128], e='act')
                    k.dma(io[dst][c * 128:(c + 1) * 128, t0:t0 + 128], f_[:], q='pool')
            k.act(gf_[:], gf_[:], AF.Sigmoid)
            ps = k.psum()
            k.mm(ps[:, 0:256], gf_[:], gup[:], True, True)
            a = T1()
            k.copy(a[:], ps[:, 0:256], e='act')
            k.dma(io['rgate_d'][t0:t0 + 128, :], a[:], q='pool')
            k.act(xf_[0:64, :], xf_[0:64, :], AF.Tanh)
            for d in range(2):
                for c in range(2):
                    ps = k.psum()
                    k.mm(ps[:, 0:128], up[d * 32:(d + 1) * 32, 0, c * 128:(c + 1) * 128], xf_[d * 32:(d + 1) * 32, :], True, True)
                    f_ = FO()
                    k.act(f_[:], ps[:, 0:128], AF.Sigmoid, bias=w0c[:, d * 2 + c:d * 2 + c + 1])
                    k.act(f_[:], f_[:], AF.Exp, scale=-0.6065306597126334)
                    k.dma(io['rw_d'][d, c * 128:(c + 1) * 128, t0:t0 + 128], f_[:], q='pool')
                ps = k.psum()
                k.mm(ps[:, 0:256], xaf[i2][d * 32:(d + 1) * 32, :], up[d * 32:(d + 1) * 32, 1, :], True, True)
                a = T1()
                k.tt(a[:], ps[:, 0:256], bc[f'a0_{d}'][:], ALU.add)
                k.act(a[:], a[:], AF.Sigmoid)
                b_ = TB()
                k.tt(b_[:], kk_[:], a[:], ALU.mult, e='pool')
                k.dma(io['rkka_d'][d, t0:t0 + 128, :], b_[:], q='pool')
                c_ = T2()
                k.stt(c_[:], a[:], -1.0, bc['k_a'][:], ALU.add, ALU.mult)
                k.stt(c_[:], c_[:], 1.0, k_, ALU.add, ALU.mult)
                cb_ = TB()
                k.copy(cb_[:], c_[:], e='pool')
                k.dma(io['rkd_d'][d, t0:t0 + 128, :], cb_[:], q='pool')
                e_ = T1()
                k.tt(e_[:], c_[:], bc['r_k'][:], ALU.mult, e='pool')
                k.tt(e_[:], e_[:], r_, ALU.mult)
                k.op('dve', lambda E: E.reduce_sum(sm_[:, 12:16], e_[:].rearrange("p (h e) -> p h e", h=4), AX.X), [e_[:]], [sm_[:]])
                f2 = T2()
                for h in range(4):
                    k.ts(f2[:, h * 64:(h + 1) * 64], v_[:, h * 64:(h + 1) * 64], sm_[:, 12 + h:13 + h], None, ALU.mult)
                k.dma(io['rbonus_d'][d, t0:t0 + 128, :], f2[:], q='pool')
        k.barrier()
    SB = 8
    with ExitStack() as st:
        sel = sb(st, nc, 'rsel', [128, 2], BF16)
        k.memset(sel[:], 0.0)
        k.memset(sel[0:64, 0:1], 1.0)
        k.memset(sel[64:128, 1:2], 1.0)
        NBUF = 3
        mkf = lambda nm, shp, dt=F32: [[sb(st, nc, f'{nm}{g}_{i}', shp, dt) for i in range(NBUF)] for g in range(2)]
        Hist = mkf('rH', [128, SB, 2, 64])
        Sb = [[sb(st, nc, f'rSb{g}_{i}', [128, 2, 64], BF16) for i in range(2)] for g in range(2)]
        S0 = [sb(st, nc, f'rS0{g}', [128, 2, 64], F32) for g in range(2)]
        Ml = [[sb(st, nc, f'rMl{g}_{i}', [128, 2, 128], BF16) for i in range(3)] for g in range(2)]
        Rr = [mkf(f'rR{d}', [128, SB]) for d in range(2)]
        Wt = [mkf(f'Wt{d}', [128, SB]) for d in range(2)]
        NK = [mkf(f'NK{d}', [2, SB, 128], BF16) for d in range(2)]
        KA = [mkf(f'KA{d}', [2, SB, 128], BF16) for d in range(2)]
        KD = [mkf(f'KD{d}', [2, SB, 128], BF16) for d in range(2)]
        VV = [mkf(f'VV{d}', [2, SB, 64], BF16) for d in range(2)]
        YY = mkf('YY', [2, SB, 2, 64])
        ytmp = [[sb(st, nc, f'rytmp{g}_{i}', [128, SB, 2, 64], BF16) for i in range(2)] for g in range(2)]
        for g in range(2):
            k.memset(S0[g][:], 0.0)
            k.memset(Sb[g][0][:], 0.0)
            for d in range(2):
                for bi_ in range(NBUF):
                    for tl in (NK, KA, KD):
                        k.memset(tl[d][g][bi_][:], 0.0)
        import os as _os
        nblk = int(_os.environ.get('RW_NBLK', N // SB))

        def tokrange(blk, d):
            s0 = blk * SB
            if s0 < LC:
                return T + s0 if d == 0 else T + LC - s0 - SB
            return s0 - LC if d == 0 else T - (s0 - LC) - SB

        def load_block(blk, g):
            bi = blk % NBUF
            for d in range(2):
                tk = tokrange(blk, d)
                k.dma(Rr[d][g][bi][:], io['rfm_d'][g * 128:(g + 1) * 128, tk:tk + SB])
                k.dma(Wt[d][g][bi][:], io['rw_d'][d, g * 128:(g + 1) * 128, tk:tk + SB])
                for hh in range(2):
                    c0 = (g * 2 + hh) * 64
                    k.dma(NK[d][g][bi][hh:hh + 1, :, hh * 64:(hh + 1) * 64], io['rnk_d'][tk:tk + SB, c0:c0 + 64])
                    k.dma(KA[d][g][bi][hh:hh + 1, :, hh * 64:(hh + 1) * 64], io['rkka_d'][d, tk:tk + SB, c0:c0 + 64])
                    k.dma(KD[d][g][bi][hh:hh + 1, :, hh * 64:(hh + 1) * 64], io['rkd_d'][d, tk:tk + SB, c0:c0 + 64])
                    k.dma(VV[d][g][bi][hh:hh + 1, :, :], io['rv_d'][tk:tk + SB, c0:c0 + 64])

        nsteps = nblk * SB

        def cols(s):
            sl = s % SB
            return (sl, SB - 1 - sl)

        def build_M(s, g):
            bi = (s // SB) % NBUF
            col = cols(s)
            pM = k.psum()
            for d in range(2):
                k.mm(pM[:, d * 128:(d + 1) * 128], NK[d][g][bi][0:2, col[d], :], KA[d][g][bi][0:2, col[d], :], True, True)
            k.copy(Ml[g][s % 3][:].rearrange("p d c -> p (d c)"), pM[:, 0:256], e='act')

        def prev_state(s, g, d):
            if s == 0:
                return S0[g][:, d, :]
            ps_ = s - 1
            return Hist[g][(ps_ // SB) % NBUF][:, cols(ps_)[d], d, :]

        def block_mul(blk, g):
            bi = blk % NBUF
            yt_ = ytmp[g][blk % 2]
            for d in range(2):
                k.tt(yt_[:, :, d, :], Hist[g][bi][:, :, d, :],
                     Rr[d][g][bi][:].unsqueeze(2).to_broadcast([128, SB, 64]), ALU.mult, e='pool')

        def block_out(blk, g):
            bi = blk % NBUF
            yt_ = ytmp[g][blk % 2]
            yv = yt_[:].rearrange("p s d v -> p (s d v)")
            yy = YY[g][bi]
            for hf in range(2):
                pY = k.psum()
                k.mm(pY[0:2, :], sel[:], yv[:, hf * 512:(hf + 1) * 512], True, True)
                k.copy(yy[:].rearrange("p s d v -> p (s d v)")[:, hf * 512:(hf + 1) * 512], pY[0:2, :], e='act')
            if not _os.environ.get('RW_NOSTORE'):
                for d in range(2):
                    tk = tokrange(blk, d)
                    for hh in range(2):
                        c0 = (g * 2 + hh) * 64
                        k.dma(io['ryscan_d'][d, tk:tk + SB, c0:c0 + 64], yy[hh:hh + 1, :, d, :], q='pool')

        for g in range(2):
            load_block(0, g)
            if nblk > 1:
                load_block(1, g)
        for g in range(2):
            build_M(0, g)
        for s in range(nsteps):
            blk, sl = s // SB, s % SB
            bi = blk % NBUF
            col = cols(s)
            if sl == 0 and blk + 2 < nblk and not _os.environ.get('RW_NOLOAD'):
                for g in range(2):
                    load_block(blk + 2, g)
            if s + 1 < nsteps:
                for g in range(2):
                    build_M(s + 1, g)
            pS = []
            for g in range(2):
                p_ = k.psum()
                so = Sb[g][s % 2]
                for d in range(2):
                    k.mm(p_[:, d * 64:(d + 1) * 64], Ml[g][s % 3][:, d, :], so[:, d, :], True, False)
                    k.mm(p_[:, d * 64:(d + 1) * 64], KD[d][g][bi][0:2, col[d], :], VV[d][g][bi][0:2, col[d], :], False, True)
                pS.append(p_)
            for g in range(2):
                sn = Sb[g][(s + 1) % 2]
                for d in range(2):
                    k.stt(sn[:, d, :], prev_state(s, g, d), Wt[d][g][bi][:, col[d]:col[d] + 1], pS[g][:, d * 64:(d + 1) * 64],
                          ALU.mult, ALU.add)
            for g in range(2):
                for d in range(2):
                    k.stt(Hist[g][bi][:, col[d], d, :], prev_state(s, g, d), Wt[d][g][bi][:, col[d]:col[d] + 1],
                          pS[g][:, d * 64:(d + 1) * 64], ALU.mult, ALU.add)
            if sl == SB - 1:
                for g in range(2):
                    block_mul(blk, g)
                if blk > 0:
                    for g in range(2):
                        block_out(blk - 1, g)
        for g in range(2):
            block_out(nblk - 1, g)
        k.barrier()
    with ExitStack() as st:
        bcw = sb(st, nc, 'rlnw', [128, 256], F32)
        bcb = sb(st, nc, 'rlnb', [128, 256], F32)
        k.dma(bcw[:], io['rwkv_ln_w'][l, :].partition_broadcast(128))
        k.dma(bcb[:], io['rwkv_ln_b'][l, :].partition_broadcast(128))
        yt = [sb(st, nc, f'ryt{i}', [128, 2, 256], F32) for i in range(2)]
        bt = [sb(st, nc, f'rbt{i}', [128, 2, 256], F32) for i in range(2)]
        gtt = [sb(st, nc, f'rgtt{i}', [128, 256], F32) for i in range(2)]
        acc = [sb(st, nc, f'racc{i}', [128, 256], F32) for i in range(2)]
        jk = sb(st, nc, 'rjk', [128, 64], F32)
        sm = [sb(st, nc, f'rsm2{i}', [128, 32], F32) for i in range(2)]
        tiles = list(range(64)) + ([64, 65] if need_ctx else [])
        for n_, ti in enumerate(tiles):
            i2 = n_ % 2
            t0 = ti * 128
            y_, b_, g_, a_, s_ = yt[i2], bt[i2], gtt[i2], acc[i2], sm[i2]
            for d in range(2):
                k.dma(y_[:, d, :], io['ryscan_d'][d, t0:t0 + 128, :])
                k.dma(b_[:, d, :], io['rbonus_d'][d, t0:t0 + 128, :])
            k.dma(g_[:], io['rgate_d'][t0:t0 + 128, :])
            k.op('dve', lambda E: E.reduce_sum(s_[:, 0:8], y_[:].rearrange("p d (h e) -> p (d h) e", h=4), AX.X), [y_[:]], [s_[:]])
            k.ts(s_[:, 0:8], s_[:, 0:8], -1.0 / 64, None, ALU.mult)
            for j in range(8):
                d, h = j // 4, j % 4
                k.ts(y_[:, d, h * 64:(h + 1) * 64], y_[:, d, h * 64:(h + 1) * 64], s_[:, j:j + 1], None, ALU.add)
                k.act(jk[:], y_[:, d, h * 64:(h + 1) * 64], AF.Square, accum_out=s_[:, 8 + j:9 + j])
            k.act(s_[:, 16:24], s_[:, 8:16], AF.Sqrt, bias=64e-5, scale=1.0 / 64)
            k.op('dve', lambda E: E.reciprocal(s_[:, 24:32], s_[:, 16:24]), [s_[:]], [s_[:]])
            for j in range(8):
                d, h = j // 4, j % 4
                k.stt(y_[:, d, h * 64:(h + 1) * 64], y_[:, d, h * 64:(h + 1) * 64], s_[:, 24 + j:25 + j], bcw[:, h * 64:(h + 1) * 64],
                      ALU.mult, ALU.mult)
            k.tt(a_[:], y_[:, 0, :], y_[:, 1, :], ALU.add)
            k.tt(b_[:, 0, :], b_[:, 0, :], b_[:, 1, :], ALU.add, e='pool')
            k.stt(a_[:], bcb[:], 2.0, a_[:], ALU.mult, ALU.add)
            k.tt(a_[:], a_[:], b_[:, 0, :], ALU.add)
            k.tt(a_[:], a_[:], g_[:], ALU.mult, e='pool')
            k.dma(io['mix_d'][t0:t0 + 128, 512:768], a_[:], q='pool')
        k.barrier()


Prog.stage_RW = stage_RW


def build_full():
    P = Prog()
    P.declare()
    out = P.nc.dram_tensor('out', [T, D], F32, kind="ExternalOutput").ap()
    for l in range(DEPTH):
        P.stage_M(l)
        P.stage_A(l)
        P.stage_ATT(l)
        P.stage_ML(l)
        P.stage_RW(l)
        P.stage_C(l, out_ap=out)
    return P


def kernel(**inputs):
    inp = {k_: np.asarray(v) for k_, v in inputs.items()}
    P = build_full()
    consts = host_consts()
    in_maps = []
    for b in range(4):
        m = host_inputs(inp, b, consts, P.plans)
        in_maps.append({k_: np.ascontiguousarray(v) for k_, v in m.items() if k_ in P.io})
    res = run_bass_kernel_spmd(P.nc, in_maps, core_ids=list(range(4)))
    return np.stack([r['out'] for r in res.results], axis=0).astype(np.float32)


def stage_C(self, l, out_ap=None):
    k, nc, io = self.k, self.nc, self.io
    last = l == DEPTH - 1
    xsrc = io['x0'] if l == 0 else io['xs']
    groups = [(g * 1024, 1024, 0) for g in range(8)] + ([] if last else [(T, 256, 1)])
    import os as _os
    if 'C_NGROUPS' in _os.environ:
        groups = groups[:int(_os.environ['C_NGROUPS'])]
    with ExitStack() as st:
        identb = sb(st, nc, 'identb', [128, 128], BF16)
        identf = sb(st, nc, 'identf', [128, 128], F32)
        k.dma(identb[:], io['ident_b']); k.dma(identf[:], io['ident_f'])
        routw = sb(st, nc, 'routw', [128, 8, NE], F32)
        routb = sb(st, nc, 'routb', [128, NE], F32)
        b2 = sb(st, nc, 'b2', [NE, D], F32)
        b1c = [sb(st, nc, f'b1c{i}', [128, 16], F32) for i in range(2)]
        wstg = [sb(st, nc, f'cwstg{i}', [128, 1024], F32) for i in range(2)]
        k.dma(routw[:], io['router_w'][l].rearrange("(kc p) e -> p kc e", p=128))
        k.dma(routb[:], io['router_b'][l, :].partition_broadcast(128))
        k.dma(b2[:], io['moe_b2'][l])
        fng = None
        if last:
            fng = sb(st, nc, 'fng', [128, D], F32)
            k.dma(fng[:], io['final_norm_g'].partition_broadcast(128))
        bct = {i: sb(st, nc, f'cbc{i}', [128, D], F32) for i in (2, 3, 4, 5)}
        yacc = sb(st, nc, 'yacc', [128, 8, D], F32)
        h2T = sb(st, nc, 'h2T', [128, 8, 1024], BF16)
        gates = sb(st, nc, 'gates', [128, 8, NE], F32)
        W1s = [sb(st, nc, f'W1b{i}', [128, 8, 2048], BF16) for i in range(2)]
        W2s = [sb(st, nc, f'W2b{i}', [128, 8, D], BF16) for i in range(2)]
        actT = sb(st, nc, 'actT', [128, 8, 512], BF16)
        xt = [sb(st, nc, f'cxt{i}', [128, D], F32) for i in range(1)]
        mixT = sb(st, nc, 'cmixT', [128, 8, 128], BF16)
        tmp = sb(st, nc, 'ctmp', [128, D], F32)
        mixf = tmp
        hb = sb(st, nc, 'chb', [128, D], BF16)
        junk = hb
        mixb = hb
        h2fT = actT[:].rearrange("p a b -> p (a b)").bitcast(F32)[:, 0:1024].rearrange("p (kc t) -> p kc t", kc=8)
        ssq = sb(st, nc, 'cssq', [128, 4], F32)
        rt = [sb(st, nc, f'crt{i}', [128, 96], F32) for i in range(2)]
        gT = sb(st, nc, 'cgT', [NE, 128], F32)
        ev = [sb(st, nc, f'cev{i}', [128, 512], F32) for i in range(3)]
        ecnt = [0]
        evc = [0]

        def EV():
            evc[0] += 1
            return ev[evc[0] % 3]

        cur_seg = [None]
        wc = [0]
        for (g0, ntg, seg) in groups:
            if cur_seg[0] != seg:
                for i in (2, 3, 4, 5):
                    k.dma(bct[i][:], io[f'modR{l}'][seg, i * D:(i + 1) * D].partition_broadcast(128))
                cur_seg[0] = seg
            G2, A2, S2, G5 = bct[2], bct[3], bct[4], bct[5]
            nti = ntg // 128
            Wout = W2s[0]
            for kc in range(8):
                s_ = wstg[kc % 2]
                k.dma(s_[:], io['w_out'][l, kc * 128:(kc + 1) * 128, :])
                k.copy(Wout[:, kc, :], s_[:], e='pool')
            for ti in range(nti):
                t0 = g0 + ti * 128
                x_ = xt[0]
                k.dma(x_[:], xsrc[t0:t0 + 128, :])
                k.dma(mixf[:], io['mix_d'][t0:t0 + 128, :])
                k.copy(mixb[:], mixf[:], e='pool')
                ps = k.psum()
                pb = ps[:].bitcast(BF16)
                for kc in range(8):
                    k.tr(pb[:, kc * 128:(kc + 1) * 128], mixb[:, kc * 128:(kc + 1) * 128], identb[:])
                k.copy(mixT[:], pb[:, 0:1024].rearrange("p (kc t) -> p kc t", kc=8), e='act')
                for half in range(2):
                    ps = k.psum()
                    for kc in range(8):
                        k.mm(ps[:, :], mixT[:, kc, :], Wout[:, kc, half * 512:(half + 1) * 512], kc == 0, kc == 7)
                    k.tt(tmp[:, half * 512:(half + 1) * 512], ps[:, :], G2[:, half * 512:(half + 1) * 512], ALU.mult)
                k.tt(x_[:], x_[:], tmp[:], ALU.add, e='pool')
                k.dma(io['xs'][t0:t0 + 128, :], x_[:], q='pool')
                k.act(junk[:], x_[:], AF.Square, accum_out=ssq[:, 0:1])
                k.act(ssq[:, 1:2], ssq[:, 0:1], AF.Sqrt, bias=1e-6, scale=1.0 / D)
                k.op('dve', lambda E: E.reciprocal(ssq[:, 2:3], ssq[:, 1:2]), [ssq[:]], [ssq[:]])
                k.stt(tmp[:], x_[:], ssq[:, 2:3], A2[:], ALU.mult, ALU.mult)
                k.tt(tmp[:], tmp[:], S2[:], ALU.add, e='pool')
                k.copy(hb[:], tmp[:], e='pool')
                ps = k.psum()
                pb = ps[:].bitcast(BF16)
                for kc in range(8):
                    k.tr(pb[:, kc * 128:(kc + 1) * 128], hb[:, kc * 128:(kc + 1) * 128], identb[:])
                k.copy(h2T[:, :, ti * 128:(ti + 1) * 128], pb[:, 0:1024].rearrange("p (kc t) -> p kc t", kc=8), e='act')
                for hh in range(2):
                    ps = k.psum()
                    for q_ in range(4):
                        kc = hh * 4 + q_
                        k.tr(ps[:, q_ * 128:(q_ + 1) * 128], tmp[:, kc * 128:(kc + 1) * 128], identf[:])
                    k.copy(h2fT[:, hh * 4:(hh + 1) * 4, :], ps[:, :].rearrange("p (kc t) -> p kc t", kc=4), e='act')
                ps = k.psum()
                for kc in range(8):
                    k.mm(ps[:, 0:NE], h2fT[:, kc, :], routw[:, kc, :], kc == 0, kc == 7)
                r_ = rt[ti % 2]
                k.tt(r_[:, 0:32], ps[:, 0:NE], routb[:], ALU.add)
                k.op('dve', lambda E: E.max(r_[:, 32:40], r_[:, 0:32]), [r_[:]], [r_[:]])
                k.ts(r_[:, 40:41], r_[:, 32:33], -1.0, None, ALU.mult)
                k.ts(r_[:, 64:96], r_[:, 0:32], r_[:, 35:36], None, ALU.is_ge)
                k.act(r_[:, 0:32], r_[:, 0:32], AF.Exp, bias=r_[:, 40:41])
                k.tt(r_[:, 0:32], r_[:, 0:32], r_[:, 64:96], ALU.mult)
                k.op('dve', lambda E: E.reduce_sum(r_[:, 41:42], r_[:, 0:32], AX.X), [r_[:]], [r_[:]])
                k.op('dve', lambda E: E.reciprocal(r_[:, 42:43], r_[:, 41:42]), [r_[:]], [r_[:]])
                k.ts(gates[:, ti, :], r_[:, 0:32], r_[:, 42:43], None, ALU.mult)
                ps = k.psum()
                k.tr(ps[0:NE, 0:128], gates[:, ti, :], identf[:])
                k.copy(gT[:], ps[0:NE, 0:128], e='act')
                for half in range(2):
                    ps = k.psum()
                    k.mm(ps[:, :], gT[:], b2[:, half * 512:(half + 1) * 512], True, True)
                    k.copy(yacc[:, ti, half * 512:(half + 1) * 512], ps[:, :], e='act')
            def weight_tasks(e):
                W1b, W2b = W1s[(e + 1) % 2], W2s[(e + 1) % 2]
                tasks = []

                def t_b1():
                    k.dma(b1c[e % 2][:], io['moe_b1'][l, e, :].rearrange("(c p) -> p c", p=128), allow_slow_non_contiguous=True)
                tasks.append(t_b1)
                for kc in range(8):
                    for half in range(2):
                        def t1(kc=kc, half=half):
                            s_ = wstg[wc[0] % 2]
                            wc[0] += 1
                            k.dma(s_[:], io['moe_w1'][l, e, kc * 128:(kc + 1) * 128, half * 1024:(half + 1) * 1024])
                            k.copy(W1b[:, kc, half * 1024:(half + 1) * 1024], s_[:], e='act')
                        tasks.append(t1)
                for fc in range(8):
                    def t2(fc=fc):
                        s_ = wstg[wc[0] % 2]
                        wc[0] += 1
                        k.dma(s_[:], io['moe_w2'][l, e, fc * 128:(fc + 1) * 128, :])
                        k.copy(W2b[:, fc, :], s_[:], e='act')
                    tasks.append(t2)
                return tasks

            for t_ in weight_tasks(0):
                t_()
            for e in range(NE):
                b1 = b1c[e % 2]
                W1b, W2b = W1s[(e + 1) % 2], W2s[(e + 1) % 2]
                pend = weight_tasks(e + 1) if e + 1 < NE else []
                hsz = min(512, ntg)
                nslots = (ntg // hsz) * (8 + (hsz // 128) * 2)
                per = -(-len(pend) // nslots) if pend else 0

                def drain():
                    for _ in range(per):
                        if pend:
                            pend.pop(0)()

                for hg in range(ntg // hsz):
                    c0 = hg * hsz
                    for i in range(8):
                        pg = k.psum()
                        for kc in range(8):
                            k.mm(pg[:, 0:hsz], W1b[:, kc, i * 128:(i + 1) * 128], h2T[:, kc, c0:c0 + hsz], kc == 0, kc == 7)
                        pl = k.psum()
                        for kc in range(8):
                            k.mm(pl[:, 0:hsz], W1b[:, kc, 1024 + i * 128:1024 + (i + 1) * 128], h2T[:, kc, c0:c0 + hsz], kc == 0, kc == 7)
                        g_, sg, lt = EV(), EV(), EV()
                        k.ts(g_[:, 0:hsz], pg[:, 0:hsz], b1[:, i:i + 1], 7.0, ALU.add, ALU.min)
                        k.act(sg[:, 0:hsz], g_[:, 0:hsz], AF.Silu, scale=1.702)
                        k.ts(lt[:, 0:hsz], pl[:, 0:hsz], b1[:, 8 + i:9 + i], 7.0, ALU.add, ALU.min)
                        k.ts(lt[:, 0:hsz], lt[:, 0:hsz], -7.0, 1.0, ALU.max, ALU.add)
                        k.stt(actT[:, i, 0:hsz], sg[:, 0:hsz], 1.0 / 1.702, lt[:, 0:hsz], ALU.mult, ALU.mult)
                        drain()
                    for tt_ in range(hsz // 128):
                        ti = (c0 // 128) + tt_
                        for half in range(2):
                            po = k.psum()
                            for fc in range(8):
                                k.mm(po[:, :], actT[:, fc, tt_ * 128:(tt_ + 1) * 128], W2b[:, fc, half * 512:(half + 1) * 512], fc == 0, fc == 7)
                            ys = yacc[:, ti, half * 512:(half + 1) * 512]
                            k.stt(ys, po[:, :], gates[:, ti, e:e + 1], ys, ALU.mult, ALU.add)
                            drain()
                while pend:
                    pend.pop(0)()
            for ti in range(nti):
                t0 = g0 + ti * 128
                x_ = xt[0]
                k.dma(x_[:], io['xs'][t0:t0 + 128, :])
                k.tt(tmp[:], yacc[:, ti, :], G5[:], ALU.mult)
                k.tt(x_[:], x_[:], tmp[:], ALU.add, e='pool')
                if not last:
                    k.dma(io['xs'][t0:t0 + 128, :], x_[:], q='pool')
                else:
                    k.act(junk[:], x_[:], AF.Square, accum_out=ssq[:, 0:1])
                    k.act(ssq[:, 1:2], ssq[:, 0:1], AF.Sqrt, bias=1e-6, scale=1.0 / D)
                    k.op('dve', lambda E: E.reciprocal(ssq[:, 2:3], ssq[:, 1:2]), [ssq[:]], [ssq[:]])
                    k.stt(tmp[:], x_[:], ssq[:, 2:3], fng[:], ALU.mult, ALU.mult)
                    k.dma(out_ap[t0:t0 + 128, :], tmp[:], q='pool')
        k.barrier()


Prog.stage_C = stage_C
```
